# Optimizing a Trainium2 kernel written in Bass

```python
import jax
import jax.numpy as jnp
from jax import lax
import numpy as np

D_MODEL = 1024
BATCH = 4
SEQ = 8192
DEPTH = 2

NORM_EPS = 1e-6
N_BRANCH = 3
N_ADA = 6

LRU_WIDTH = D_MODEL
LRU_BLOCKS = 16
LRU_BLOCK_DIM = LRU_WIDTH // LRU_BLOCKS
CONV_WIDTH = 4
LRU_C = 8.0

SB_HEADS = 8
SB_HEAD_DIM = D_MODEL // SB_HEADS
SB_WIDTH = SB_HEADS * SB_HEAD_DIM
Q_BLOCK = 128

RW_HEAD_DIM = 64
RW_HEADS = D_MODEL // RW_HEAD_DIM
RW_WIDTH = RW_HEADS * RW_HEAD_DIM
DECAY_LORA = 64
AAA_LORA = 64
GATE_LORA = 128
RW_IN = 3 * RW_WIDTH + DECAY_LORA + AAA_LORA + GATE_LORA
RW_GN_EPS = 1e-5 * RW_HEAD_DIM

N_IN = 2 * LRU_WIDTH + 3 * SB_WIDTH + RW_IN + N_BRANCH * D_MODEL

N_EXPERTS = 32
TOP_K = 4
D_FF = D_MODEL
SWIGLU_LIMIT = 7.0
SWIGLU_ALPHA = 1.702
EXPERT_BLOCK = 256

kernel_name = 'hybrid_rglru_stickbreak_rwkv7_moe_adaln'


def _split_at(t, widths):
    idx = [int(i) for i in np.cumsum(widths)[:-1]]
    return jnp.split(t, idx, axis=-1)


def rmsnorm(x, gain):
    xf = x.astype(jnp.float32)
    y = xf * lax.rsqrt(jnp.mean(xf * xf, axis=-1, keepdims=True) + NORM_EPS)
    return (y * gain.astype(jnp.float32)).astype(x.dtype)


def modulate(h, shift, scale):
    return h * (1 + scale[:, None, :]) + shift[:, None, :]


def shift_right(t, n):
    return jnp.pad(t, ((0, 0), (n, 0), (0, 0)))[:, : t.shape[1], :]


def _linear_combine(left, right):
    a_l, b_l = left
    a_r, b_r = right
    return a_l * a_r, a_r * b_l + b_r


def rglru_branch(x_in, x_gate, conv_w, conv_b, wa, ba, wx, bx, lam):
    bsz, seq, _ = x_in.shape
    xc = conv_b + sum(conv_w[k] * shift_right(x_in, k) for k in range(CONV_WIDTH))
    xb = xc.reshape(bsz, seq, LRU_BLOCKS, LRU_BLOCK_DIM)
    r = jax.nn.sigmoid(jnp.einsum('bsni,nij->bsnj', xb, wa).reshape(bsz, seq, LRU_WIDTH) + ba)
    i = jax.nn.sigmoid(jnp.einsum('bsni,nij->bsnj', xb, wx).reshape(bsz, seq, LRU_WIDTH) + bx)
    log_a = -LRU_C * r.astype(jnp.float32) * jax.nn.softplus(-lam.astype(jnp.float32))
    a = jnp.exp(log_a)
    b = jnp.sqrt(-jnp.expm1(2.0 * log_a)) * (i * xc).astype(jnp.float32)
    _, h = lax.associative_scan(_linear_combine, (a, b), axis=1)
    return jax.nn.gelu(x_gate) * h.astype(x_in.dtype)


def stick_breaking_attention(q, k, v):
    bsz, seq, n_heads, head_dim = q.shape
    n_blk = seq // Q_BLOCK
    f32 = jnp.float32
    qf = q.astype(f32) * (head_dim ** -0.5)
    kf = k.astype(f32)
    vf = v.astype(f32)
    qb = qf.reshape(bsz, n_blk, Q_BLOCK, n_heads, head_dim).transpose(1, 0, 3, 2, 4)
    kpos = jnp.arange(seq)

    def one_block(args):
        q_blk, blk = args
        qpos = blk * Q_BLOCK + jnp.arange(Q_BLOCK)
        z = jnp.einsum('bhqd,bkhd->bhqk', q_blk, kf)
        mask = kpos[None, :] < qpos[:, None]
        log_stay = jnp.where(mask, jax.nn.log_sigmoid(-z), 0.0)
        later = lax.cumsum(log_stay, axis=3, reverse=True) - log_stay
        attn = jnp.where(mask, jnp.exp(jax.nn.log_sigmoid(z) + later), 0.0)
        return jnp.einsum('bhqk,bkhd->bqhd', attn, vf)

    out = lax.map(one_block, (qb, jnp.arange(n_blk)))
    return out.transpose(1, 0, 2, 3, 4).reshape(bsz, seq, n_heads * head_dim).astype(q.dtype)


def rwkv7_branch(cols, mu, w0, w_up, a0, a_up, g_up, k_k, k_a, r_k, lnx_w, lnx_b):
    bsz, seq, _ = cols.shape
    f32 = jnp.float32
    cols = cols + mu * (shift_right(cols, 1) - cols)
    r, k, v, xw, xa, xg = _split_at(cols, (RW_WIDTH, RW_WIDTH, RW_WIDTH, DECAY_LORA, AAA_LORA, GATE_LORA))
    w_log = -jax.nn.softplus(-(w0 + jnp.tanh(xw) @ w_up).astype(f32)) - 0.5
    decay = jnp.exp(-jnp.exp(w_log))
    a = jax.nn.sigmoid(a0 + xa @ a_up)
    g = jax.nn.sigmoid(xg) @ g_up

    def heads(t):
        return t.reshape(bsz, seq, RW_HEADS, RW_HEAD_DIM).astype(f32)

    kk = heads(k * k_k)
    kk = kk / jnp.maximum(jnp.linalg.norm(kk, axis=-1, keepdims=True), 1e-12)
    k = k * (1 + (a - 1) * k_a)
    r_h, k_h, v_h, a_h, w_h = heads(r), heads(k), heads(v), heads(a), heads(decay)

    def step(state, inp):
        r_t, w_t, k_t, v_t, kk_t, a_t = inp
        sa = jnp.einsum('bhij,bhj->bhi', state, -kk_t)
        state = (state * w_t[:, :, None, :]
                 + sa[..., None] * (kk_t * a_t)[:, :, None, :]
                 + v_t[..., None] * k_t[:, :, None, :])
        return state, jnp.einsum('bhij,bhj->bhi', state, r_t)

    def time_major(t):
        return jnp.swapaxes(t, 0, 1)

    state0 = jnp.zeros((bsz, RW_HEADS, RW_HEAD_DIM, RW_HEAD_DIM), f32)
    _, y = lax.scan(step, state0, tuple(time_major(t) for t in (r_h, w_h, k_h, v_h, kk, a_h)))
    y = time_major(y)
    mean = jnp.mean(y, axis=-1, keepdims=True)
    var = jnp.mean(jnp.square(y - mean), axis=-1, keepdims=True)
    y = ((y - mean) * lax.rsqrt(var + RW_GN_EPS)).reshape(bsz, seq, RW_WIDTH) * lnx_w + lnx_b
    bonus = jnp.sum(r_h * k_h * r_k, axis=-1, keepdims=True) * v_h
    y = y + bonus.reshape(bsz, seq, RW_WIDTH)
    return (y * g).astype(cols.dtype)


def hybrid_mixer(h, w_in, conv_w, conv_b, lru_wa, lru_ba, lru_wx, lru_bx, lru_lambda,
                 rw_mu, rw_w0, rw_w_up, rw_a0, rw_a_up, rw_g_up, rw_k_k, rw_k_a, rw_r_k,
                 rw_lnx_w, rw_lnx_b, p_lru, p_sb, p_rwkv, w_out):
    bsz, seq, _ = h.shape
    cols = h @ w_in
    lru_x, lru_gate, q, k, v, rw_cols, gate_logits = _split_at(
        cols, (LRU_WIDTH, LRU_WIDTH, SB_WIDTH, SB_WIDTH, SB_WIDTH, RW_IN, N_BRANCH * D_MODEL))
    y_a = rglru_branch(lru_x, lru_gate, conv_w, conv_b, lru_wa, lru_ba, lru_wx, lru_bx, lru_lambda)

    def sb_heads(t):
        return t.reshape(bsz, seq, SB_HEADS, SB_HEAD_DIM)

    y_b = stick_breaking_attention(sb_heads(q), sb_heads(k), sb_heads(v))
    y_c = rwkv7_branch(rw_cols, rw_mu, rw_w0, rw_w_up, rw_a0, rw_a_up, rw_g_up,
                       rw_k_k, rw_k_a, rw_r_k, rw_lnx_w, rw_lnx_b)
    g_a, g_b, g_c = jnp.split(jax.nn.sigmoid(gate_logits), N_BRANCH, axis=-1)
    merged = g_a * (y_a @ p_lru) + g_b * (y_b @ p_sb) + g_c * (y_c @ p_rwkv)
    return merged @ w_out


def moe_ffn(h, w_router, b_router, w_gu, b_gu, w_down, b_down):
    bsz, seq, d = h.shape
    x = h.reshape(-1, d)
    n_tok = x.shape[0]
    logits = (x @ w_router + b_router).astype(jnp.float32)
    top_logit, top_idx = lax.top_k(logits, TOP_K)
    top_w = jax.nn.softmax(top_logit, axis=-1)
    n_assign = n_tok * TOP_K
    flat_e = top_idx.reshape(-1)
    order = jnp.argsort(flat_e)
    sorted_e = flat_e[order]
    counts = jnp.bincount(flat_e, length=N_EXPERTS)
    padded = (counts + EXPERT_BLOCK - 1) // EXPERT_BLOCK * EXPERT_BLOCK
    pad_end = jnp.cumsum(padded)
    pad_start = pad_end - padded
    start = jnp.cumsum(counts) - counts
    slot = pad_start[sorted_e] + jnp.arange(n_assign) - start[sorted_e]
    n_blocks = -(-n_assign // EXPERT_BLOCK) + N_EXPERTS
    n_slots = n_blocks * EXPERT_BLOCK
    slot_token = jnp.zeros((n_slots,), jnp.int32).at[slot].set((order // TOP_K).astype(jnp.int32))
    slot_weight = jnp.zeros((n_slots,), jnp.float32).at[slot].set(top_w.reshape(-1)[order])
    block_expert = jnp.minimum(
        jnp.searchsorted(pad_end, jnp.arange(n_blocks) * EXPERT_BLOCK, side='right'), N_EXPERTS - 1)

    def expert_block(args):
        tok, e = args
        xb = x[tok]
        gu = xb @ w_gu[e] + b_gu[e]
        gate, up = jnp.split(gu, 2, axis=-1)
        gate = jnp.minimum(gate, SWIGLU_LIMIT)
        up = jnp.clip(up, -SWIGLU_LIMIT, SWIGLU_LIMIT)
        act = (up + 1) * gate * jax.nn.sigmoid(SWIGLU_ALPHA * gate)
        return act @ w_down[e] + b_down[e]

    yb = lax.map(expert_block, (slot_token.reshape(n_blocks, EXPERT_BLOCK), block_expert))
    y = jnp.zeros_like(x).at[slot_token].add(yb.reshape(-1, d) * slot_weight[:, None].astype(x.dtype))
    return y.reshape(bsz, seq, d)


def setup_inputs(seed: int = 0) -> dict:
    key = jax.random.key(seed)
    ks = iter(jax.random.split(key, 48))
    L, D = DEPTH, D_MODEL

    def nrm(shape, scale):
        return jax.random.normal(next(ks), shape, jnp.float32) * scale

    def unif(shape, lo, hi):
        return jax.random.uniform(next(ks), shape, jnp.float32, lo, hi)

    u = unif((L, LRU_WIDTH), 0.9, 0.999) ** (1.0 / LRU_C)
    lru_lambda = jnp.log(u) - jnp.log1p(-u)
    return {
        'x': nrm((BATCH, SEQ, D), 1.0),
        'c': nrm((BATCH, D), 1.0),
        'w_ada': nrm((L, D, N_ADA * D), 0.02),
        'b_ada': nrm((L, N_ADA * D), 0.01),
        'norm_mix': 1.0 + nrm((L, D), 0.02),
        'norm_moe': 1.0 + nrm((L, D), 0.02),
        'norm_final': 1.0 + nrm((D,), 0.02),
        'w_in': nrm((L, D, N_IN), D ** -0.5),
        'conv_w': nrm((L, CONV_WIDTH, LRU_WIDTH), CONV_WIDTH ** -0.5),
        'conv_b': nrm((L, LRU_WIDTH), 0.01),
        'lru_wa': nrm((L, LRU_BLOCKS, LRU_BLOCK_DIM, LRU_BLOCK_DIM), LRU_BLOCK_DIM ** -0.5),
        'lru_ba': nrm((L, LRU_WIDTH), 0.01),
        'lru_wx': nrm((L, LRU_BLOCKS, LRU_BLOCK_DIM, LRU_BLOCK_DIM), LRU_BLOCK_DIM ** -0.5),
        'lru_bx': nrm((L, LRU_WIDTH), 0.01),
        'lru_lambda': lru_lambda,
        'rw_mu': unif((L, RW_IN), 0.0, 1.0),
        'rw_w0': unif((L, RW_WIDTH), -6.0, -1.0),
        'rw_w_up': nrm((L, DECAY_LORA, RW_WIDTH), 0.1),
        'rw_a0': nrm((L, RW_WIDTH), 0.1),
        'rw_a_up': nrm((L, AAA_LORA, RW_WIDTH), 0.1),
        'rw_g_up': nrm((L, GATE_LORA, RW_WIDTH), GATE_LORA ** -0.5),
        'rw_k_k': 0.85 + nrm((L, RW_WIDTH), 0.02),
        'rw_k_a': 1.0 + nrm((L, RW_WIDTH), 0.02),
        'rw_r_k': nrm((L, RW_HEADS, RW_HEAD_DIM), 0.1),
        'rw_lnx_w': 1.0 + nrm((L, RW_WIDTH), 0.02),
        'rw_lnx_b': nrm((L, RW_WIDTH), 0.01),
        'p_lru': nrm((L, LRU_WIDTH, D), LRU_WIDTH ** -0.5),
        'p_sb': nrm((L, SB_WIDTH, D), SB_WIDTH ** -0.5),
        'p_rwkv': nrm((L, RW_WIDTH, D), RW_WIDTH ** -0.5),
        'w_out': nrm((L, D, D), D ** -0.5),
        'w_router': nrm((L, D, N_EXPERTS), D ** -0.5),
        'b_router': nrm((L, N_EXPERTS), 0.01),
        'w_gu': nrm((L, N_EXPERTS, D, 2 * D_FF), D ** -0.5),
        'b_gu': nrm((L, N_EXPERTS, 2 * D_FF), 0.01),
        'w_down': nrm((L, N_EXPERTS, D_FF, D), D_FF ** -0.5),
        'b_down': nrm((L, N_EXPERTS, D), 0.01),
    }


def reference(x, c, w_ada, b_ada, norm_mix, norm_moe, norm_final, w_in, conv_w, conv_b,
              lru_wa, lru_ba, lru_wx, lru_bx, lru_lambda, rw_mu, rw_w0, rw_w_up, rw_a0, rw_a_up,
              rw_g_up, rw_k_k, rw_k_a, rw_r_k, rw_lnx_w, rw_lnx_b, p_lru, p_sb, p_rwkv, w_out,
              w_router, b_router, w_gu, b_gu, w_down, b_down):
    c_act = jax.nn.silu(c)
    for l in range(DEPTH):
        ada = c_act @ w_ada[l] + b_ada[l]
        sh_mix, sc_mix, g_mix, sh_ffn, sc_ffn, g_ffn = jnp.split(ada, N_ADA, axis=-1)
        h = modulate(rmsnorm(x, norm_mix[l]), sh_mix, sc_mix)
        mix = hybrid_mixer(h, w_in[l], conv_w[l], conv_b[l], lru_wa[l], lru_ba[l], lru_wx[l], lru_bx[l],
                           lru_lambda[l], rw_mu[l], rw_w0[l], rw_w_up[l], rw_a0[l], rw_a_up[l], rw_g_up[l],
                           rw_k_k[l], rw_k_a[l], rw_r_k[l], rw_lnx_w[l], rw_lnx_b[l],
                           p_lru[l], p_sb[l], p_rwkv[l], w_out[l])
        x = x + g_mix[:, None, :] * mix
        h = modulate(rmsnorm(x, norm_moe[l]), sh_ffn, sc_ffn)
        ffn = moe_ffn(h, w_router[l], b_router[l], w_gu[l], b_gu[l], w_down[l], b_down[l])
        x = x + g_ffn[:, None, :] * ffn
    return rmsnorm(x, norm_final)
```

```python
import concourse.bass as bass
import concourse.mybir as mybir

F32 = mybir.dt.float32
BF16 = mybir.dt.bfloat16
ALU = mybir.AluOpType
AF = mybir.ActivationFunctionType
AX = mybir.AxisListType


class Region:
    __slots__ = ("w", "rs", "name", "excl")

    def __init__(self, name=""):
        self.w = None
        self.rs = {}
        self.name = name
        self.excl = False


class Ins:
    __slots__ = ("eng", "fn", "deps", "sig", "val", "dma", "dsem", "dval", "dprev")

    def __init__(self, eng, fn, dma=False):
        self.eng = eng
        self.fn = fn
        self.deps = []
        self.sig = False
        self.val = 0
        self.dma = dma
        self.dsem = None
        self.dval = 0
        self.dprev = 0


class Prog:
    ENGS = ("pe", "act", "dve", "pool", "sp")
    NDMA = 10

    def __init__(self, nc):
        self.nc = nc
        self.q = {e: [] for e in self.ENGS}
        self.dcount = {"sp": 0, "pool": 0, "act": 0}
        self.out_dmas = []

    def op(self, eng, fn, r=(), w=(), dma=False):
        ins = Ins(eng, fn, dma)
        deps = {}

        def add(d):
            if d is None or d is ins:
                return
            if (not d.dma) and (not dma) and d.eng == "pe" and eng == "pe":
                return
            deps[id(d)] = d

        for reg in r:
            add(reg.w)
            if reg.excl:
                for k, v in reg.rs.items():
                    if k != eng and k != "dma":
                        add(v)
        for reg in w:
            add(reg.w)
            for k, v in reg.rs.items():
                if k == "dma":
                    for d in v:
                        add(d)
                else:
                    add(v)
        ins.deps = list(deps.values())
        for d in ins.deps:
            d.sig = True
        for reg in r:
            if dma:
                reg.rs.setdefault("dma", []).append(ins)
            else:
                reg.rs[eng] = ins
        for reg in w:
            reg.w = ins
            reg.rs = {}
        if dma:
            i = self.dcount[eng]
            self.dcount[eng] = i + 1
            ins.dsem = (eng, i % self.NDMA)
            ins.dval = 16 * (i // self.NDMA + 1)
            ins.dprev = 16 * (i // self.NDMA)
        self.q[eng].append(ins)
        return ins

    def mm(self, out, lhsT, rhs, start=True, stop=True, r=(), w=(), **kw):
        return self.op("pe", lambda e: e.matmul(out, lhsT, rhs, start=start, stop=stop, **kw), r, w)

    def tr(self, out, in_, ident, r=(), w=()):
        return self.op("pe", lambda e: e.transpose(out, in_, ident), r, w)

    def act(self, out, in_, func, r=(), w=(), **kw):
        return self.op("act", lambda e: e.activation(out, in_, func, **kw), r, w)

    def dma(self, out, in_, r=(), w=(), q="sp", is_out=False, **kw):
        ins = self.op(q, lambda e: e.dma_start(out=out, in_=in_, **kw), r, w, dma=True)
        if is_out:
            self.out_dmas.append(ins)
        return ins

    def emit(self):
        nc = self.nc
        for e in self.ENGS:
            c = 0
            for ins in self.q[e]:
                if ins.sig and not ins.dma:
                    c += 1
                    ins.val = c
        import contextlib
        with contextlib.ExitStack() as st:
            esem = {e: st.enter_context(nc.semaphore("es_" + e)) for e in self.ENGS}
            dsem = {}
            for qn in ("sp", "pool"):
                for i in range(self.NDMA):
                    dsem[(qn, i)] = st.enter_context(nc.semaphore(f"ds_{qn}{i}"))
            block = st.enter_context(nc.Block())
            final = self.out_dmas

            def body(ename, eng):
                waited = {}

                def wait(key, sem, val):
                    if val <= 0:
                        return
                    if waited.get(key, 0) >= val:
                        return
                    eng.wait_ge(sem, val)
                    waited[key] = val

                for ins in self.q[ename]:
                    need = {}
                    for d in ins.deps:
                        if d.dma:
                            key, sem, val = d.dsem, dsem[d.dsem], d.dval
                        else:
                            key, sem, val = d.eng, esem[d.eng], d.val
                        if key not in need or need[key][1] < val:
                            need[key] = (sem, val)
                    for key, (sem, val) in need.items():
                        wait(key, sem, val)
                    if ins.dma:
                        wait(ins.dsem, dsem[ins.dsem], ins.dprev)
                    i = ins.fn(eng)
                    if ins.dma:
                        i.then_inc(dsem[ins.dsem], 16)
                    elif ins.sig:
                        i.then_inc(esem[ename], 1)
                if ename == "sp":
                    for d in final:
                        wait(d.dsem, dsem[d.dsem], d.dval)

            @block.tensor
            def _(e):
                body("pe", e)

            @block.scalar
            def _(e):
                body("act", e)

            @block.vector
            def _(e):
                body("dve", e)

            @block.gpsimd
            def _(e):
                body("pool", e)

            @block.sync
            def _(e):
                body("sp", e)
import contextlib
import numpy as np

D = 1024
KC = 8
N_IN = 11520
EPS = 1e-6


class RegMap(dict):
    def __init__(self, P, name, persistent=False):
        super().__init__()
        self.P = P
        self.name = name
        self.persistent = persistent

    def __missing__(self, k):
        r = self.P.region(f"{self.name}{k}", self.persistent)
        self[k] = r
        return r


class Ctx:
    def __init__(self, nc, P, S, debug):
        self.nc = nc
        self.P = P
        self.S = S
        self.debug = debug
        self.dram_regs = {}
        self.stack = None

    def din(self, name, shape, dt=F32):
        return self.nc.dram_tensor(name, list(shape), dt, kind="ExternalInput").ap()

    def dout(self, name, shape, dt=F32):
        return self.nc.dram_tensor(name, list(shape), dt, kind="ExternalOutput").ap()

    def dscr(self, name, shape, dt):
        kind = "ExternalOutput" if self.debug else "Internal"
        return self.nc.dram_tensor(name, list(shape), dt, kind=kind).ap()

    def sb(self, name, shape, dt=F32):
        self.uid = getattr(self, "uid", 0) + 1
        return self.stack.enter_context(self.nc.sbuf_tensor(f"{name}_{self.uid}", list(shape), dt))

    def ps(self, name, shape, dt=F32):
        self.uid = getattr(self, "uid", 0) + 1
        return self.stack.enter_context(self.nc.psum_tensor(f"{name}_{self.uid}", list(shape), dt))

    def regs(self, name, persistent=False):
        return RegMap(self.P, name, persistent)


def prog_extend(Prog):
    def region(self, name="", persistent=False):
        r = Region(name)
        if not persistent:
            r.w = self.cur_bar
            self.phase_regions.append(r)
        return r

    def end_phase(self):
        bar = self.op("sp", lambda e: e.nop(), w=list(self.phase_regions))
        self.cur_bar = bar
        self.phase_regions = []

    def tt(self, eng, out, in0, in1, op, r=(), w=()):
        return self.op(eng, lambda e: e.tensor_tensor(out=out, in0=in0, in1=in1, op=op), r, w)

    def ts(self, eng, out, in0, s1, s2=None, op0=ALU.mult, op1=None, r=(), w=()):
        if op1 is None:
            return self.op(eng, lambda e: e.tensor_scalar(out=out, in0=in0, scalar1=s1, scalar2=None, op0=op0), r, w)
        return self.op(eng, lambda e: e.tensor_scalar(out=out, in0=in0, scalar1=s1, scalar2=s2, op0=op0, op1=op1), r, w)

    def stt(self, out, in0, scalar, in1, op0, op1, r=(), w=()):
        return self.op("dve", lambda e: e.scalar_tensor_tensor(out=out, in0=in0, scalar=scalar, in1=in1, op0=op0, op1=op1), r, w)

    def cp(self, eng, out, in_, r=(), w=()):
        if eng == "act":
            return self.op(eng, lambda e: e.activation(out, in_, AF.Copy), r, w)
        return self.op(eng, lambda e: e.tensor_copy(out=out, in_=in_), r, w)

    def memset(self, eng, ap, val, r=(), w=()):
        return self.op(eng, lambda e: e.memset(ap, val), r, w)

    Prog.region = region
    Prog.end_phase = end_phase
    Prog.tt = tt
    Prog.ts = ts
    Prog.stt = stt
    Prog.cp = cp
    Prog.memset = memset
    Prog.cur_bar = None
    Prog.phase_regions = []


def make_consts():
    cols = {}
    parts = []
    off = 0

    def add(name, arr):
        nonlocal off
        arr = np.asarray(arr, np.float32).reshape(128, -1)
        cols[name] = (off, arr.shape[1])
        parts.append(arr)
        off += arr.shape[1]

    i = np.arange(128)
    add("ident", np.eye(128))
    add("ones", np.ones((128, 128)))
    add("tri_ge", (i[:, None] >= i[None, :]))
    add("tri_lt", (i[:, None] < i[None, :]))
    add("tri_le", (i[:, None] <= i[None, :]))
    t = np.arange(512)
    m = np.stack([(128 * k + i[:, None] < t[None, :]) for k in range(4)], axis=1)
    add("sbmask", m)
    add("m_sl", (i[None, :] < i[:, None]))
    add("m_li", (i[None, :] <= i[:, None]))
    add("m_su", (i[:, None] < i[None, :]))
    add("m_ui", (i[:, None] <= i[None, :]))
    add("mask3", np.concatenate([(i[None, :] < i[:, None]), (i[:, None] < i[None, :]), (i[:, None] < i[None, :])], axis=1))
    return np.concatenate(parts, axis=1), cols


def phase_adaln(C, l, cst, cvec_d, w_ada_d, b_ada_d, nmix_d, nmoe_d, per):
    P, nc = C.P, C.nc
    with contextlib.ExitStack() as st:
        C.stack = st
        R = C.regs("p0_")
        cact = C.sb("cact", [128, 8])
        wst = [C.sb(f"wada{i}", [128, 8, 768]) for i in range(2)]
        ada = C.sb("ada", [128, 48])
        bada = C.sb("bada", [128, 48])
        nm = C.sb("nm", [128, 16])
        psa = C.ps("psa", [128, 48])
        P.dma(cact[:], cvec_d[:, :], w=[R["cact"]])
        P.dma(bada[:], b_ada_d[:, :], w=[R["bada"]])
        P.dma(nm[:, 0:8], nmix_d[:, :], w=[R["nm"]])
        P.dma(nm[:, 8:16], nmoe_d[:, :], w=[R["nm"]])
        P.act(cact[:], cact[:], AF.Silu, r=[R["cact"]], w=[R["cact"]])
        wv = w_ada_d.rearrange("(k p) j -> p k j", p=128)
        for g in range(8):
            wt = wst[g % 2]
            P.dma(wt[:], wv[:, :, g * 768:(g + 1) * 768], w=[R[f"w{g % 2}"]])
            for cc in range(6):
                j = g * 6 + cc
                for k in range(8):
                    P.mm(psa[:, j:j + 1], wt[:, k, cc * 128:(cc + 1) * 128], cact[:, k:k + 1],
                         start=(k == 0), stop=(k == 7), r=[R[f"w{g % 2}"], R["cact"]], w=[R["psa"]])
        P.tt("dve", ada[:], psa[:], bada[:], ALU.add, r=[R["psa"], R["bada"]], w=[R["ada"]])
        Rp = per["reg"]
        pt = per["t"]
        for (dst, sc_i, nofs) in ((0, 1, 0), (3, 4, 8)):
            P.ts("dve", pt[:, dst, :], ada[:, sc_i * 8:(sc_i + 1) * 8], 1.0, None, op0=ALU.add, r=[R["ada"]], w=[Rp])
            P.tt("dve", pt[:, dst, :], pt[:, dst, :], nm[:, nofs:nofs + 8], ALU.mult, r=[Rp, R["nm"]], w=[Rp])
        for (dst, src) in ((1, 0), (2, 2), (4, 3), (5, 5)):
            P.cp("dve", pt[:, dst, :], ada[:, src * 8:(src + 1) * 8], r=[R["ada"]], w=[Rp])
    P.end_phase()


def emit_norm_tile(C, R, xt, hT_out_fn, scale_ap, shift_ap, ones_f, sq, rs, tmp, ps_s, TT, eps_ap, tag):
    P = C.P
    P.act(sq[:], xt, AF.Square, r=[R["xt" + tag]], w=[R["sq"]])
    for k in range(8):
        P.mm(ps_s[:, :TT], ones_f, sq[:, k, :], start=(k == 0), stop=(k == 7), r=[R["sq"]], w=[R["ps_s"]])
    P.act(rs[:], ps_s[:, :TT], AF.Ln, scale=1.0 / 1024, bias=eps_ap, r=[R["ps_s"]], w=[R["rs"]])
    P.act(rs[:], rs[:], AF.Exp, scale=-0.5, r=[R["rs"]], w=[R["rs"]])
    for k in range(8):
        if shift_ap is None:
            P.stt(hT_out_fn(k), xt[:, k, :], scale_ap[:, k:k + 1], rs[:], ALU.mult, ALU.mult,
                  r=[R["xt" + tag], R["rs"]], w=[R["hT"]])
        else:
            P.stt(tmp[:, k, :], xt[:, k, :], scale_ap[:, k:k + 1], rs[:], ALU.mult, ALU.mult,
                  r=[R["xt" + tag], R["rs"]], w=[R["tmp"]])
            P.ts("pool", hT_out_fn(k), tmp[:, k, :], shift_ap[:, k:k + 1], None, op0=ALU.add,
                 r=[R["tmp"]], w=[R["hT"]])


def phase_proj(C, l, cst, xT_d, w_in_d, mu_d, per, scr):
    P, nc, S = C.P, C.nc, C.S
    TT = 256
    with contextlib.ExitStack() as st:
        C.stack = st
        R = C.regs("p1_")
        import os
        TOK = min(int(os.environ.get("PROJ_TOK", "4096")), S)
        hT = C.sb("hT", [128, 8, TOK + 1], BF16)
        off = {"p0": 0}
        xb = [C.sb(f"xb{i}", [128, 8, TT]) for i in range(2)]
        sq = C.sb("sq", [128, 8, TT])
        tmp = C.sb("tmp", [128, 8, TT])
        rs = C.sb("rs", [128, TT])
        ps_s = C.ps("ps_s", [128, 512])
        pt = per["t"]
        xv = xT_d.rearrange("(k p) s -> p k s", p=128)
        def norm_pass(p0):
            if p0 == 0:
                P.memset("pool", hT[:, :, 0:1], 0.0, w=[R["hT"]])
            else:
                P.cp("dve", hT[:, :, 0:1], hT[:, :, TOK:TOK + 1], r=[R["hT"]], w=[R["hT"]])
            for ti in range(TOK // TT):
                xt = xb[ti % 2]
                tag = str(ti % 2)
                P.dma(xt[:], xv[:, :, p0 + ti * TT:p0 + (ti + 1) * TT], w=[R["xt" + tag]])
                emit_norm_tile(C, R, xt[:], lambda k: hT[:, k, 1 + ti * TT:1 + (ti + 1) * TT],
                               pt[:, 0, :], pt[:, 1, :], cst["ones"], sq, rs, tmp, ps_s, TT, cst["eps"], tag)
        wst = [C.sb(f"wst{i}", [128, 8, 512]) for i in range(2)]
        wb = [C.sb(f"wb{i}", [128, 8, 512], BF16) for i in range(2)]
        wb2 = [C.sb(f"wb2{i}", [128, 8, 512], BF16) for i in range(2)]
        mug = [C.sb(f"mug{i}", [128, 512]) for i in range(2)]
        omug = [C.sb(f"omug{i}", [128, 512]) for i in range(2)]
        ev = [C.sb(f"ev{i}", [128, 512]) for i in range(4)]
        evb = [C.sb(f"evb{i}", [128, 512], BF16) for i in range(4)]
        pso = [C.ps(f"pso{i}", [128, 512]) for i in range(6)]
        wv = w_in_d.rearrange("(k p) j -> p k j", p=128)
        cnt = {"g": 0, "ps": 0, "ev": 0}
        NT5 = TOK // 512
        NT1 = TOK // 128

        def load_group(c0, width, rw):
            g = cnt["g"] % 2
            cnt["g"] += 1
            P.dma(wst[g][:, :, :width], wv[:, :, c0:c0 + width], w=[R[f"wst{g}"]])
            if not rw:
                P.cp("pool", wb[g][:, :, :width], wst[g][:, :, :width], r=[R[f"wst{g}"]], w=[R[f"wb{g}"]])
                return wb[g], None, [R[f"wb{g}"]]
            m0 = c0 - 5120
            P.dma(mug[g][:, :width], mu_d[:, m0:m0 + width], w=[R[f"mug{g}"]])
            P.ts("pool", omug[g][:, :width], mug[g][:, :width], -1.0, 1.0, op0=ALU.mult, op1=ALU.add,
                 r=[R[f"mug{g}"]], w=[R[f"omug{g}"]])
            for k in range(8):
                P.tt("pool" if k % 2 else "dve", wb[g][:, k, :width], wst[g][:, k, :width], omug[g][:, :width], ALU.mult,
                     r=[R[f"wst{g}"], R[f"omug{g}"]], w=[R[f"wb{g}"]])
                P.tt("dve" if k % 2 else "pool", wb2[g][:, k, :width], wst[g][:, k, :width], mug[g][:, :width], ALU.mult,
                     r=[R[f"wst{g}"], R[f"mug{g}"]], w=[R[f"wb2{g}"]])
            return wb[g], wb2[g], [R[f"wb{g}"], R[f"wb2{g}"]]

        def next_ps():
            i = cnt["ps"] % 6
            cnt["ps"] += 1
            return pso[i], R[f"pso{i}"]

        def next_ev(bf):
            i = cnt["ev"] % 4
            cnt["ev"] += 1
            return (evb[i], R[f"evb{i}"]) if bf else (ev[i], R[f"ev{i}"])

        def fm_group(c0, width, rw, evac):
            w1, w2, wr = load_group(c0, width, rw)
            for cc in range(width // 128):
                for tt_ in range(NT5):
                    pt_, pr = next_ps()
                    n = 16 if rw else 8
                    for k in range(8):
                        P.mm(pt_[:], w1[:, k, cc * 128:(cc + 1) * 128], hT[:, k, 1 + tt_ * 512:1 + (tt_ + 1) * 512],
                             start=(k == 0), stop=(k == 7 and not rw), r=wr + [R["hT"]], w=[pr])
                    if rw:
                        for k in range(8):
                            P.mm(pt_[:], w2[:, k, cc * 128:(cc + 1) * 128], hT[:, k, tt_ * 512:(tt_ + 1) * 512],
                                 start=False, stop=(k == 7), r=wr + [R["hT"]], w=[pr])
                    evac(c0 + cc * 128, tt_, pt_, pr)

        def tm_group(c0, width, rw, evac):
            w1, w2, wr = load_group(c0, width, rw)
            for t1 in range(NT1):
                pt_, pr = next_ps()
                for k in range(8):
                    P.mm(pt_[:, :width], hT[:, k, 1 + t1 * 128:1 + (t1 + 1) * 128], w1[:, k, :width],
                         start=(k == 0), stop=(k == 7 and not rw), r=wr + [R["hT"]], w=[pr])
                if rw:
                    for k in range(8):
                        P.mm(pt_[:, :width], hT[:, k, t1 * 128:(t1 + 1) * 128], w2[:, k, :width],
                             start=False, stop=(k == 7), r=wr + [R["hT"]], w=[pr])
                evac(c0, t1, pt_, pr)

        def ev_lru(col, tt_, pt_, pr):
            e, er = next_ev(False)
            P.cp("act", e[:], pt_[:], r=[pr], w=[er])
            P.dma(scr["lruT"][col:col + 128, off['p0'] + tt_ * 512:off['p0'] + (tt_ + 1) * 512], e[:], r=[er], w=[P.region()])

        def ev_q(col, tt_, pt_, pr):
            e, er = next_ev(True)
            P.ts("dve", e[:], pt_[:], float(128 ** -0.5), None, op0=ALU.mult, r=[pr], w=[er])
            c = col - 2048
            P.dma(scr["qT"][c:c + 128, off['p0'] + tt_ * 512:off['p0'] + (tt_ + 1) * 512], e[:], r=[er], w=[P.region()])

        def ev_k(col, tt_, pt_, pr):
            e, er = next_ev(True)
            P.cp("act", e[:], pt_[:], r=[pr], w=[er])
            c = col - 3072
            P.dma(scr["kT"][c:c + 128, off['p0'] + tt_ * 512:off['p0'] + (tt_ + 1) * 512], e[:], r=[er], w=[P.region()])

        def ev_v(c0, t1, pt_, pr):
            e, er = next_ev(True)
            P.cp("dve", e[:], pt_[:], r=[pr], w=[er])
            c = c0 - 4096
            P.dma(scr["v"][off['p0'] + t1 * 128:off['p0'] + (t1 + 1) * 128, c:c + 512], e[:], r=[er], w=[P.region()])

        def ev_rkv(c0, t1, pt_, pr):
            e, er = next_ev(False)
            P.cp("act" if t1 % 2 else "dve", e[:], pt_[:], r=[pr], w=[er])
            c = c0 - 5120
            P.dma(scr["rkv"][off['p0'] + t1 * 128:off['p0'] + (t1 + 1) * 128, c:c + 512], e[:], r=[er], w=[P.region()])

        def ev_lora(col, tt_, pt_, pr):
            e, er = next_ev(True)
            if col == 8192:
                P.act(e[0:64, :], pt_[0:64, :], AF.Tanh, r=[pr], w=[er])
                P.cp("dve", e[64:128, :], pt_[64:128, :], r=[pr], w=[er])
            else:
                P.act(e[:], pt_[:], AF.Sigmoid, r=[pr], w=[er])
            c = col - 8192
            P.dma(scr["loraT"][c:c + 128, off['p0'] + tt_ * 512:off['p0'] + (tt_ + 1) * 512], e[:], r=[er], w=[P.region()])

        def ev_gate(col, tt_, pt_, pr):
            e, er = next_ev(False)
            P.act(e[:], pt_[:], AF.Sigmoid, r=[pr], w=[er])
            c = col - 8448
            P.dma(scr["gatesT"][c:c + 128, off['p0'] + tt_ * 512:off['p0'] + (tt_ + 1) * 512], e[:], r=[er], w=[P.region()])

        for p0 in range(0, S, TOK):
          off["p0"] = p0
          norm_pass(p0)
          for c0 in range(0, 2048, 512):
            fm_group(c0, 512, False, ev_lru)
          for c0 in range(2048, 3072, 512):
              fm_group(c0, 512, False, ev_q)
          for c0 in range(3072, 4096, 512):
              fm_group(c0, 512, False, ev_k)
          for c0 in range(4096, 5120, 512):
              tm_group(c0, 512, False, ev_v)
          for c0 in range(5120, 8192, 512):
              tm_group(c0, 512, True, ev_rkv)
          fm_group(8192, 256, True, ev_lora)
          for c0 in range(8448, 11520, 512):
              fm_group(c0, 512, False, ev_gate)
    P.end_phase()
GELU_K = 1.5957691216057308


def phase_lru(C, l, cst, Wl, scr):
    P, nc, S = C.P, C.nc, C.S
    import os
    TL = min(int(os.environ.get("LRU_TL", "2048")), S)
    with contextlib.ExitStack() as st:
        C.stack = st
        R = C.regs("lru_")
        lv = C.sb("lv", [128, 8, 8])
        wa = C.sb("wa", [128, 8, 128])
        wx = C.sb("wx", [128, 8, 128])
        c12 = C.sb("c12", [128, 2, 8])
        tiny = C.sb("tiny", [128, 1])
        carry = C.sb("carry", [128, 1])
        xin = [C.sb(f"xin{i}", [128, TL + 3]) for i in range(2)]
        gin = [C.sb(f"gin{i}", [128, TL]) for i in range(2)]
        xc = C.sb("xc", [128, TL])
        rg = C.sb("rg", [128, TL])
        ig = C.sb("ig", [128, TL])
        a_t = C.sb("a_t", [128, TL])
        e2 = C.sb("e2", [128, TL])
        b_t = C.sb("b_t", [128, TL])
        h_t = C.sb("h_t", [128, TL])
        u_t = C.sb("u_t", [128, TL])
        y_t = [C.sb(f"y_t{i}", [128, TL], BF16) for i in range(2)]
        psr = [C.ps(f"psr{i}", [128, 512]) for i in range(2)]
        psi = [C.ps(f"psi{i}", [128, 512]) for i in range(2)]
        P.dma(lv[:], Wl["lruv"][:, :, :], w=[R["lv"]])
        P.dma(wa[:], Wl["lru_wa"][:, :, :], w=[R["wa"]])
        P.dma(wx[:], Wl["lru_wx"][:, :, :], w=[R["wx"]])
        P.memset("pool", tiny[:], 1e-20, w=[R["tiny"]])
        P.act(c12[:, 0, :], lv[:, 7, :], AF.Exp, scale=-1.0, r=[R["lv"]], w=[R["c12"]])
        P.act(c12[:, 0, :], c12[:, 0, :], AF.Ln, bias=1.0, r=[R["c12"]], w=[R["c12"]])
        P.ts("dve", c12[:, 1, :], c12[:, 0, :], -16.0, None, op0=ALU.mult, r=[R["c12"]], w=[R["c12b"]])
        P.ts("dve", c12[:, 0, :], c12[:, 0, :], -8.0, None, op0=ALU.mult, r=[R["c12"], R["c12b"]], w=[R["c12"]])
        it = 0
        for c in range(8):
            rows = slice(c * 128, (c + 1) * 128)
            grows = slice(1024 + c * 128, 1024 + (c + 1) * 128)
            for ti in range(S // TL):
                t0 = ti * TL
                xi, gi = xin[it % 2], gin[it % 2]
                xr, gr = R[f"xin{it % 2}"], R[f"gin{it % 2}"]
                if ti == 0:
                    P.memset("pool", xi[:, 0:3], 0.0, w=[xr])
                    P.dma(xi[:, 3:3 + TL], scr["lruT"][rows, 0:TL], w=[xr])
                else:
                    P.dma(xi[:, 0:3 + TL], scr["lruT"][rows, t0 - 3:t0 + TL], w=[xr])
                P.dma(gi[:], scr["lruT"][grows, t0:t0 + TL], w=[gr])
                cw = lambda k: lv[:, k, c:c + 1]
                P.ts("dve", xc[:], xi[:, 3:3 + TL], cw(0), cw(4), op0=ALU.mult, op1=ALU.add, r=[xr, R["lv"]], w=[R["xc"]])
                for k in (1, 2, 3):
                    P.stt(xc[:], xi[:, 3 - k:3 - k + TL], cw(k), xc[:], ALU.mult, ALU.add, r=[xr, R["xc"]], w=[R["xc"]])
                for j in range(TL // 512):
                    sl = slice(j * 512, (j + 1) * 512)
                    P.mm(psr[j % 2][:], wa[:, c, :], xc[:, sl], r=[R["wa"], R["xc"]], w=[R[f"psr{j % 2}"]])
                    P.mm(psi[j % 2][:], wx[:, c, :], xc[:, sl], r=[R["wx"], R["xc"]], w=[R[f"psi{j % 2}"]])
                    P.act(rg[:, sl], psr[j % 2][:], AF.Sigmoid, bias=lv[:, 5, c:c + 1], r=[R[f"psr{j % 2}"]], w=[R["rg"]])
                    P.act(ig[:, sl], psi[j % 2][:], AF.Sigmoid, bias=lv[:, 6, c:c + 1], r=[R[f"psi{j % 2}"]], w=[R["ig"]])
                P.act(a_t[:], rg[:], AF.Exp, scale=c12[:, 0, c:c + 1], r=[R["rg"], R["c12"]], w=[R["a"]])
                P.act(e2[:], rg[:], AF.Exp, scale=c12[:, 1, c:c + 1], r=[R["rg"], R["c12b"]], w=[R["e2"]])
                P.ts("pool", e2[:], e2[:], -1.0, 1.0, op0=ALU.mult, op1=ALU.add, r=[R["e2"]], w=[R["e2"]])
                P.act(e2[:], e2[:], AF.Ln, bias=tiny[:], r=[R["e2"], R["tiny"]], w=[R["e2"]])
                P.act(e2[:], e2[:], AF.Exp, scale=0.5, r=[R["e2"]], w=[R["e2"]])
                P.tt("dve", b_t[:], e2[:], ig[:], ALU.mult, r=[R["e2"], R["ig"]], w=[R["b"]])
                P.tt("pool", b_t[:], b_t[:], xc[:], ALU.mult, r=[R["b"], R["xc"]], w=[R["b"]])
                init = 0.0 if ti == 0 else carry[:, 0:1]
                P.op("dve", (lambda init=init: (lambda e: e.tensor_tensor_scan(out=h_t[:], data0=a_t[:], data1=b_t[:],
                                                                             initial=init, op0=ALU.mult, op1=ALU.add)))(),
                     r=[R["a"], R["b"], R["carry"]], w=[R["h"]])
                P.cp("pool", carry[:], h_t[:, TL - 1:TL], r=[R["h"]], w=[R["carry"]])
                P.tt("pool", u_t[:], gi[:], gi[:], ALU.mult, r=[gr], w=[R["u"]])
                P.ts("pool", u_t[:], u_t[:], 0.044715, 1.0, op0=ALU.mult, op1=ALU.add, r=[R["u"]], w=[R["u"]])
                P.tt("dve", u_t[:], u_t[:], gi[:], ALU.mult, r=[R["u"], gr], w=[R["u"]])
                P.act(u_t[:], u_t[:], AF.Sigmoid, scale=GELU_K, r=[R["u"]], w=[R["u"]])
                P.tt("pool", u_t[:], u_t[:], gi[:], ALU.mult, r=[R["u"], gr], w=[R["u"]])
                yt, yr = y_t[it % 2], R[f"y{it % 2}"]
                P.tt("dve", yt[:], u_t[:], h_t[:], ALU.mult, r=[R["u"], R["h"]], w=[yr])
                P.dma(scr["yaT"][rows, t0:t0 + TL], yt[:], r=[yr], w=[P.region()])
                it += 1
    P.end_phase()


def phase_sb(C, l, cst, scr):
    P, nc, S = C.P, C.nc, C.S
    NQ = S // 512
    NB = S // 128
    with contextlib.ExitStack() as st:
        C.stack = st
        R = C.regs("sb_")
        qh = [C.sb(f"qh{i}", [128, S], BF16) for i in range(2)]
        kh = [C.sb(f"kh{i}", [128, S], BF16) for i in range(2)]
        vh = [C.sb(f"vh{i}", [128, NB, 128], BF16) for i in range(2)]
        NS = 2
        e_b = [[C.sb(f"e{s}_{i}", [128, 512]) for i in range(2)] for s in range(NS)]
        sp_b = [[C.sb(f"sp{s}_{i}", [128, 512], BF16) for i in range(2)] for s in range(NS)]
        d_b = [[C.sb(f"d{s}_{i}", [128, 512]) for i in range(2)] for s in range(NS)]
        at_b = [[C.sb(f"at{s}_{i}", [128, 512], BF16) for i in range(2)] for s in range(NS)]
        yo = [C.sb(f"yo{s}", [128, 512], BF16) for s in range(NS)]
        pz = [[C.ps(f"pz{s}_{i}", [128, 512]) for i in range(2)] for s in range(NS)]
        pc = [C.ps(f"pc{s}", [128, 512]) for s in range(NS)]
        po = [C.ps(f"po{s}", [128, 512]) for s in range(NS)]
        tri_ge, tri_lt, mask = cst["tri_ge_b"], cst["tri_lt_b"], cst["sbmask_b"]
        vv = scr["v"].rearrange("(n p) d -> p n d", p=128)

        def stream(s, hd, qt, hb):
            q_t, k_t, v_t = qh[hb], kh[hb], vh[hb]
            hr = [R[f"q{hb}"], R[f"k{hb}"], R[f"v{hb}"]]
            kbs = list(range(4 * qt + 3, -1, -1))
            for idx, kb in enumerate(kbs):
                last = idx == len(kbs) - 1
                i2 = idx % 2
                z, zr = pz[s][i2], R[f"pz{s}_{i2}"]
                et, er = e_b[s][i2], R[f"e{s}_{i2}"]
                spt, spr = sp_b[s][i2], R[f"sp{s}_{i2}"]
                dt_, dr = d_b[s][i2], R[f"d{s}_{i2}"]
                att, atr = at_b[s][i2], R[f"at{s}_{i2}"]
                pcr, por = R[f"pc{s}"], R[f"po{s}"]
                P.mm(z[:], k_t[:, kb * 128:(kb + 1) * 128], q_t[:, qt * 512:(qt + 1) * 512], r=hr[0:2], w=[zr])
                P.act(et[:], z[:], AF.Exp, r=[zr], w=[er])
                P.act(spt[:], et[:], AF.Ln, bias=1.0, r=[er], w=[spr])
                mi = kb - 4 * qt
                if mi >= 0:
                    P.tt("dve", spt[:], spt[:], mask[:, mi * 512:(mi + 1) * 512], ALU.mult, r=[spr], w=[spr])
                P.mm(pc[s][:], tri_ge, spt[:], start=(idx == 0), stop=last, r=[spr], w=[pcr], skip_group_check=True)
                P.act(dt_[:], pc[s][:], AF.Exp, scale=-1.0, r=[pcr], w=[dr])
                if not last:
                    P.mm(pc[s][:], tri_lt, spt[:], start=False, stop=False, r=[spr], w=[pcr], skip_group_check=True)
                P.tt("dve", att[:], et[:], dt_[:], ALU.mult, r=[er, dr], w=[atr])
                if mi >= 0:
                    P.tt("dve", att[:], att[:], mask[:, mi * 512:(mi + 1) * 512], ALU.mult, r=[atr], w=[atr])
                P.mm(po[s][:], v_t[:, kb, :], att[:], start=(idx == 0), stop=last, r=[atr, hr[2]], w=[por])
                yield
            P.cp("dve", yo[s][:], po[s][:], r=[R[f"po{s}"]], w=[R[f"yo{s}"]])
            P.dma(scr["ybT"][hd * 128:(hd + 1) * 128, qt * 512:(qt + 1) * 512], yo[s][:], r=[R[f"yo{s}"]], w=[P.region()])

        for hd in range(8):
            hb = hd % 2
            rows = slice(hd * 128, (hd + 1) * 128)
            P.dma(qh[hb][:], scr["qT"][rows, :], w=[R[f"q{hb}"]])
            P.dma(kh[hb][:], scr["kT"][rows, :], w=[R[f"k{hb}"]])
            for n0 in range(0, NB, 16):
                n1 = min(NB, n0 + 16)
                P.dma(vh[hb][:, n0:n1, :], vv[:, n0:n1, rows], w=[R[f"v{hb}"]])
            order = []
            lo, hi = 0, NQ - 1
            while lo <= hi:
                order.append(hi)
                if lo != hi:
                    order.append(lo)
                lo += 1
                hi -= 1
            pending = list(order)
            active = []
            free_slots = list(range(NS))
            while pending or active:
                while pending and free_slots:
                    s = free_slots.pop(0)
                    active.append((s, stream(s, hd, pending.pop(0), hb)))
                nxt = []
                for (s, g) in active:
                    try:
                        next(g)
                        nxt.append((s, g))
                    except StopIteration:
                        free_slots.append(s)
                active = nxt
    P.end_phase()


RW_C0 = 0.6065306597126334


def phase_rwkv(C, l, cst, Wl, scr):
    P, nc, S = C.P, C.nc, C.S
    NCH = S // 128
    with contextlib.ExitStack() as st:
        C.stack = st
        R = C.regs("rw_")
        rwv = C.sb("rwv", [128, 7, 1024])
        wst = C.sb("rw_wst", [128, 1024])
        waup = C.sb("waup", [128, 1024], BF16)
        gup = C.sb("gup", [128, 1024], BF16)
        epsg = C.sb("epsg", [128, 1])
        T1 = C.sb("T1", [128, 3, 1024])
        lt = C.sb("lt", [128, 2, 128], BF16)
        lta = C.sb("lta", [64, 128], BF16)
        aup = C.sb("aup", [64, 1024], BF16)
        sgw = C.sb("sgw", [128, 1024])
        a_t = C.sb("rwa", [128, 1024])
        kk = C.sb("kk", [128, 1024])
        b_t = C.sb("rwb", [128, 1024])
        clsb = C.sb("clsb", [128, 1024])
        tmp = C.sb("rwtmp", [128, 1024])
        E = [C.sb(f"rwE{i}", [128, 1024]) for i in range(2)]
        gC = C.sb("gC", [64, 1024])
        gate = C.sb("rwgate", [128, 1024])
        small = C.sb("rwsmall", [128, 8, 16])
        prod = {n: C.sb("pr_" + n, [128, 1024], BF16) for n in ("kq", "bh", "kh", "rq", "bE", "kE", "v")}
        XT = {n: C.sb("xt_" + n, [64, 16, 128], BF16) for n in ("kq", "bh", "kh", "rq")}
        G = 4
        xs = [C.sb(f"xs{i}", [128, 256]) for i in range(G)]
        akt = [C.sb(f"akt{i}", [128, 128], BF16) for i in range(G)]
        Mn = [[C.sb(f"Mn{i}_{j}", [128, 256]) for j in range(2)] for i in range(G)]
        TTb = [[C.sb(f"TT{i}_{j}", [128, 128]) for j in range(2)] for i in range(G)]
        TTf = [C.sb(f"TTf{i}", [128, 128], BF16) for i in range(G)]
        kcw = [C.sb(f"kcw{i}", [128, 128], BF16) for i in range(G)]
        dg = [C.sb(f"dg{i}", [64, 64]) for i in range(G)]
        BbT = C.sb("BbT", [128, 16, 128], BF16)
        BkT = C.sb("BkT", [128, 16, 128], BF16)
        Uv = C.sb("Uv", [128, 16, 64], BF16)
        RcT = C.sb("RcT", [64, 16, 128], BF16)
        GT = C.sb("GT", [64, 16, 64])
        H_a = C.sb("H_a", [64, 16, 64])
        st_f = C.sb("st_f", [64, 16, 64])
        st_b = C.sb("st_b", [64, 16, 64], BF16)
        ysb = C.sb("ysb", [128, 1024])
        ycs = C.sb("ycs", [128, 8, 128], BF16)
        PW = C.ps("PW", [128, 1024])
        PT = C.ps("PT", [128, 2048], BF16)
        banks = [C.ps(f"rwbk{i}", [128, 512]) for i in range(4)]
        for _k in ("PW", "PT", "bk0", "bk1", "bk2", "bk3"):
            R[_k].excl = True
        bcnt = [0]

        def bank():
            i = bcnt[0] % 4
            bcnt[0] += 1
            return banks[i], R[f"bk{i}"]

        ident_f, ident_b, ones_f, tri_le = cst["ident"], cst["ident_b"], cst["ones"], cst["tri_le"]
        mask3, m_ui = cst["mask3"], cst["m_ui"]
        P.dma(rwv[:], Wl["rwv"][:, :, :], w=[R["rwv"]])
        P.dma(wst[0:64, :], Wl["rw_w_up"][:, :], w=[R["wst"]])
        P.dma(wst[64:128, :], Wl["rw_a_up"][:, :], w=[R["wst"]])
        P.cp("dve", waup[:], wst[:], r=[R["wst"]], w=[R["waup"]])
        P.dma(wst[0:64, :], Wl["rw_a_up"][:, :], w=[R["wst"]])
        P.cp("dve", aup[:], wst[0:64, :], r=[R["wst"]], w=[R["aup"]])
        P.dma(wst[:], Wl["rw_g_up"][:, :], w=[R["wst"]])
        P.cp("dve", gup[:], wst[:], r=[R["wst"]], w=[R["gup"]])
        P.memset("pool", epsg[:], 64e-5, w=[R["epsg"]])
        P.memset("pool", st_f[:], 0.0, w=[R["st_f"]])
        P.memset("pool", st_b[:], 0.0, w=[R["st_b"]])
        ycv = scr["ycT"].rearrange("(k p) s -> p k s", p=128)
        hv = lambda t: t.rearrange("p (h j) -> p h j", j=64)
        rW, rA, rKK, rKA, rRK, rLW, rLB = (rwv[:, i, :] for i in range(7))

        for n in range(NCH):
            t0 = n * 128
            r_, k_, v_ = T1[:, 0, :], T1[:, 1, :], T1[:, 2, :]
            P.dma(T1[:], scr["rkv"][t0:t0 + 128, :].rearrange("p (a c) -> p a c", a=3), w=[R["T1"]])
            P.dma(lt[:, 0, :], scr["loraT"][0:128, t0:t0 + 128], w=[R["lt"]])
            P.dma(lt[:, 1, :], scr["loraT"][128:256, t0:t0 + 128], w=[R["lt"]])
            P.dma(lta[:], scr["loraT"][64:128, t0:t0 + 128], w=[R["lta"]])
            for hf in range(2):
                sl = slice(hf * 512, (hf + 1) * 512)
                P.mm(PW[:, sl], lt[0:64, 0, :], waup[0:64, sl], r=[R["lt"], R["waup"]], w=[R["PW"]])
            P.tt("dve", sgw[:], PW[:], rW, ALU.add, r=[R["PW"], R["rwv"]], w=[R["sgw"]])
            P.act(sgw[:], sgw[:], AF.Sigmoid, r=[R["sgw"]], w=[R["sgw"]])
            for hf in range(2):
                sl = slice(hf * 512, (hf + 1) * 512)
                P.mm(PW[:, sl], lta[:], aup[:, sl], r=[R["lta"], R["aup"]], w=[R["PW"]])
            P.tt("dve", a_t[:], PW[:], rA, ALU.add, r=[R["PW"], R["rwv"]], w=[R["a"]])
            P.act(a_t[:], a_t[:], AF.Sigmoid, r=[R["a"]], w=[R["a"]])
            for hf in range(2):
                sl = slice(hf * 512, (hf + 1) * 512)
                P.mm(PW[:, sl], lt[:, 1, :], gup[:, sl], r=[R["lt"], R["gup"]], w=[R["PW"]])
            P.cp("act", gate[:], PW[:], r=[R["PW"]], w=[R["gate"]])
            P.tt("dve", kk[:], k_, rKK, ALU.mult, r=[R["T1"], R["rwv"]], w=[R["kk"]])
            P.tt("pool", tmp[:], kk[:], kk[:], ALU.mult, r=[R["kk"]], w=[R["tmp"]])
            P.op("dve", lambda e: e.tensor_reduce(out=small[:, 0, :], in_=hv(tmp[:]), axis=AX.X, op=ALU.add),
                 r=[R["tmp"]], w=[R["sm0"]])
            P.ts("dve", small[:, 0, :], small[:, 0, :], 1e-24, None, op0=ALU.max, r=[R["sm0"]], w=[R["sm0"]])
            P.act(small[:, 0, :], small[:, 0, :], AF.Ln, r=[R["sm0"]], w=[R["sm0"]])
            P.act(small[:, 0, :], small[:, 0, :], AF.Exp, scale=-0.5, r=[R["sm0"]], w=[R["sm0"]])
            for h in range(16):
                hs = slice(h * 64, (h + 1) * 64)
                P.ts("dve" if h % 2 else "pool", kk[:, hs], kk[:, hs], small[:, 0, h:h + 1], None, op0=ALU.mult,
                     r=[R["kk"], R["sm0"]], w=[R["kk"]])
            P.stt(tmp[:], a_t[:], -1.0, rKA, ALU.add, ALU.mult, r=[R["a"], R["rwv"], R["tmp"]], w=[R["tmp"]])
            P.stt(k_, tmp[:], 1.0, k_, ALU.add, ALU.mult, r=[R["tmp"], R["T1"], R["kk"]], w=[R["T1"]])
            P.tt("pool", b_t[:], kk[:], a_t[:], ALU.mult, r=[R["kk"], R["a"]], w=[R["b"]])
            P.tt("pool", tmp[:], r_, k_, ALU.mult, r=[R["T1"]], w=[R["tmp"]])
            P.tt("pool", tmp[:], tmp[:], rRK, ALU.mult, r=[R["tmp"], R["rwv"]], w=[R["tmp"]])
            P.op("dve", lambda e: e.tensor_reduce(out=small[:, 1, :], in_=hv(tmp[:]), axis=AX.X, op=ALU.add),
                 r=[R["tmp"]], w=[R["sm1"]])
            for hf in range(2):
                sl = slice(hf * 512, (hf + 1) * 512)
                P.mm(PW[:, sl], tri_le, sgw[:, sl], r=[R["sgw"]], w=[R["PW"]])
            P.cp("act", clsb[:], PW[:], r=[R["PW"]], w=[R["clsb"]])
            for hf in range(2):
                sl = slice(hf * 512, (hf + 1) * 512)
                P.mm(PW[:, sl], ones_f, sgw[:, sl], r=[R["sgw"]], w=[R["PW"]])
            E0, E1 = E[0], E[1]
            P.act(E0[:], clsb[:], AF.Exp, scale=-RW_C0, r=[R["clsb"]], w=[R["E0"]])
            P.tt("dve", prod["rq"][:], r_, E0[:], ALU.mult, r=[R["T1"], R["E0"]], w=[R["p_rq"]])
            P.act(E1[:], clsb[:], AF.Exp, scale=RW_C0, r=[R["clsb"]], w=[R["E1"]])
            P.tt("pool", prod["bh"][:], b_t[:], E1[:], ALU.mult, r=[R["b"], R["E1"]], w=[R["p_bh"]])
            P.tt("dve", prod["kh"][:], k_, E1[:], ALU.mult, r=[R["T1"], R["E1"]], w=[R["p_kh"]])
            P.tt("pool", tmp[:], clsb[:], sgw[:], ALU.subtract, r=[R["clsb"], R["sgw"], R["tmp"]], w=[R["tmp"]])
            P.act(E0[:], tmp[:], AF.Exp, scale=-RW_C0, r=[R["tmp"], R["E0"]], w=[R["E0"]])
            P.tt("dve", prod["kq"][:], kk[:], E0[:], ALU.mult, r=[R["kk"], R["E0"]], w=[R["p_kq"]])
            P.tt("dve", tmp[:], PW[:], clsb[:], ALU.subtract, r=[R["PW"], R["clsb"], R["tmp"]], w=[R["tmp"]])
            P.act(E1[:], tmp[:], AF.Exp, scale=-RW_C0, r=[R["tmp"], R["E1"]], w=[R["E1"]])
            P.tt("pool", prod["bE"][:], b_t[:], E1[:], ALU.mult, r=[R["b"], R["E1"]], w=[R["p_bE"]])
            P.tt("dve", prod["kE"][:], k_, E1[:], ALU.mult, r=[R["T1"], R["E1"]], w=[R["p_kE"]])
            P.act(gC[:], PW[0:64, :], AF.Exp, scale=-RW_C0, r=[R["PW"]], w=[R["gC"]])
            P.cp("pool", prod["v"][:], v_, r=[R["T1"]], w=[R["p_v"]])
            import os as _os
            _stop = _os.environ.get("RW_STOP", "")
            if _stop == "A":
                continue
            PTv = PT[0:64, :].rearrange("p (h t) -> p h t", t=128)
            for qi, qn in enumerate(("kq", "bh", "kh", "rq")):
                for h in range(16):
                    P.tr(PTv[:, h, :], prod[qn][:, h * 64:(h + 1) * 64], ident_b, r=[R["p_" + qn]], w=[R["PT"]])
                P.cp("act" if qi % 2 else "dve", XT[qn][:], PTv, r=[R["PT"]], w=[R["xt_" + qn]])

            if _stop == "T":
                continue
            def head(slot, h):
                hs = slice(h * 64, (h + 1) * 64)
                kqT, bhT, khT, rqT = (XT[q][:, h, :] for q in ("kq", "bh", "kh", "rq"))
                xr = [R["xt_kq"], R["xt_bh"], R["xt_kh"], R["xt_rq"]]
                X, Xr = banks[slot], R[f"bk{slot}"]
                P.mm(X[:, 0:128], kqT, bhT, r=xr, w=[Xr])
                P.mm(X[:, 128:256], bhT, kqT, r=xr, w=[Xr])
                P.mm(X[:, 256:384], khT, kqT, r=xr, w=[Xr])
                P.mm(X[:, 384:512], bhT, rqT, r=xr, w=[Xr])
                yield
                xs_, xsr = xs[slot], R[f"xs{slot}"]
                P.tt("dve", xs_[:], X[:, 0:256], mask3[:, 0:256], ALU.mult, r=[Xr], w=[xsr])
                P.tt("dve", akt[slot][:], X[:, 256:384], mask3[:, 256:384], ALU.mult, r=[Xr], w=[R[f"akt{slot}"]])
                P.tt("dve", BbT[:, h, :], X[:, 384:512], m_ui, ALU.mult, r=[Xr], w=[R[f"BbT{h}"]])
                tcur = 0
                TT_, TTr = TTb[slot][tcur], R[f"TT{slot}_{tcur}"]
                P.tt("pool", TT_[:], ident_f, xs_[:, 128:256], ALU.subtract, r=[xsr], w=[TTr])
                M_, MT_, Mr = xs_[:, 0:128], xs_[:, 128:256], xsr
                yield
                for lev in range(6):
                    L, Lr = X, Xr
                    P.mm(L[:, 0:128], MT_, M_, r=[Mr], w=[Lr])
                    if lev < 5:
                        P.mm(L[:, 128:256], M_, MT_, r=[Mr], w=[Lr])
                    if lev == 0:
                        P.mm(L[:, 384:512], khT, rqT, r=xr, w=[Lr])
                    yield
                    if lev == 0:
                        P.tt("dve", BkT[:, h, :], L[:, 384:512], m_ui, ALU.mult, r=[Lr], w=[R[f"BkT{h}"]])
                    mn, mnr = Mn[slot][lev % 2], R[f"Mn{slot}_{lev % 2}"]
                    wdt = 256 if lev < 5 else 128
                    P.cp("act" if (h + lev) % 2 else "dve", mn[:, 0:wdt], L[:, 0:wdt], r=[Lr], w=[mnr])
                    P.mm(L[:, 256:384], mn[:, 0:128], TT_[:], r=[mnr, TTr], w=[Lr])
                    yield
                    tn = 1 - tcur
                    TTn, TTnr = TTb[slot][tn], R[f"TT{slot}_{tn}"]
                    P.tt("dve", TTn[:], L[:, 256:384], TT_[:], ALU.add, r=[Lr, TTr], w=[TTnr])
                    TT_, TTr, tcur = TTn, TTnr, tn
                    M_, MT_, Mr = mn[:, 0:128], mn[:, 128:256], mnr
                Z2, Z2r = X, Xr
                P.cp("pool", TTf[slot][:], TT_[:], r=[TTr], w=[R[f"TTf{slot}"]])
                TT_, TTr = TTf[slot], R[f"TTf{slot}"]
                P.mm(Z2[:, 0:64], TT_[:], prod["kq"][:, hs], r=[TTr, R["p_kq"]], w=[Z2r])
                P.mm(Z2[:, 64:128], akt[slot][:], prod["v"][:, hs], r=[R[f"akt{slot}"], R["p_v"]], w=[Z2r])
                yield
                kc_, kcr = kcw[slot], R[f"kcw{slot}"]
                P.cp("act", kc_[:], Z2[:, 0:128], r=[Z2r], w=[kcr])
                P.mm(Z2[:, 128:192], TT_[:], kc_[:, 64:128], r=[TTr, kcr], w=[Z2r])
                P.mm(Z2[0:64, 192:256], kc_[:, 0:64], prod["bE"][:, hs], r=[kcr, R["p_bE"]], w=[Z2r])
                P.mm(Z2[0:64, 256:384], kc_[:, 0:64], BbT[:, h, :], r=[kcr, R[f"BbT{h}"]], w=[Z2r])
                yield
                P.ts("dve", Uv[:, h, :], Z2[:, 128:192], -1.0, None, op0=ALU.mult, r=[Z2r], w=[R[f"Uv{h}"]])
                P.tt("pool", dg[slot][:], ident_f[0:64, 0:64], gC[:, hs], ALU.mult, r=[R["gC"]], w=[R[f"dg{slot}"]])
                P.tt("dve", GT[:, h, :], dg[slot][:], Z2[0:64, 192:256], ALU.subtract, r=[R[f"dg{slot}"], Z2r], w=[R[f"GT{h}"]])
                P.tt("dve", RcT[:, h, :], rqT, Z2[0:64, 256:384], ALU.subtract, r=[R["xt_rq"], Z2r], w=[R[f"RcT{h}"]])
                P.mm(Z2[0:64, 384:448], prod["bE"][:, hs], Uv[:, h, :], start=True, stop=False,
                     r=[R["p_bE"], R[f"Uv{h}"]], w=[Z2r])
                P.mm(Z2[0:64, 384:448], prod["kE"][:, hs], prod["v"][:, hs], start=False, stop=True,
                     r=[R["p_kE"], R["p_v"]], w=[Z2r])
                yield
                P.cp("act", H_a[:, h, :], Z2[0:64, 384:448], r=[Z2r], w=[R[f"H{h}"]])

            pending = list(range(16))
            active = []
            _hc = {}
            free_slots = list(range(G))
            while pending or active:
                while pending and free_slots:
                    s_ = free_slots.pop(0)
                    active.append((s_, head(s_, pending.pop(0))))
                nxt = []
                for (s_, g_) in active:
                    try:
                        next(g_)
                        _hc[s_] = _hc.get(s_, 0) + 1
                        if _hc[s_] >= int(_os.environ.get("RW_HSTOP", "999")):
                            _hc[s_] = 0
                            g_.close()
                            raise StopIteration
                        nxt.append((s_, g_))
                    except StopIteration:
                        _hc[s_] = 0
                        free_slots.append(s_)
                active = nxt
            if _stop == "B":
                continue
            for h in range(16):
                hs = slice(h * 64, (h + 1) * 64)
                P.mm(PW[:, hs], RcT[:, h, :], st_b[:, h, :], start=True, stop=False,
                     r=[R[f"RcT{h}"], R["st_b"]], w=[R["PW"]])
                P.mm(PW[:, hs], BbT[:, h, :], Uv[:, h, :], start=False, stop=False,
                     r=[R[f"BbT{h}"], R[f"Uv{h}"]], w=[R["PW"]])
                P.mm(PW[:, hs], BkT[:, h, :], prod["v"][:, hs], start=False, stop=True,
                     r=[R[f"BkT{h}"], R["p_v"]], w=[R["PW"]])
            P.cp("act", ysb[:], PW[:], r=[R["PW"]], w=[R["ysb"]])
            for h in range(16):
                hs = slice(h * 64, (h + 1) * 64)
                P.mm(PW[0:64, hs], GT[:, h, :], st_f[:, h, :], r=[R[f"GT{h}"], R["st_f"]], w=[R["PW"]])
            P.tt("dve", st_f[:].rearrange("p h j -> p (h j)"), PW[0:64, :], H_a[:].rearrange("p h j -> p (h j)"), ALU.add,
                 r=[R["PW"]] + [R[f"H{h}"] for h in range(16)], w=[R["st_f"]])
            P.cp("act", st_b[:], st_f[:], r=[R["st_f"]], w=[R["st_b"]])
            P.op("dve", lambda e: e.tensor_reduce(out=small[:, 2, :], in_=hv(ysb[:]), axis=AX.X, op=ALU.add),
                 r=[R["ysb"]], w=[R["sm2"]])
            P.tt("pool", tmp[:], ysb[:], ysb[:], ALU.mult, r=[R["ysb"], R["tmp"]], w=[R["tmp"]])
            P.op("dve", lambda e: e.tensor_reduce(out=small[:, 3, :], in_=hv(tmp[:]), axis=AX.X, op=ALU.add),
                 r=[R["tmp"]], w=[R["sm3"]])
            P.ts("dve", small[:, 4, :], small[:, 2, :], 1.0 / 64, None, op0=ALU.mult, r=[R["sm2"]], w=[R["sm4"]])
            P.tt("dve", small[:, 5, :], small[:, 4, :], small[:, 4, :], ALU.mult, r=[R["sm4"]], w=[R["sm5"]])
            P.stt(small[:, 6, :], small[:, 3, :], 1.0 / 64, small[:, 5, :], ALU.mult, ALU.subtract,
                  r=[R["sm3"], R["sm5"]], w=[R["sm6"]])
            P.act(small[:, 6, :], small[:, 6, :], AF.Ln, bias=epsg[:], r=[R["sm6"], R["epsg"]], w=[R["sm6"]])
            P.act(small[:, 6, :], small[:, 6, :], AF.Exp, scale=-0.5, r=[R["sm6"]], w=[R["sm6"]])
            for h in range(16):
                hs = slice(h * 64, (h + 1) * 64)
                P.ts("dve" if h % 2 else "pool", ysb[:, hs], ysb[:, hs], small[:, 4, h:h + 1], small[:, 6, h:h + 1],
                     op0=ALU.subtract, op1=ALU.mult, r=[R["ysb"], R["sm4"], R["sm6"]], w=[R["ysb"]])
            P.tt("pool", ysb[:], ysb[:], rLW, ALU.mult, r=[R["ysb"], R["rwv"]], w=[R["ysb"]])
            P.tt("dve", ysb[:], ysb[:], rLB, ALU.add, r=[R["ysb"], R["rwv"]], w=[R["ysb"]])
            for h in range(16):
                hs = slice(h * 64, (h + 1) * 64)
                P.stt(ysb[:, hs], v_[:, hs], small[:, 1, h:h + 1], ysb[:, hs], ALU.mult, ALU.add,
                      r=[R["T1"], R["sm1"], R["ysb"]], w=[R["ysb"]])
            P.tt("pool", ysb[:], ysb[:], gate[:], ALU.mult, r=[R["ysb"], R["gate"]], w=[R["ysb"]])
            for k in range(8):
                P.tr(PW[:, k * 128:(k + 1) * 128], ysb[:, k * 128:(k + 1) * 128], ident_f, r=[R["ysb"]], w=[R["PW"]])
            P.cp("act", ycs[:].rearrange("p k t -> p (k t)"), PW[:], r=[R["PW"]], w=[R["ycs"]])
            P.dma(ycv[:, :, t0:t0 + 128], ycs[:], r=[R["ycs"]], w=[P.region()])
    P.end_phase()
def phase_merge(C, l, cst, Wl, x_in, per, scr):
    P, nc, S = C.P, C.nc, C.S
    TT = 256
    with contextlib.ExitStack() as st:
        C.stack = st
        R = C.regs("mg_")
        wts = {n: C.sb("mg_" + n, [128, 8, 1024], BF16) for n in ("p_lru", "p_sb", "p_rwkv", "w_out")}
        wr = C.sb("mg_wr", [128, 8, 32])
        brt = C.sb("mg_br", [128, 32])
        pt = per["t"]
        with contextlib.ExitStack() as st2:
            C.stack = st2
            wst = C.sb("mg_wst", [128, 8, 1024])
            for i, n in enumerate(("p_lru", "p_sb", "p_rwkv", "w_out")):
                P.dma(wst[:], Wl[n].rearrange("(k p) j -> p k j", p=128), w=[R["wst"]])
                P.cp("pool" if i % 2 else "dve", wts[n][:], wst[:], r=[R["wst"]], w=[R["w_" + n]])
            P.end_phase()
        C.stack = st
        P.dma(wr[:], Wl["w_router"].rearrange("(k p) j -> p k j", p=128), w=[R["wr"]])
        P.dma(brt[:], Wl["b_router"][:, :], w=[R["br"]])
        yt = {n: C.sb("mg_y" + n, [128, 8, TT], BF16) for n in ("a", "b", "c")}
        gt = [C.sb(f"mg_g{i}", [128, 3, TT]) for i in range(2)]
        mrg = C.sb("mg_mrg", [128, 8, TT], BF16)
        m1 = C.sb("mg_m1", [128, TT])
        m2 = C.sb("mg_m2", [128, TT])
        xt = C.sb("mg_xt", [128, 8, TT])
        x1 = C.sb("mg_x1", [128, 8, TT])
        h2f = C.sb("mg_h2f", [128, 8, TT])
        h2b = C.sb("mg_h2b", [128, 8, TT], BF16)
        sq = C.sb("mg_sq", [128, 8, TT])
        tmp = C.sb("mg_tmp", [128, 8, TT])
        rs = C.sb("mg_rs", [128, TT])
        lg = C.sb("mg_lg", [128, 32])
        m8 = C.sb("mg_m8", [128, 8])
        nmx = C.sb("mg_nmx", [128, 1])
        msk = C.sb("mg_msk", [128, 32])
        ex = C.sb("mg_ex", [128, 32])
        ssum = C.sb("mg_ssum", [128, 1])
        rwo = [C.sb(f"mg_rwo{i}", [128, 32]) for i in range(2)]
        psb = [C.ps(f"mg_ps{i}", [128, 512]) for i in range(3)]
        psm = C.ps("mg_psm", [128, 512])
        ps_s = C.ps("mg_pss", [128, 512])
        psl = C.ps("mg_psl", [128, 512])
        for k_ in ("ps0", "ps1", "ps2", "psm", "ps_s", "psl"):
            R[k_].excl = True
        xv = x_in.rearrange("(k p) s -> p k s", p=128)
        yv = {n: scr["y%sT" % n].rearrange("(k p) s -> p k s", p=128) for n in ("a", "b", "c")}
        gv = scr["gatesT"].rearrange("(g k p) s -> p g k s", p=128, k=8)
        x1v = scr["x1T"].rearrange("(k p) s -> p k s", p=128)
        h2v = scr["h2T"].rearrange("(k p) s -> p k s", p=128)
        pw = (("a", "p_lru"), ("b", "p_sb"), ("c", "p_rwkv"))
        gi = 0
        for ti in range(S // TT):
            ts_ = slice(ti * TT, (ti + 1) * TT)
            for n in ("a", "b", "c"):
                P.dma(yt[n][:], yv[n][:, :, ts_], w=[R["y" + n]])
            P.dma(xt[:], xv[:, :, ts_], w=[R["xt"]])
            for oc in range(8):
                g_ = gt[gi % 2]
                gr = R[f"g{gi % 2}"]
                gi += 1
                P.dma(g_[:], gv[:, :, oc, ts_], w=[gr])
                for bi, (yn, wn) in enumerate(pw):
                    for k in range(8):
                        P.mm(psb[bi][:, :TT], wts[wn][:, k, oc * 128:(oc + 1) * 128], yt[yn][:, k, :],
                             start=(k == 0), stop=(k == 7), r=[R["w_" + wn], R["y" + yn]], w=[R[f"ps{bi}"]])
                P.tt("dve", m1[:], psb[0][:, :TT], g_[:, 0, :], ALU.mult, r=[R["ps0"], gr], w=[R["m1"]])
                P.tt("dve", m2[:], psb[1][:, :TT], g_[:, 1, :], ALU.mult, r=[R["ps1"], gr], w=[R["m2"]])
                P.tt("pool", m1[:], m1[:], m2[:], ALU.add, r=[R["m1"], R["m2"]], w=[R["m1"]])
                P.tt("dve", m2[:], psb[2][:, :TT], g_[:, 2, :], ALU.mult, r=[R["ps2"], gr, R["m2"]], w=[R["m2"]])
                P.tt("pool", mrg[:, oc, :], m1[:], m2[:], ALU.add, r=[R["m1"], R["m2"]], w=[R["mrg"]])
            for oc in range(8):
                for k in range(8):
                    P.mm(psm[:, :TT], wts["w_out"][:, k, oc * 128:(oc + 1) * 128], mrg[:, k, :],
                         start=(k == 0), stop=(k == 7), r=[R["w_w_out"], R["mrg"]], w=[R["psm"]])
                P.stt(x1[:, oc, :], psm[:, :TT], pt[:, 2, oc:oc + 1], xt[:, oc, :], ALU.mult, ALU.add,
                      r=[R["psm"], R["xt"]], w=[R["xt1"]])
            P.dma(x1v[:, :, ts_], x1[:], r=[R["xt1"]], w=[P.region()])
            emit_norm_tile(C, R, x1[:], lambda k: h2f[:, k, :], pt[:, 3, :], pt[:, 4, :], cst["ones"], sq, rs, tmp,
                           ps_s, TT, cst["eps"], "1")
            P.cp("pool", h2b[:], h2f[:], r=[R["hT"]], w=[R["h2b"]])
            P.dma(h2v[:, :, ts_], h2b[:], r=[R["h2b"]], w=[P.region()])
            for sub in range(TT // 128):
                for k in range(8):
                    P.mm(psl[:, 0:32], h2f[:, k, sub * 128:(sub + 1) * 128], wr[:, k, :], start=(k == 0), stop=(k == 7),
                         r=[R["hT"], R["wr"]], w=[R["psl"]])
                P.tt("dve", lg[:], psl[:, 0:32], brt[:], ALU.add, r=[R["psl"], R["br"]], w=[R["lg"]])
                P.op("dve", lambda e: e.max(out=m8[:], in_=lg[:]), r=[R["lg"]], w=[R["m8"]])
                P.ts("dve", nmx[:], m8[:, 0:1], -1.0, None, op0=ALU.mult, r=[R["m8"]], w=[R["nmx"]])
                P.ts("dve", msk[:], lg[:], m8[:, 3:4], None, op0=ALU.is_ge, r=[R["lg"], R["m8"]], w=[R["msk"]])
                P.act(ex[:], lg[:], AF.Exp, bias=nmx[:], r=[R["lg"], R["nmx"]], w=[R["ex"]])
                P.tt("dve", ex[:], ex[:], msk[:], ALU.mult, r=[R["ex"], R["msk"]], w=[R["ex"]])
                P.op("dve", lambda e: e.tensor_reduce(out=ssum[:], in_=ex[:], axis=AX.X, op=ALU.add),
                     r=[R["ex"]], w=[R["ssum"]])
                P.op("dve", lambda e: e.reciprocal(out=ssum[:], in_=ssum[:]), r=[R["ssum"]], w=[R["ssum"]])
                ro = rwo[sub % 2]
                P.ts("dve", ro[:], ex[:], ssum[:, 0:1], None, op0=ALU.mult, r=[R["ex"], R["ssum"]], w=[R[f"rwo{sub % 2}"]])
                t0 = ti * TT + sub * 128
                P.dma(scr["rwt"][t0:t0 + 128, :], ro[:], r=[R[f"rwo{sub % 2}"]], w=[P.region()])
    P.end_phase()


def phase_moe(C, l, cst, Wl, per, scr, nfin_d, out_d):
    P, nc, S = C.P, C.nc, C.S
    import os
    NE = int(os.environ.get("MOE_NE", "32"))
    ST = min(512, S)
    NTB = ST // 128
    with contextlib.ExitStack() as st:
        C.stack = st
        R = C.regs("moe_")
        pt = per["t"]
        gub = scr["gub"]
        dnb = scr["dnb"]
        for e in range(NE):
            P.dma(gub[e], Wl["w_gu"][e], w=[P.region()], q="pool")
            P.dma(dnb[e], Wl["w_down"][e], w=[P.region()], q="pool")
        P.end_phase()
        C.stack = st
        h2 = C.sb("moe_h2", [128, 8, ST], BF16)
        yacc = C.sb("moe_yacc", [128, NTB, 1024])
        wgu = [C.sb(f"moe_wgu{i}", [128, 8, 2048], BF16) for i in range(2)]
        wdn = [C.sb(f"moe_wdn{i}", [128, 8, 1024], BF16) for i in range(2)]
        actT = [C.sb(f"moe_act{i}", [128, 8, 512], BF16) for i in range(2)]
        bgu = C.sb("moe_bgu", [128, 32, 16])
        bdn = C.sb("moe_bdn", [32, 1024])
        rwt = C.sb("moe_rwt", [128, NTB, 32])
        rwT = C.sb("moe_rwT", [32, 128])
        g_t = [C.sb(f"moe_g{i}", [128, 512]) for i in range(2)]
        u_t = [C.sb(f"moe_u{i}", [128, 512]) for i in range(1)] * 2
        s_t = [C.sb(f"moe_s{i}", [128, 512]) for i in range(1)] * 2
        x1t = C.sb("moe_x1", [128, 8, 128])
        x2t = C.sb("moe_x2", [128, 8, 128])
        nfin = C.sb("moe_nfin", [128, 8])
        sq = C.sb("moe_sq", [128, 8, 128])
        rs = C.sb("moe_rs", [128, 128])
        o_t = C.sb("moe_ot", [128, 8, 128])
        psg = [C.ps(f"moe_psg{i}", [128, 512]) for i in range(2)]
        psu = [C.ps(f"moe_psu{i}", [128, 512]) for i in range(2)]
        psd = [C.ps(f"moe_psd{i}", [128, 512]) for i in range(2)]
        psx = C.ps("moe_psx", [128, 1024])
        for k_ in ("psg0", "psg1", "psu0", "psu1", "psd0", "psd1", "psx"):
            R[k_].excl = True
        P.dma(bgu[:], Wl["b_gu"][:, :, :], w=[R["bgu"]])
        P.dma(bdn[:], Wl["b_down"][:, :], w=[R["bdn"]])
        if nfin_d is not None:
            P.dma(nfin[:], nfin_d[:, :], w=[R["nfin"]])
        h2v = scr["h2T"].rearrange("(k p) s -> p k s", p=128)
        x1v = scr["x1T"].rearrange("(k p) s -> p k s", p=128)
        ov = out_d.rearrange("(k p) s -> p k s", p=128)
        rwv_ = scr["rwt"].rearrange("(n p) e -> p n e", p=128)
        wi = 0
        ci = 0
        for si in range(S // ST):
            s0 = si * ST
            P.dma(h2[:], h2v[:, :, s0:s0 + ST], w=[R["h2"]])
            P.dma(rwt[:], rwv_[:, si * NTB:(si + 1) * NTB, :], w=[R["rwt"]])
            for tb in range(NTB):
                P.tr(psx[0:32, 0:128], rwt[:, tb, :], cst["ident"], r=[R["rwt"]], w=[R["psx"]])
                P.cp("dve", rwT[:], psx[0:32, 0:128], r=[R["psx"]], w=[R["rwT"]])
                for dh in range(2):
                    P.mm(psd[dh][:], rwT[:], bdn[:, dh * 512:(dh + 1) * 512], r=[R["rwT"], R["bdn"]], w=[R[f"psd{dh}"]])
                    P.cp("act", yacc[:, tb, dh * 512:(dh + 1) * 512], psd[dh][:], r=[R[f"psd{dh}"]], w=[R[f"yacc{tb}"]])
            for e in range(NE):
                wg, wd = wgu[wi % 2], wdn[wi % 2]
                wgr, wdr = R[f"wgu{wi % 2}"], R[f"wdn{wi % 2}"]
                wi += 1
                P.dma(wg[:], gub[e].rearrange("(k p) j -> p k j", p=128), w=[wgr])
                P.dma(wd[:], dnb[e].rearrange("(k p) j -> p k j", p=128), w=[wdr])
                for tt_ in range(ST // 512):
                    tsl = slice(tt_ * 512, (tt_ + 1) * 512)
                    at, atr = actT[ci % 2], R[f"act{ci % 2}"]
                    ci += 1
                    for fc in range(8):
                        i2 = fc % 2
                        for k in range(8):
                            P.mm(psg[i2][:], wg[:, k, fc * 128:(fc + 1) * 128], h2[:, k, tsl], start=(k == 0), stop=(k == 7),
                                 r=[wgr, R["h2"]], w=[R[f"psg{i2}"]])
                        for k in range(8):
                            P.mm(psu[i2][:], wg[:, k, 1024 + fc * 128:1024 + (fc + 1) * 128], h2[:, k, tsl],
                                 start=(k == 0), stop=(k == 7), r=[wgr, R["h2"]], w=[R[f"psu{i2}"]])
                        P.ts("dve", g_t[i2][:], psg[i2][:], bgu[:, e, fc:fc + 1], 7.0, op0=ALU.add, op1=ALU.min,
                             r=[R[f"psg{i2}"], R["bgu"]], w=[R[f"g{i2}"]])
                        P.ts("dve", u_t[i2][:], psu[i2][:], bgu[:, e, 8 + fc:9 + fc], 7.0, op0=ALU.add, op1=ALU.min,
                             r=[R[f"psu{i2}"], R["bgu"]], w=[R["u0"]])
                        P.ts("pool", u_t[i2][:], u_t[i2][:], -7.0, 1.0, op0=ALU.max, op1=ALU.add, r=[R["u0"]], w=[R["u0"]])
                        P.act(s_t[i2][:], g_t[i2][:], AF.Sigmoid, scale=1.702, r=[R[f"g{i2}"]], w=[R["s0"]])
                        P.tt("pool", s_t[i2][:], s_t[i2][:], g_t[i2][:], ALU.mult, r=[R["s0"], R[f"g{i2}"]], w=[R["s0"]])
                        P.tt("dve", at[:, fc, :], s_t[i2][:], u_t[i2][:], ALU.mult, r=[R["s0"], R["u0"]], w=[atr])
                    for sub in range(4):
                        tb = tt_ * 4 + sub
                        for dh in range(2):
                            for fc in range(8):
                                P.mm(psd[dh][:], at[:, fc, sub * 128:(sub + 1) * 128], wd[:, fc, dh * 512:(dh + 1) * 512],
                                     start=(fc == 0), stop=(fc == 7), r=[atr, wdr], w=[R[f"psd{dh}"]])
                            P.stt(yacc[:, tb, dh * 512:(dh + 1) * 512], psd[dh][:], rwt[:, tb, e:e + 1],
                                  yacc[:, tb, dh * 512:(dh + 1) * 512], ALU.mult, ALU.add,
                                  r=[R[f"psd{dh}"], R["rwt"], R[f"yacc{tb}"]], w=[R[f"yacc{tb}"]])
            for tb in range(NTB):
                t0 = s0 + tb * 128
                P.dma(x1t[:], x1v[:, :, t0:t0 + 128], w=[R["x1t"]])
                for k in range(8):
                    P.tr(psx[:, k * 128:(k + 1) * 128], yacc[:, tb, k * 128:(k + 1) * 128], cst["ident"],
                         r=[R[f"yacc{tb}"]], w=[R["psx"]])
                for k in range(8):
                    P.stt(x2t[:, k, :], psx[:, k * 128:(k + 1) * 128], pt[:, 5, k:k + 1], x1t[:, k, :], ALU.mult, ALU.add,
                          r=[R["psx"], R["x1t"]], w=[R["x2t"]])
                if nfin_d is None:
                    P.dma(ov[:, :, t0:t0 + 128], x2t[:], r=[R["x2t"]], w=[P.region()])
                else:
                    P.act(sq[:], x2t[:], AF.Square, r=[R["x2t"]], w=[R["sq"]])
                    for k in range(8):
                        P.mm(psd[0][:, :128], cst["ones"], sq[:, k, :], start=(k == 0), stop=(k == 7), r=[R["sq"]], w=[R["psd0"]])
                    P.act(rs[:], psd[0][:, :128], AF.Ln, scale=1.0 / 1024, bias=cst["eps"], r=[R["psd0"]], w=[R["rs"]])
                    P.act(rs[:], rs[:], AF.Exp, scale=-0.5, r=[R["rs"]], w=[R["rs"]])
                    for k in range(8):
                        P.stt(o_t[:, k, :], x2t[:, k, :], nfin[:, k:k + 1], rs[:], ALU.mult, ALU.mult,
                              r=[R["x2t"], R["rs"], R["nfin"]], w=[R["o_t"]])
                    P.dma(ov[:, :, t0:t0 + 128], o_t[:], r=[R["o_t"]], w=[P.region()], is_out=True)
    P.end_phase()
from concourse.bass_utils import run_bass_kernel_spmd

LAYER_W = ["w_ada", "b_ada", "norm_mix", "norm_moe", "w_in", "conv_w", "conv_b", "lru_wa", "lru_ba", "lru_wx",
           "lru_bx", "lru_lambda", "rw_mu", "rw_w0", "rw_w_up", "rw_a0", "rw_a_up", "rw_g_up", "rw_k_k", "rw_k_a",
           "rw_r_k", "rw_lnx_w", "rw_lnx_b", "p_lru", "p_sb", "p_rwkv", "w_out", "w_router", "b_router",
           "w_gu", "b_gu", "w_down", "b_down"]


def fm(v):
    v = np.asarray(v, np.float32).reshape(-1, 128)
    return np.ascontiguousarray(v.T)


def bc(v):
    v = np.asarray(v, np.float32).reshape(1, -1)
    return np.ascontiguousarray(np.broadcast_to(v, (128, v.shape[1])))


def layer_inputs(inp, l):
    g = lambda n: np.asarray(inp[n][l], np.float32)
    d = {}
    d["w_ada"] = g("w_ada")
    d["b_ada"] = fm(g("b_ada"))
    d["norm_mix"] = fm(g("norm_mix"))
    d["norm_moe"] = fm(g("norm_moe"))
    d["w_in"] = g("w_in")
    d["mu"] = bc(g("rw_mu"))
    cw = g("conv_w")
    vecs = [cw[0], cw[1], cw[2], cw[3], g("conv_b"), g("lru_ba"), g("lru_bx"), g("lru_lambda")]
    d["lruv"] = np.ascontiguousarray(np.stack([fm(v) for v in vecs], axis=1))
    for nm in ("lru_wa", "lru_wx"):
        w = g(nm)
        bd = np.zeros((128, 8, 128), np.float32)
        for c in range(8):
            bd[0:64, c, 0:64] = w[2 * c]
            bd[64:128, c, 64:128] = w[2 * c + 1]
        d[nm] = bd
    d["rwv"] = np.ascontiguousarray(np.stack(
        [bc(g(n).reshape(-1)) for n in ("rw_w0", "rw_a0", "rw_k_k", "rw_k_a", "rw_r_k", "rw_lnx_w", "rw_lnx_b")], axis=1))
    d["rw_w_up"] = g("rw_w_up")
    d["rw_a_up"] = g("rw_a_up")
    d["rw_g_up"] = g("rw_g_up")
    for nm in ("p_lru", "p_sb", "p_rwkv", "w_out", "w_router", "w_gu", "w_down", "b_down"):
        d[nm] = g(nm)
    d["b_router"] = bc(g("b_router"))
    bg = g("b_gu")
    d["b_gu"] = np.ascontiguousarray(bg.reshape(32, 16, 128).transpose(2, 0, 1))
    return d


LAYER_SHAPES = {
    "w_ada": (1024, 6144), "b_ada": (128, 48), "norm_mix": (128, 8), "norm_moe": (128, 8), "w_in": (1024, 11520),
    "mu": (128, 3328), "lruv": (128, 8, 8), "lru_wa": (128, 8, 128), "lru_wx": (128, 8, 128),
    "rwv": (128, 7, 1024), "rw_w_up": (64, 1024), "rw_a_up": (64, 1024), "rw_g_up": (128, 1024),
    "p_lru": (1024, 1024), "p_sb": (1024, 1024), "p_rwkv": (1024, 1024), "w_out": (1024, 1024),
    "w_router": (1024, 32), "w_gu": (32, 1024, 2048), "w_down": (32, 1024, 1024), "b_down": (32, 1024),
    "b_router": (128, 32), "b_gu": (128, 32, 16),
}


def build(S, depth, debug=False, phases=None):
    nc = bass.Bass("TRN2", target_bir_lowering=False)
    P = Prog(nc)
    C = Ctx(nc, P, S, debug)
    cst_np, ccols = make_consts()
    NCC = cst_np.shape[1]
    xT_d = C.din("xT", [1024, S])
    cvec_d = C.din("cvec", [128, 8])
    nfin_d = C.din("norm_final", [128, 8])
    cst_d = C.din("consts", [128, NCC])
    class LazyW(dict):
        def __init__(self, l):
            super().__init__()
            self.l = l

        def __missing__(self, n):
            ap = C.din(f"{n}_{self.l}", LAYER_SHAPES[n])
            self[n] = ap
            C.declared.add(f"{n}_{self.l}")
            return ap
    C.declared = set()
    W = [LazyW(l) for l in range(depth)]
    out_d = C.dout("outT", [1024, S])
    scr = {
        "lruT": C.dscr("s_lruT", [2048, S], F32),
        "qT": C.dscr("s_qT", [1024, S], BF16),
        "kT": C.dscr("s_kT", [1024, S], BF16),
        "v": C.dscr("s_v", [S, 1024], BF16),
        "rkv": C.dscr("s_rkv", [S, 3072], F32),
        "loraT": C.dscr("s_loraT", [256, S], BF16),
        "gatesT": C.dscr("s_gatesT", [3072, S], F32),
        "yaT": C.dscr("s_yaT", [1024, S], BF16),
        "ybT": C.dscr("s_ybT", [1024, S], BF16),
        "ycT": C.dscr("s_ycT", [1024, S], BF16),
        "x1T": C.dscr("s_x1T", [1024, S], F32),
        "x2T": C.dscr("s_x2T", [1024, S], F32),
        "h2T": C.dscr("s_h2T", [1024, S], BF16),
        "rwt": C.dscr("s_rwt", [S, 32], F32),
        "gub": nc.dram_tensor("s_gub", [32, 1024, 2048], BF16, kind="Internal").ap(),
        "dnb": nc.dram_tensor("s_dnb", [32, 1024, 1024], BF16, kind="Internal").ap(),
    }
    with contextlib.ExitStack() as top:
        C.stack = top
        cst_t = top.enter_context(nc.sbuf_tensor("cst", [128, NCC], F32))
        cstb_t = top.enter_context(nc.sbuf_tensor("cstb", [128, NCC], BF16))
        eps_t = top.enter_context(nc.sbuf_tensor("eps", [128, 1], F32))
        per_t = top.enter_context(nc.sbuf_tensor("per", [128, 6, 8], F32))
        R0 = C.regs("init_")
        P.dma(cst_t[:], cst_d[:, :], w=[R0["cst"]])
        P.cp("dve", cstb_t[:], cst_t[:], r=[R0["cst"]], w=[R0["cstb"]])
        P.memset("pool", eps_t[:], EPS, w=[R0["eps"]])
        P.end_phase()
        cst = {"eps": eps_t[:]}
        for n, (o, wd) in ccols.items():
            cst[n] = cst_t[:, o:o + wd]
            cst[n + "_b"] = cstb_t[:, o:o + wd]
        per = {"t": per_t, "reg": P.region("per", persistent=True)}
        x_in = xT_d
        for l in range(depth):
            Wl = W[l]
            if phases is None or "ada" in phases:
                phase_adaln(C, l, cst, cvec_d, Wl["w_ada"], Wl["b_ada"], Wl["norm_mix"], Wl["norm_moe"], per)
            if phases is None or "proj" in phases:
                phase_proj(C, l, cst, x_in, Wl["w_in"], Wl["mu"], per, scr)
            if phases is None or "lru" in phases:
                phase_lru(C, l, cst, Wl, scr)
            if phases is None or "sb" in phases:
                phase_sb(C, l, cst, scr)
            if phases is None or "rwkv" in phases:
                phase_rwkv(C, l, cst, Wl, scr)
            if phases is None or "merge" in phases:
                phase_merge(C, l, cst, Wl, x_in, per, scr)
            if phases is None or "moe" in phases:
                last = (l == depth - 1)
                phase_moe(C, l, cst, Wl, per, scr, nfin_d if last else None, out_d if last else scr["x2T"])
            x_in = scr["x2T"]
        if phases is not None and "moe" not in phases:
            with contextlib.ExitStack() as st:
                C.stack = st
                z = C.sb("zz", [128, 8])
                rz = P.region()
                P.memset("pool", z[:], 0.0, w=[rz])
                P.dma(out_d[0:128, 0:8], z[:], r=[rz], w=[P.region()], is_out=True)
        P.emit()
    nc.declared_inputs = set(C.declared)
    return nc, cst_np


def core_inputs(inp, b, S, depth, cst_np, layer_cache, declared=None):
    m = {"xT": np.ascontiguousarray(np.asarray(inp["x"][b, :S], np.float32).T),
         "cvec": fm(np.asarray(inp["c"][b], np.float32)),
         "norm_final": fm(np.asarray(inp["norm_final"], np.float32)),
         "consts": cst_np}
    for l in range(depth):
        for n, a in layer_cache[l].items():
            if declared is None or f"{n}_{l}" in declared:
                m[f"{n}_{l}"] = a
    return m


N_ACTIVE = 4


def kernel(**inputs):
    S, depth, B = 8192, 2, 4
    nc, cst_np = build(S, depth)
    lc = [layer_inputs(inputs, l) for l in range(depth)]
    in_maps = [core_inputs(inputs, b, S, depth, cst_np, lc) for b in range(B)]
    res = run_bass_kernel_spmd(nc, in_maps, core_ids=list(range(B)))
    out = np.stack([np.asarray(res.results[b]["outT"], np.float32).T for b in range(B)], axis=0)
    return np.ascontiguousarray(out)
prog_extend(Prog)
```

```python
import concourse.bass as bass
import concourse.mybir as mybir

F32 = mybir.dt.float32
BF16 = mybir.dt.bfloat16
ALU = mybir.AluOpType
AF = mybir.ActivationFunctionType
AX = mybir.AxisListType


class Region:
    __slots__ = ("w", "rs", "name", "excl")

    def __init__(self, name=""):
        self.w = None
        self.rs = {}
        self.name = name
        self.excl = False


class Ins:
    __slots__ = ("eng", "fn", "deps", "sig", "val", "dma", "dsem", "dval", "dprev")

    def __init__(self, eng, fn, dma=False):
        self.eng = eng
        self.fn = fn
        self.deps = []
        self.sig = False
        self.val = 0
        self.dma = dma
        self.dsem = None
        self.dval = 0
        self.dprev = 0


class Prog:
    ENGS = ("pe", "act", "dve", "pool", "sp")
    NDMA = 10

    def __init__(self, nc):
        self.nc = nc
        self.q = {e: [] for e in self.ENGS}
        self.dcount = {"sp": 0, "pool": 0, "act": 0}
        self.out_dmas = []

    def op(self, eng, fn, r=(), w=(), dma=False):
        ins = Ins(eng, fn, dma)
        deps = {}

        def add(d):
            if d is None or d is ins:
                return
            if (not d.dma) and (not dma) and d.eng == "pe" and eng == "pe":
                return
            deps[id(d)] = d

        for reg in r:
            add(reg.w)
            if reg.excl:
                for k, v in reg.rs.items():
                    if k != eng and k != "dma":
                        add(v)
        for reg in w:
            add(reg.w)
            for k, v in reg.rs.items():
                if k == "dma":
                    for d in v:
                        add(d)
                else:
                    add(v)
        ins.deps = list(deps.values())
        for d in ins.deps:
            d.sig = True
        for reg in r:
            if dma:
                reg.rs.setdefault("dma", []).append(ins)
            else:
                reg.rs[eng] = ins
        for reg in w:
            reg.w = ins
            reg.rs = {}
        if dma:
            i = self.dcount[eng]
            self.dcount[eng] = i + 1
            ins.dsem = (eng, i % self.NDMA)
            ins.dval = 16 * (i // self.NDMA + 1)
            ins.dprev = 16 * (i // self.NDMA)
        self.q[eng].append(ins)
        return ins

    def mm(self, out, lhsT, rhs, start=True, stop=True, r=(), w=(), **kw):
        return self.op("pe", lambda e: e.matmul(out, lhsT, rhs, start=start, stop=stop, **kw), r, w)

    def tr(self, out, in_, ident, r=(), w=()):
        return self.op("pe", lambda e: e.transpose(out, in_, ident), r, w)

    def act(self, out, in_, func, r=(), w=(), **kw):
        return self.op("act", lambda e: e.activation(out, in_, func, **kw), r, w)

    def dma(self, out, in_, r=(), w=(), q="sp", is_out=False, **kw):
        ins = self.op(q, lambda e: e.dma_start(out=out, in_=in_, **kw), r, w, dma=True)
        if is_out:
            self.out_dmas.append(ins)
        return ins

    def emit(self):
        nc = self.nc
        for e in self.ENGS:
            c = 0
            for ins in self.q[e]:
                if ins.sig and not ins.dma:
                    c += 1
                    ins.val = c
        import contextlib
        with contextlib.ExitStack() as st:
            esem = {e: st.enter_context(nc.semaphore("es_" + e)) for e in self.ENGS}
            dsem = {}
            for qn in ("sp", "pool"):
                for i in range(self.NDMA):
                    dsem[(qn, i)] = st.enter_context(nc.semaphore(f"ds_{qn}{i}"))
            block = st.enter_context(nc.Block())
            final = self.out_dmas

            def body(ename, eng):
                waited = {}

                def wait(key, sem, val):
                    if val <= 0:
                        return
                    if waited.get(key, 0) >= val:
                        return
                    eng.wait_ge(sem, val)
                    waited[key] = val

                for ins in self.q[ename]:
                    need = {}
                    for d in ins.deps:
                        if d.dma:
                            key, sem, val = d.dsem, dsem[d.dsem], d.dval
                        else:
                            key, sem, val = d.eng, esem[d.eng], d.val
                        if key not in need or need[key][1] < val:
                            need[key] = (sem, val)
                    for key, (sem, val) in need.items():
                        wait(key, sem, val)
                    if ins.dma:
                        wait(ins.dsem, dsem[ins.dsem], ins.dprev)
                    i = ins.fn(eng)
                    if ins.dma:
                        i.then_inc(dsem[ins.dsem], 16)
                    elif ins.sig:
                        i.then_inc(esem[ename], 1)
                if ename == "sp":
                    for d in final:
                        wait(d.dsem, dsem[d.dsem], d.dval)

            @block.tensor
            def _(e):
                body("pe", e)

            @block.scalar
            def _(e):
                body("act", e)

            @block.vector
            def _(e):
                body("dve", e)

            @block.gpsimd
            def _(e):
                body("pool", e)

            @block.sync
            def _(e):
                body("sp", e)
import contextlib
import numpy as np

D = 1024
KC = 8
N_IN = 11520
EPS = 1e-6


class RegMap(dict):
    def __init__(self, P, name, persistent=False):
        super().__init__()
        self.P = P
        self.name = name
        self.persistent = persistent

    def __missing__(self, k):
        r = self.P.region(f"{self.name}{k}", self.persistent)
        self[k] = r
        return r


class Ctx:
    def __init__(self, nc, P, S, debug):
        self.nc = nc
        self.P = P
        self.S = S
        self.debug = debug
        self.dram_regs = {}
        self.stack = None

    def din(self, name, shape, dt=F32):
        return self.nc.dram_tensor(name, list(shape), dt, kind="ExternalInput").ap()

    def dout(self, name, shape, dt=F32):
        return self.nc.dram_tensor(name, list(shape), dt, kind="ExternalOutput").ap()

    def dscr(self, name, shape, dt):
        kind = "ExternalOutput" if self.debug else "Internal"
        return self.nc.dram_tensor(name, list(shape), dt, kind=kind).ap()

    def sb(self, name, shape, dt=F32):
        self.uid = getattr(self, "uid", 0) + 1
        return self.stack.enter_context(self.nc.sbuf_tensor(f"{name}_{self.uid}", list(shape), dt))

    def ps(self, name, shape, dt=F32):
        self.uid = getattr(self, "uid", 0) + 1
        return self.stack.enter_context(self.nc.psum_tensor(f"{name}_{self.uid}", list(shape), dt))

    def regs(self, name, persistent=False):
        return RegMap(self.P, name, persistent)


def prog_extend(Prog):
    def region(self, name="", persistent=False):
        r = Region(name)
        if not persistent:
            r.w = self.cur_bar
            self.phase_regions.append(r)
        return r

    def end_phase(self):
        bar = self.op("sp", lambda e: e.nop(), w=list(self.phase_regions))
        self.cur_bar = bar
        self.phase_regions = []

    def tt(self, eng, out, in0, in1, op, r=(), w=()):
        return self.op(eng, lambda e: e.tensor_tensor(out=out, in0=in0, in1=in1, op=op), r, w)

    def ts(self, eng, out, in0, s1, s2=None, op0=ALU.mult, op1=None, r=(), w=()):
        if op1 is None:
            return self.op(eng, lambda e: e.tensor_scalar(out=out, in0=in0, scalar1=s1, scalar2=None, op0=op0), r, w)
        return self.op(eng, lambda e: e.tensor_scalar(out=out, in0=in0, scalar1=s1, scalar2=s2, op0=op0, op1=op1), r, w)

    def stt(self, out, in0, scalar, in1, op0, op1, r=(), w=()):
        return self.op("dve", lambda e: e.scalar_tensor_tensor(out=out, in0=in0, scalar=scalar, in1=in1, op0=op0, op1=op1), r, w)

    def cp(self, eng, out, in_, r=(), w=()):
        if eng == "act":
            return self.op(eng, lambda e: e.activation(out, in_, AF.Copy), r, w)
        return self.op(eng, lambda e: e.tensor_copy(out=out, in_=in_), r, w)

    def memset(self, eng, ap, val, r=(), w=()):
        return self.op(eng, lambda e: e.memset(ap, val), r, w)

    Prog.region = region
    Prog.end_phase = end_phase
    Prog.tt = tt
    Prog.ts = ts
    Prog.stt = stt
    Prog.cp = cp
    Prog.memset = memset
    Prog.cur_bar = None
    Prog.phase_regions = []


def make_consts():
    cols = {}
    parts = []
    off = 0

    def add(name, arr):
        nonlocal off
        arr = np.asarray(arr, np.float32).reshape(128, -1)
        cols[name] = (off, arr.shape[1])
        parts.append(arr)
        off += arr.shape[1]

    i = np.arange(128)
    add("ident", np.eye(128))
    add("ones", np.ones((128, 128)))
    add("tri_ge", (i[:, None] >= i[None, :]))
    add("tri_lt", (i[:, None] < i[None, :]))
    add("tri_le", (i[:, None] <= i[None, :]))
    t = np.arange(512)
    m = np.stack([(128 * k + i[:, None] < t[None, :]) for k in range(4)], axis=1)
    sbm = np.asarray(m, np.float32).reshape(128, -1)
    add("m_sl", (i[None, :] < i[:, None]))
    add("m_li", (i[None, :] <= i[:, None]))
    add("m_su", (i[:, None] < i[None, :]))
    add("m_ui", (i[:, None] <= i[None, :]))
    add("mask3", np.concatenate([(i[None, :] < i[:, None]), (i[:, None] < i[None, :]), (i[:, None] < i[None, :])], axis=1))
    cols["__sbm__"] = (off, sbm.shape[1])
    parts.append(sbm)
    return np.concatenate(parts, axis=1), cols


def phase_adaln(C, l, cst, cvec_d, w_ada_d, b_ada_d, nmix_d, nmoe_d, per):
    P, nc = C.P, C.nc
    with contextlib.ExitStack() as st:
        C.stack = st
        R = C.regs("p0_")
        cact = C.sb("cact", [128, 8])
        wst = [C.sb(f"wada{i}", [128, 8, 768]) for i in range(2)]
        ada = C.sb("ada", [128, 48])
        bada = C.sb("bada", [128, 48])
        nm = C.sb("nm", [128, 16])
        psa = C.ps("psa", [128, 48])
        P.dma(cact[:], cvec_d[:, :], w=[R["cact"]])
        P.dma(bada[:], b_ada_d[:, :], w=[R["bada"]])
        P.dma(nm[:, 0:8], nmix_d[:, :], w=[R["nm"]])
        P.dma(nm[:, 8:16], nmoe_d[:, :], w=[R["nm"]])
        P.act(cact[:], cact[:], AF.Silu, r=[R["cact"]], w=[R["cact"]])
        wv = w_ada_d.rearrange("(k p) j -> p k j", p=128)
        for g in range(8):
            wt = wst[g % 2]
            P.dma(wt[:], wv[:, :, g * 768:(g + 1) * 768], w=[R[f"w{g % 2}"]])
            for cc in range(6):
                j = g * 6 + cc
                for k in range(8):
                    P.mm(psa[:, j:j + 1], wt[:, k, cc * 128:(cc + 1) * 128], cact[:, k:k + 1],
                         start=(k == 0), stop=(k == 7), r=[R[f"w{g % 2}"], R["cact"]], w=[R["psa"]])
        P.tt("dve", ada[:], psa[:], bada[:], ALU.add, r=[R["psa"], R["bada"]], w=[R["ada"]])
        Rp = per["reg"]
        pt = per["t"]
        for (dst, sc_i, nofs) in ((0, 1, 0), (3, 4, 8)):
            P.ts("dve", pt[:, dst, :], ada[:, sc_i * 8:(sc_i + 1) * 8], 1.0, None, op0=ALU.add, r=[R["ada"]], w=[Rp])
            P.tt("dve", pt[:, dst, :], pt[:, dst, :], nm[:, nofs:nofs + 8], ALU.mult, r=[Rp, R["nm"]], w=[Rp])
        for (dst, src) in ((1, 0), (2, 2), (4, 3), (5, 5)):
            P.cp("dve", pt[:, dst, :], ada[:, src * 8:(src + 1) * 8], r=[R["ada"]], w=[Rp])
    P.end_phase()


def emit_norm_tile(C, R, xt, hT_out_fn, scale_ap, shift_ap, ones_f, sq, rs, tmp, ps_s, TT, eps_ap, tag):
    P = C.P
    P.act(sq[:], xt, AF.Square, r=[R["xt" + tag]], w=[R["sq"]])
    for k in range(8):
        P.mm(ps_s[:, :TT], ones_f, sq[:, k, :], start=(k == 0), stop=(k == 7), r=[R["sq"]], w=[R["ps_s"]])
    P.act(rs[:], ps_s[:, :TT], AF.Ln, scale=1.0 / 1024, bias=eps_ap, r=[R["ps_s"]], w=[R["rs"]])
    P.act(rs[:], rs[:], AF.Exp, scale=-0.5, r=[R["rs"]], w=[R["rs"]])
    for k in range(8):
        if shift_ap is None:
            P.stt(hT_out_fn(k), xt[:, k, :], scale_ap[:, k:k + 1], rs[:], ALU.mult, ALU.mult,
                  r=[R["xt" + tag], R["rs"]], w=[R["hT"]])
        else:
            P.stt(tmp[:, k, :], xt[:, k, :], scale_ap[:, k:k + 1], rs[:], ALU.mult, ALU.mult,
                  r=[R["xt" + tag], R["rs"]], w=[R["tmp"]])
            P.ts("pool", hT_out_fn(k), tmp[:, k, :], shift_ap[:, k:k + 1], None, op0=ALU.add,
                 r=[R["tmp"]], w=[R["hT"]])


def phase_proj(C, l, cst, xT_d, w_in_d, mu_d, per, scr):
    P, nc, S = C.P, C.nc, C.S
    TT = 256
    with contextlib.ExitStack() as st:
        C.stack = st
        R = C.regs("p1_")
        import os
        TOK = min(int(os.environ.get("PROJ_TOK", "4096")), S)
        hT = C.sb("hT", [128, 8, TOK + 1], BF16)
        off = {"p0": 0}
        xb = [C.sb(f"xb{i}", [128, 8, TT]) for i in range(2)]
        sq = C.sb("sq", [128, 8, TT])
        tmp = C.sb("tmp", [128, 8, TT])
        rs = C.sb("rs", [128, TT])
        ps_s = C.ps("ps_s", [128, 512])
        pt = per["t"]
        xv = xT_d.rearrange("(k p) s -> p k s", p=128)
        def norm_pass(p0):
            if p0 == 0:
                P.memset("pool", hT[:, :, 0:1], 0.0, w=[R["hT"]])
            else:
                P.cp("dve", hT[:, :, 0:1], hT[:, :, TOK:TOK + 1], r=[R["hT"]], w=[R["hT"]])
            for ti in range(TOK // TT):
                xt = xb[ti % 2]
                tag = str(ti % 2)
                P.dma(xt[:], xv[:, :, p0 + ti * TT:p0 + (ti + 1) * TT], w=[R["xt" + tag]])
                emit_norm_tile(C, R, xt[:], lambda k: hT[:, k, 1 + ti * TT:1 + (ti + 1) * TT],
                               pt[:, 0, :], pt[:, 1, :], cst["ones"], sq, rs, tmp, ps_s, TT, cst["eps"], tag)
        wst = [C.sb(f"wst{i}", [128, 8, 512]) for i in range(2)]
        wb = [C.sb(f"wb{i}", [128, 8, 512], BF16) for i in range(2)]
        wb2 = [C.sb(f"wb2{i}", [128, 8, 512], BF16) for i in range(2)]
        mug = [C.sb(f"mug{i}", [128, 512]) for i in range(2)]
        omug = [C.sb(f"omug{i}", [128, 512]) for i in range(2)]
        ev = [C.sb(f"ev{i}", [128, 512]) for i in range(4)]
        evb = [C.sb(f"evb{i}", [128, 512], BF16) for i in range(4)]
        pso = [C.ps(f"pso{i}", [128, 512]) for i in range(6)]
        wv = w_in_d.rearrange("(k p) j -> p k j", p=128)
        cnt = {"g": 0, "ps": 0, "ev": 0}
        NT5 = TOK // 512
        NT1 = TOK // 128

        def load_group(c0, width, rw):
            g = cnt["g"] % 2
            cnt["g"] += 1
            P.dma(wst[g][:, :, :width], wv[:, :, c0:c0 + width], w=[R[f"wst{g}"]])
            if not rw:
                P.cp("pool", wb[g][:, :, :width], wst[g][:, :, :width], r=[R[f"wst{g}"]], w=[R[f"wb{g}"]])
                return wb[g], None, [R[f"wb{g}"]]
            m0 = c0 - 5120
            P.dma(mug[g][:, :width], mu_d[:, m0:m0 + width], w=[R[f"mug{g}"]])
            P.ts("pool", omug[g][:, :width], mug[g][:, :width], -1.0, 1.0, op0=ALU.mult, op1=ALU.add,
                 r=[R[f"mug{g}"]], w=[R[f"omug{g}"]])
            for k in range(8):
                P.tt("pool" if k % 2 else "dve", wb[g][:, k, :width], wst[g][:, k, :width], omug[g][:, :width], ALU.mult,
                     r=[R[f"wst{g}"], R[f"omug{g}"]], w=[R[f"wb{g}"]])
                P.tt("dve" if k % 2 else "pool", wb2[g][:, k, :width], wst[g][:, k, :width], mug[g][:, :width], ALU.mult,
                     r=[R[f"wst{g}"], R[f"mug{g}"]], w=[R[f"wb2{g}"]])
            return wb[g], wb2[g], [R[f"wb{g}"], R[f"wb2{g}"]]

        def next_ps():
            i = cnt["ps"] % 6
            cnt["ps"] += 1
            return pso[i], R[f"pso{i}"]

        def next_ev(bf):
            i = cnt["ev"] % 4
            cnt["ev"] += 1
            return (evb[i], R[f"evb{i}"]) if bf else (ev[i], R[f"ev{i}"])

        def fm_group(c0, width, rw, evac):
            w1, w2, wr = load_group(c0, width, rw)
            for cc in range(width // 128):
                for tt_ in range(NT5):
                    pt_, pr = next_ps()
                    n = 16 if rw else 8
                    for k in range(8):
                        P.mm(pt_[:], w1[:, k, cc * 128:(cc + 1) * 128], hT[:, k, 1 + tt_ * 512:1 + (tt_ + 1) * 512],
                             start=(k == 0), stop=(k == 7 and not rw), r=wr + [R["hT"]], w=[pr])
                    if rw:
                        for k in range(8):
                            P.mm(pt_[:], w2[:, k, cc * 128:(cc + 1) * 128], hT[:, k, tt_ * 512:(tt_ + 1) * 512],
                                 start=False, stop=(k == 7), r=wr + [R["hT"]], w=[pr])
                    evac(c0 + cc * 128, tt_, pt_, pr)

        def tm_group(c0, width, rw, evac):
            w1, w2, wr = load_group(c0, width, rw)
            for t1 in range(NT1):
                pt_, pr = next_ps()
                for k in range(8):
                    P.mm(pt_[:, :width], hT[:, k, 1 + t1 * 128:1 + (t1 + 1) * 128], w1[:, k, :width],
                         start=(k == 0), stop=(k == 7 and not rw), r=wr + [R["hT"]], w=[pr])
                if rw:
                    for k in range(8):
                        P.mm(pt_[:, :width], hT[:, k, t1 * 128:(t1 + 1) * 128], w2[:, k, :width],
                             start=False, stop=(k == 7), r=wr + [R["hT"]], w=[pr])
                evac(c0, t1, pt_, pr)

        def ev_lru(col, tt_, pt_, pr):
            e, er = next_ev(False)
            P.cp("act", e[:], pt_[:], r=[pr], w=[er])
            P.dma(scr["lruT"][col:col + 128, off['p0'] + tt_ * 512:off['p0'] + (tt_ + 1) * 512], e[:], r=[er], w=[P.region()])

        def ev_q(col, tt_, pt_, pr):
            e, er = next_ev(True)
            P.ts("dve", e[:], pt_[:], float(128 ** -0.5), None, op0=ALU.mult, r=[pr], w=[er])
            c = col - 2048
            P.dma(scr["qT"][c:c + 128, off['p0'] + tt_ * 512:off['p0'] + (tt_ + 1) * 512], e[:], r=[er], w=[P.region()])

        def ev_k(col, tt_, pt_, pr):
            e, er = next_ev(True)
            P.cp("act", e[:], pt_[:], r=[pr], w=[er])
            c = col - 3072
            P.dma(scr["kT"][c:c + 128, off['p0'] + tt_ * 512:off['p0'] + (tt_ + 1) * 512], e[:], r=[er], w=[P.region()])

        def ev_v(c0, t1, pt_, pr):
            e, er = next_ev(True)
            P.cp("dve", e[:], pt_[:], r=[pr], w=[er])
            c = c0 - 4096
            P.dma(scr["v"][off['p0'] + t1 * 128:off['p0'] + (t1 + 1) * 128, c:c + 512], e[:], r=[er], w=[P.region()])

        def ev_rkv(c0, t1, pt_, pr):
            e, er = next_ev(False)
            P.cp("act" if t1 % 2 else "dve", e[:], pt_[:], r=[pr], w=[er])
            c = c0 - 5120
            P.dma(scr["rkv"][off['p0'] + t1 * 128:off['p0'] + (t1 + 1) * 128, c:c + 512], e[:], r=[er], w=[P.region()])

        def ev_lora(col, tt_, pt_, pr):
            e, er = next_ev(True)
            if col == 8192:
                P.act(e[0:64, :], pt_[0:64, :], AF.Tanh, r=[pr], w=[er])
                P.cp("dve", e[64:128, :], pt_[64:128, :], r=[pr], w=[er])
            else:
                P.act(e[:], pt_[:], AF.Sigmoid, r=[pr], w=[er])
            c = col - 8192
            P.dma(scr["loraT"][c:c + 128, off['p0'] + tt_ * 512:off['p0'] + (tt_ + 1) * 512], e[:], r=[er], w=[P.region()])

        def ev_gate(col, tt_, pt_, pr):
            e, er = next_ev(False)
            P.act(e[:], pt_[:], AF.Sigmoid, r=[pr], w=[er])
            c = col - 8448
            P.dma(scr["gatesT"][c:c + 128, off['p0'] + tt_ * 512:off['p0'] + (tt_ + 1) * 512], e[:], r=[er], w=[P.region()])

        for p0 in range(0, S, TOK):
          off["p0"] = p0
          norm_pass(p0)
          for c0 in range(0, 2048, 512):
            fm_group(c0, 512, False, ev_lru)
          for c0 in range(2048, 3072, 512):
              fm_group(c0, 512, False, ev_q)
          for c0 in range(3072, 4096, 512):
              fm_group(c0, 512, False, ev_k)
          for c0 in range(4096, 5120, 512):
              tm_group(c0, 512, False, ev_v)
          for c0 in range(5120, 8192, 512):
              tm_group(c0, 512, True, ev_rkv)
          fm_group(8192, 256, True, ev_lora)
          for c0 in range(8448, 11520, 512):
              fm_group(c0, 512, False, ev_gate)
    P.end_phase()
GELU_K = 1.5957691216057308


def phase_lru(C, l, cst, Wl, scr):
    P, nc, S = C.P, C.nc, C.S
    import os
    TL = min(int(os.environ.get("LRU_TL", "2048")), S)
    with contextlib.ExitStack() as st:
        C.stack = st
        R = C.regs("lru_")
        lv = C.sb("lv", [128, 8, 8])
        wa = C.sb("wa", [128, 8, 128])
        wx = C.sb("wx", [128, 8, 128])
        c12 = C.sb("c12", [128, 2, 8])
        tiny = C.sb("tiny", [128, 1])
        carry = C.sb("carry", [128, 1])
        xin = [C.sb(f"xin{i}", [128, TL + 3]) for i in range(2)]
        gin = [C.sb(f"gin{i}", [128, TL]) for i in range(2)]
        xc = C.sb("xc", [128, TL])
        rg = C.sb("rg", [128, TL])
        ig = C.sb("ig", [128, TL])
        a_t = C.sb("a_t", [128, TL])
        e2 = C.sb("e2", [128, TL])
        b_t = C.sb("b_t", [128, TL])
        h_t = C.sb("h_t", [128, TL])
        u_t = C.sb("u_t", [128, TL])
        y_t = [C.sb(f"y_t{i}", [128, TL], BF16) for i in range(2)]
        psr = [C.ps(f"psr{i}", [128, 512]) for i in range(2)]
        psi = [C.ps(f"psi{i}", [128, 512]) for i in range(2)]
        P.dma(lv[:], Wl["lruv"][:, :, :], w=[R["lv"]])
        P.dma(wa[:], Wl["lru_wa"][:, :, :], w=[R["wa"]])
        P.dma(wx[:], Wl["lru_wx"][:, :, :], w=[R["wx"]])
        P.memset("pool", tiny[:], 1e-20, w=[R["tiny"]])
        P.act(c12[:, 0, :], lv[:, 7, :], AF.Exp, scale=-1.0, r=[R["lv"]], w=[R["c12"]])
        P.act(c12[:, 0, :], c12[:, 0, :], AF.Ln, bias=1.0, r=[R["c12"]], w=[R["c12"]])
        P.ts("dve", c12[:, 1, :], c12[:, 0, :], -16.0, None, op0=ALU.mult, r=[R["c12"]], w=[R["c12b"]])
        P.ts("dve", c12[:, 0, :], c12[:, 0, :], -8.0, None, op0=ALU.mult, r=[R["c12"], R["c12b"]], w=[R["c12"]])
        it = 0
        for c in range(8):
            rows = slice(c * 128, (c + 1) * 128)
            grows = slice(1024 + c * 128, 1024 + (c + 1) * 128)
            for ti in range(S // TL):
                t0 = ti * TL
                xi, gi = xin[it % 2], gin[it % 2]
                xr, gr = R[f"xin{it % 2}"], R[f"gin{it % 2}"]
                if ti == 0:
                    P.memset("pool", xi[:, 0:3], 0.0, w=[xr])
                    P.dma(xi[:, 3:3 + TL], scr["lruT"][rows, 0:TL], w=[xr])
                else:
                    P.dma(xi[:, 0:3 + TL], scr["lruT"][rows, t0 - 3:t0 + TL], w=[xr])
                P.dma(gi[:], scr["lruT"][grows, t0:t0 + TL], w=[gr])
                cw = lambda k: lv[:, k, c:c + 1]
                P.ts("dve", xc[:], xi[:, 3:3 + TL], cw(0), cw(4), op0=ALU.mult, op1=ALU.add, r=[xr, R["lv"]], w=[R["xc"]])
                for k in (1, 2, 3):
                    P.stt(xc[:], xi[:, 3 - k:3 - k + TL], cw(k), xc[:], ALU.mult, ALU.add, r=[xr, R["xc"]], w=[R["xc"]])
                for j in range(TL // 512):
                    sl = slice(j * 512, (j + 1) * 512)
                    P.mm(psr[j % 2][:], wa[:, c, :], xc[:, sl], r=[R["wa"], R["xc"]], w=[R[f"psr{j % 2}"]])
                    P.mm(psi[j % 2][:], wx[:, c, :], xc[:, sl], r=[R["wx"], R["xc"]], w=[R[f"psi{j % 2}"]])
                    P.act(rg[:, sl], psr[j % 2][:], AF.Sigmoid, bias=lv[:, 5, c:c + 1], r=[R[f"psr{j % 2}"]], w=[R["rg"]])
                    P.act(ig[:, sl], psi[j % 2][:], AF.Sigmoid, bias=lv[:, 6, c:c + 1], r=[R[f"psi{j % 2}"]], w=[R["ig"]])
                P.act(a_t[:], rg[:], AF.Exp, scale=c12[:, 0, c:c + 1], r=[R["rg"], R["c12"]], w=[R["a"]])
                P.act(e2[:], rg[:], AF.Exp, scale=c12[:, 1, c:c + 1], r=[R["rg"], R["c12b"]], w=[R["e2"]])
                P.ts("pool", e2[:], e2[:], -1.0, 1.0, op0=ALU.mult, op1=ALU.add, r=[R["e2"]], w=[R["e2"]])
                P.act(e2[:], e2[:], AF.Ln, bias=tiny[:], r=[R["e2"], R["tiny"]], w=[R["e2"]])
                P.act(e2[:], e2[:], AF.Exp, scale=0.5, r=[R["e2"]], w=[R["e2"]])
                P.tt("dve", b_t[:], e2[:], ig[:], ALU.mult, r=[R["e2"], R["ig"]], w=[R["b"]])
                P.tt("pool", b_t[:], b_t[:], xc[:], ALU.mult, r=[R["b"], R["xc"]], w=[R["b"]])
                init = 0.0 if ti == 0 else carry[:, 0:1]
                P.op("dve", (lambda init=init: (lambda e: e.tensor_tensor_scan(out=h_t[:], data0=a_t[:], data1=b_t[:],
                                                                             initial=init, op0=ALU.mult, op1=ALU.add)))(),
                     r=[R["a"], R["b"], R["carry"]], w=[R["h"]])
                P.cp("pool", carry[:], h_t[:, TL - 1:TL], r=[R["h"]], w=[R["carry"]])
                P.tt("pool", u_t[:], gi[:], gi[:], ALU.mult, r=[gr], w=[R["u"]])
                P.ts("pool", u_t[:], u_t[:], 0.044715, 1.0, op0=ALU.mult, op1=ALU.add, r=[R["u"]], w=[R["u"]])
                P.tt("dve", u_t[:], u_t[:], gi[:], ALU.mult, r=[R["u"], gr], w=[R["u"]])
                P.act(u_t[:], u_t[:], AF.Sigmoid, scale=GELU_K, r=[R["u"]], w=[R["u"]])
                P.tt("pool", u_t[:], u_t[:], gi[:], ALU.mult, r=[R["u"], gr], w=[R["u"]])
                yt, yr = y_t[it % 2], R[f"y{it % 2}"]
                P.tt("dve", yt[:], u_t[:], h_t[:], ALU.mult, r=[R["u"], R["h"]], w=[yr])
                P.dma(scr["yaT"][rows, t0:t0 + TL], yt[:], r=[yr], w=[P.region()])
                it += 1
    P.end_phase()


def phase_sb(C, l, cst, scr):
    P, nc, S = C.P, C.nc, C.S
    NQ = S // 512
    NB = S // 128
    with contextlib.ExitStack() as st:
        C.stack = st
        R = C.regs("sb_")
        qh = [C.sb(f"qh{i}", [128, S], BF16) for i in range(2)]
        kh = [C.sb(f"kh{i}", [128, S], BF16) for i in range(2)]
        vh = [C.sb(f"vh{i}", [128, NB, 128], BF16) for i in range(2)]
        NS = 2
        e_b = [[C.sb(f"e{s}_{i}", [128, 512]) for i in range(2)] for s in range(NS)]
        sp_b = [[C.sb(f"sp{s}_{i}", [128, 512], BF16) for i in range(2)] for s in range(NS)]
        d_b = [[C.sb(f"d{s}_{i}", [128, 512]) for i in range(2)] for s in range(NS)]
        at_b = [[C.sb(f"at{s}_{i}", [128, 512], BF16) for i in range(2)] for s in range(NS)]
        yo = [C.sb(f"yo{s}", [128, 512], BF16) for s in range(NS)]
        pz = [[C.ps(f"pz{s}_{i}", [128, 512]) for i in range(2)] for s in range(NS)]
        pc = [C.ps(f"pc{s}", [128, 512]) for s in range(NS)]
        po = [C.ps(f"po{s}", [128, 512]) for s in range(NS)]
        tri_ge, tri_lt, mask = cst["tri_ge_b"], cst["tri_lt_b"], cst["sbmask_b"]
        vv = scr["v"].rearrange("(n p) d -> p n d", p=128)

        def stream(s, hd, qt, hb):
            q_t, k_t, v_t = qh[hb], kh[hb], vh[hb]
            hr = [R[f"q{hb}"], R[f"k{hb}"], R[f"v{hb}"]]
            kbs = list(range(4 * qt + 3, -1, -1))
            pcr, por = R[f"pc{s}"], R[f"po{s}"]

            def front(idx):
                kb = kbs[idx]
                i2 = idx % 2
                z, zr = pz[s][i2], R[f"pz{s}_{i2}"]
                et, er = e_b[s][i2], R[f"e{s}_{i2}"]
                spt, spr = sp_b[s][i2], R[f"sp{s}_{i2}"]
                P.mm(z[:], k_t[:, kb * 128:(kb + 1) * 128], q_t[:, qt * 512:(qt + 1) * 512], r=hr[0:2], w=[zr])
                P.act(et[:], z[:], AF.Exp, r=[zr], w=[er])
                P.act(spt[:], et[:], AF.Ln, bias=1.0, r=[er], w=[spr])
                mi = kb - 4 * qt
                if mi >= 0:
                    P.tt("dve", spt[:], spt[:], mask[:, mi * 512:(mi + 1) * 512], ALU.mult, r=[spr], w=[spr])

            def back(idx):
                kb = kbs[idx]
                last = idx == len(kbs) - 1
                i2 = idx % 2
                et, er = e_b[s][i2], R[f"e{s}_{i2}"]
                spt, spr = sp_b[s][i2], R[f"sp{s}_{i2}"]
                dt_, dr = d_b[s][i2], R[f"d{s}_{i2}"]
                att, atr = at_b[s][i2], R[f"at{s}_{i2}"]
                P.mm(pc[s][:], tri_ge, spt[:], start=(idx == 0), stop=last, r=[spr], w=[pcr], skip_group_check=True)
                P.act(dt_[:], pc[s][:], AF.Exp, scale=-1.0, r=[pcr], w=[dr])
                if not last:
                    P.mm(pc[s][:], tri_lt, spt[:], start=False, stop=False, r=[spr], w=[pcr], skip_group_check=True)
                P.tt("dve", att[:], et[:], dt_[:], ALU.mult, r=[er, dr], w=[atr])
                mi = kb - 4 * qt
                if mi >= 0:
                    P.tt("dve", att[:], att[:], mask[:, mi * 512:(mi + 1) * 512], ALU.mult, r=[atr], w=[atr])
                P.mm(po[s][:], v_t[:, kb, :], att[:], start=(idx == 0), stop=last, r=[atr, hr[2]], w=[por])

            front(0)
            for idx in range(len(kbs)):
                if idx + 1 < len(kbs):
                    front(idx + 1)
                back(idx)
                yield
            P.cp("dve", yo[s][:], po[s][:], r=[R[f"po{s}"]], w=[R[f"yo{s}"]])
            P.dma(scr["ybT"][hd * 128:(hd + 1) * 128, qt * 512:(qt + 1) * 512], yo[s][:], r=[R[f"yo{s}"]], w=[P.region()])

        for hd in range(8):
            hb = hd % 2
            rows = slice(hd * 128, (hd + 1) * 128)
            P.dma(qh[hb][:], scr["qT"][rows, :], w=[R[f"q{hb}"]])
            P.dma(kh[hb][:], scr["kT"][rows, :], w=[R[f"k{hb}"]])
            for n0 in range(0, NB, 16):
                n1 = min(NB, n0 + 16)
                P.dma(vh[hb][:, n0:n1, :], vv[:, n0:n1, rows], w=[R[f"v{hb}"]])
            order = []
            lo, hi = 0, NQ - 1
            while lo <= hi:
                order.append(hi)
                if lo != hi:
                    order.append(lo)
                lo += 1
                hi -= 1
            pending = list(order)
            active = []
            free_slots = list(range(NS))
            while pending or active:
                while pending and free_slots:
                    s = free_slots.pop(0)
                    active.append((s, stream(s, hd, pending.pop(0), hb)))
                nxt = []
                for (s, g) in active:
                    try:
                        next(g)
                        nxt.append((s, g))
                    except StopIteration:
                        free_slots.append(s)
                active = nxt
    P.end_phase()


RW_C0 = 0.6065306597126334


def phase_rwkv(C, l, cst, Wl, scr):
    P, nc, S = C.P, C.nc, C.S
    NCH = S // 128
    with contextlib.ExitStack() as st:
        C.stack = st
        R = C.regs("rw_")
        rwv = C.sb("rwv", [128, 7, 1024])
        wst = C.sb("rw_wst", [128, 1024])
        waup = C.sb("waup", [128, 1024], BF16)
        gup = C.sb("gup", [128, 1024], BF16)
        epsg = C.sb("epsg", [128, 1])
        T1 = C.sb("T1", [128, 3, 1024])
        lt = C.sb("lt", [128, 2, 128], BF16)
        lta = C.sb("lta", [64, 128], BF16)
        aup = C.sb("aup", [64, 1024], BF16)
        sgw = C.sb("sgw", [128, 1024])
        a_t = C.sb("rwa", [128, 1024])
        kk = C.sb("kk", [128, 1024])
        b_t = C.sb("rwb", [128, 1024])
        clsb = C.sb("clsb", [128, 1024])
        tmp = C.sb("rwtmp", [128, 1024])
        E = [C.sb(f"rwE{i}", [128, 1024]) for i in range(2)]
        gC = C.sb("gC", [64, 1024])
        gate = C.sb("rwgate", [128, 1024])
        small = C.sb("rwsmall", [128, 8, 16])
        prod = {n: C.sb("pr_" + n, [128, 1024], BF16) for n in ("kq", "bh", "kh", "rq", "bE", "kE", "v")}
        XT = {n: C.sb("xt_" + n, [64, 16, 128], BF16) for n in ("kq", "bh", "kh", "rq")}
        G = 4
        xs = [C.sb(f"xs{i}", [128, 256]) for i in range(G)]
        akt = [C.sb(f"akt{i}", [128, 128], BF16) for i in range(G)]
        Mn = [[C.sb(f"Mn{i}_{j}", [128, 256]) for j in range(2)] for i in range(G)]
        TTb = [[C.sb(f"TT{i}_{j}", [128, 128]) for j in range(2)] for i in range(G)]
        TTf = [C.sb(f"TTf{i}", [128, 128], BF16) for i in range(G)]
        kcw = [C.sb(f"kcw{i}", [128, 128], BF16) for i in range(G)]
        dg = [C.sb(f"dg{i}", [64, 64]) for i in range(G)]
        BbT = C.sb("BbT", [128, 16, 128], BF16)
        BkT = C.sb("BkT", [128, 16, 128], BF16)
        Uv = C.sb("Uv", [128, 16, 64], BF16)
        RcT = C.sb("RcT", [64, 16, 128], BF16)
        GT = C.sb("GT", [64, 16, 64])
        H_a = C.sb("H_a", [64, 16, 64])
        st_f = C.sb("st_f", [64, 16, 64])
        st_b = C.sb("st_b", [64, 16, 64], BF16)
        ysb = C.sb("ysb", [128, 1024])
        ycs = C.sb("ycs", [128, 8, 128], BF16)
        PW = C.ps("PW", [128, 1024])
        PT = C.ps("PT", [128, 2048], BF16)
        banks = [C.ps(f"rwbk{i}", [128, 512]) for i in range(4)]
        for _k in ("PW", "PT", "bk0", "bk1", "bk2", "bk3"):
            R[_k].excl = True
        bcnt = [0]

        def bank():
            i = bcnt[0] % 4
            bcnt[0] += 1
            return banks[i], R[f"bk{i}"]

        ident_f, ident_b, ones_f, tri_le = cst["ident"], cst["ident_b"], cst["ones"], cst["tri_le"]
        mask3, m_ui = cst["mask3"], cst["m_ui"]
        P.dma(rwv[:], Wl["rwv"][:, :, :], w=[R["rwv"]])
        P.dma(wst[0:64, :], Wl["rw_w_up"][:, :], w=[R["wst"]])
        P.dma(wst[64:128, :], Wl["rw_a_up"][:, :], w=[R["wst"]])
        P.cp("dve", waup[:], wst[:], r=[R["wst"]], w=[R["waup"]])
        P.dma(wst[0:64, :], Wl["rw_a_up"][:, :], w=[R["wst"]])
        P.cp("dve", aup[:], wst[0:64, :], r=[R["wst"]], w=[R["aup"]])
        P.dma(wst[:], Wl["rw_g_up"][:, :], w=[R["wst"]])
        P.cp("dve", gup[:], wst[:], r=[R["wst"]], w=[R["gup"]])
        P.memset("pool", epsg[:], 64e-5, w=[R["epsg"]])
        P.memset("pool", st_f[:], 0.0, w=[R["st_f"]])
        P.memset("pool", st_b[:], 0.0, w=[R["st_b"]])
        ycv = scr["ycT"].rearrange("(k p) s -> p k s", p=128)
        hv = lambda t: t.rearrange("p (h j) -> p h j", j=64)
        rW, rA, rKK, rKA, rRK, rLW, rLB = (rwv[:, i, :] for i in range(7))

        for n in range(NCH):
            t0 = n * 128
            r_, k_, v_ = T1[:, 0, :], T1[:, 1, :], T1[:, 2, :]
            P.dma(T1[:], scr["rkv"][t0:t0 + 128, :].rearrange("p (a c) -> p a c", a=3), w=[R["T1"]])
            P.dma(lt[:, 0, :], scr["loraT"][0:128, t0:t0 + 128], w=[R["lt"]])
            P.dma(lt[:, 1, :], scr["loraT"][128:256, t0:t0 + 128], w=[R["lt"]])
            P.dma(lta[:], scr["loraT"][64:128, t0:t0 + 128], w=[R["lta"]])
            for hf in range(2):
                sl = slice(hf * 512, (hf + 1) * 512)
                P.mm(PW[:, sl], lt[0:64, 0, :], waup[0:64, sl], r=[R["lt"], R["waup"]], w=[R["PW"]])
            P.tt("dve", sgw[:], PW[:], rW, ALU.add, r=[R["PW"], R["rwv"]], w=[R["sgw"]])
            P.act(sgw[:], sgw[:], AF.Sigmoid, r=[R["sgw"]], w=[R["sgw"]])
            for hf in range(2):
                sl = slice(hf * 512, (hf + 1) * 512)
                P.mm(PW[:, sl], lta[:], aup[:, sl], r=[R["lta"], R["aup"]], w=[R["PW"]])
            P.tt("dve", a_t[:], PW[:], rA, ALU.add, r=[R["PW"], R["rwv"]], w=[R["a"]])
            P.act(a_t[:], a_t[:], AF.Sigmoid, r=[R["a"]], w=[R["a"]])
            for hf in range(2):
                sl = slice(hf * 512, (hf + 1) * 512)
                P.mm(PW[:, sl], lt[:, 1, :], gup[:, sl], r=[R["lt"], R["gup"]], w=[R["PW"]])
            P.cp("act", gate[:], PW[:], r=[R["PW"]], w=[R["gate"]])
            P.tt("dve", kk[:], k_, rKK, ALU.mult, r=[R["T1"], R["rwv"]], w=[R["kk"]])
            P.tt("pool", tmp[:], kk[:], kk[:], ALU.mult, r=[R["kk"]], w=[R["tmp"]])
            P.op("dve", lambda e: e.tensor_reduce(out=small[:, 0, :], in_=hv(tmp[:]), axis=AX.X, op=ALU.add),
                 r=[R["tmp"]], w=[R["sm0"]])
            P.ts("dve", small[:, 0, :], small[:, 0, :], 1e-24, None, op0=ALU.max, r=[R["sm0"]], w=[R["sm0"]])
            P.act(small[:, 0, :], small[:, 0, :], AF.Ln, r=[R["sm0"]], w=[R["sm0"]])
            P.act(small[:, 0, :], small[:, 0, :], AF.Exp, scale=-0.5, r=[R["sm0"]], w=[R["sm0"]])
            for h in range(16):
                hs = slice(h * 64, (h + 1) * 64)
                P.ts("dve" if h % 2 else "pool", kk[:, hs], kk[:, hs], small[:, 0, h:h + 1], None, op0=ALU.mult,
                     r=[R["kk"], R["sm0"]], w=[R["kk"]])
            P.stt(tmp[:], a_t[:], -1.0, rKA, ALU.add, ALU.mult, r=[R["a"], R["rwv"], R["tmp"]], w=[R["tmp"]])
            P.stt(k_, tmp[:], 1.0, k_, ALU.add, ALU.mult, r=[R["tmp"], R["T1"], R["kk"]], w=[R["T1"]])
            P.tt("pool", b_t[:], kk[:], a_t[:], ALU.mult, r=[R["kk"], R["a"]], w=[R["b"]])
            P.tt("pool", tmp[:], r_, k_, ALU.mult, r=[R["T1"]], w=[R["tmp"]])
            P.tt("pool", tmp[:], tmp[:], rRK, ALU.mult, r=[R["tmp"], R["rwv"]], w=[R["tmp"]])
            P.op("dve", lambda e: e.tensor_reduce(out=small[:, 1, :], in_=hv(tmp[:]), axis=AX.X, op=ALU.add),
                 r=[R["tmp"]], w=[R["sm1"]])
            for hf in range(2):
                sl = slice(hf * 512, (hf + 1) * 512)
                P.mm(PW[:, sl], tri_le, sgw[:, sl], r=[R["sgw"]], w=[R["PW"]])
            P.cp("act", clsb[:], PW[:], r=[R["PW"]], w=[R["clsb"]])
            for hf in range(2):
                sl = slice(hf * 512, (hf + 1) * 512)
                P.mm(PW[:, sl], ones_f, sgw[:, sl], r=[R["sgw"]], w=[R["PW"]])
            E0, E1 = E[0], E[1]
            P.act(E0[:], clsb[:], AF.Exp, scale=-RW_C0, r=[R["clsb"]], w=[R["E0"]])
            P.tt("dve", prod["rq"][:], r_, E0[:], ALU.mult, r=[R["T1"], R["E0"]], w=[R["p_rq"]])
            P.act(E1[:], clsb[:], AF.Exp, scale=RW_C0, r=[R["clsb"]], w=[R["E1"]])
            P.tt("pool", prod["bh"][:], b_t[:], E1[:], ALU.mult, r=[R["b"], R["E1"]], w=[R["p_bh"]])
            P.tt("dve", prod["kh"][:], k_, E1[:], ALU.mult, r=[R["T1"], R["E1"]], w=[R["p_kh"]])
            P.tt("pool", tmp[:], clsb[:], sgw[:], ALU.subtract, r=[R["clsb"], R["sgw"], R["tmp"]], w=[R["tmp"]])
            P.act(E0[:], tmp[:], AF.Exp, scale=-RW_C0, r=[R["tmp"], R["E0"]], w=[R["E0"]])
            P.tt("dve", prod["kq"][:], kk[:], E0[:], ALU.mult, r=[R["kk"], R["E0"]], w=[R["p_kq"]])
            P.tt("dve", tmp[:], PW[:], clsb[:], ALU.subtract, r=[R["PW"], R["clsb"], R["tmp"]], w=[R["tmp"]])
            P.act(E1[:], tmp[:], AF.Exp, scale=-RW_C0, r=[R["tmp"], R["E1"]], w=[R["E1"]])
            P.tt("pool", prod["bE"][:], b_t[:], E1[:], ALU.mult, r=[R["b"], R["E1"]], w=[R["p_bE"]])
            P.tt("dve", prod["kE"][:], k_, E1[:], ALU.mult, r=[R["T1"], R["E1"]], w=[R["p_kE"]])
            P.act(gC[:], PW[0:64, :], AF.Exp, scale=-RW_C0, r=[R["PW"]], w=[R["gC"]])
            P.cp("pool", prod["v"][:], v_, r=[R["T1"]], w=[R["p_v"]])
            import os as _os
            _stop = _os.environ.get("RW_STOP", "")
            if _stop == "A":
                continue
            PTv = PT[0:64, :].rearrange("p (h t) -> p h t", t=128)
            for qi, qn in enumerate(("kq", "bh", "kh", "rq")):
                for h in range(16):
                    P.tr(PTv[:, h, :], prod[qn][:, h * 64:(h + 1) * 64], ident_b, r=[R["p_" + qn]], w=[R["PT"]])
                P.cp("act" if qi % 2 else "dve", XT[qn][:], PTv, r=[R["PT"]], w=[R["xt_" + qn]])

            if _stop == "T":
                continue
            def head(slot, h):
                hs = slice(h * 64, (h + 1) * 64)
                kqT, bhT, khT, rqT = (XT[q][:, h, :] for q in ("kq", "bh", "kh", "rq"))
                xr = [R["xt_kq"], R["xt_bh"], R["xt_kh"], R["xt_rq"]]
                X, Xr = banks[slot], R[f"bk{slot}"]
                P.mm(X[:, 0:128], kqT, bhT, r=xr, w=[Xr])
                P.mm(X[:, 128:256], bhT, kqT, r=xr, w=[Xr])
                P.mm(X[:, 256:384], khT, kqT, r=xr, w=[Xr])
                P.mm(X[:, 384:512], bhT, rqT, r=xr, w=[Xr])
                yield
                xs_, xsr = xs[slot], R[f"xs{slot}"]
                P.tt("dve", xs_[:], X[:, 0:256], mask3[:, 0:256], ALU.mult, r=[Xr], w=[xsr])
                P.tt("dve", akt[slot][:], X[:, 256:384], mask3[:, 256:384], ALU.mult, r=[Xr], w=[R[f"akt{slot}"]])
                P.tt("dve", BbT[:, h, :], X[:, 384:512], m_ui, ALU.mult, r=[Xr], w=[R[f"BbT{h}"]])
                tcur = 0
                TT_, TTr = TTb[slot][tcur], R[f"TT{slot}_{tcur}"]
                P.tt("pool", TT_[:], ident_f, xs_[:, 128:256], ALU.subtract, r=[xsr], w=[TTr])
                M_, MT_, Mr = xs_[:, 0:128], xs_[:, 128:256], xsr
                yield
                for lev in range(6):
                    L, Lr = X, Xr
                    P.mm(L[:, 0:128], MT_, M_, r=[Mr], w=[Lr])
                    if lev < 5:
                        P.mm(L[:, 128:256], M_, MT_, r=[Mr], w=[Lr])
                    if lev == 0:
                        P.mm(L[:, 384:512], khT, rqT, r=xr, w=[Lr])
                    yield
                    if lev == 0:
                        P.tt("dve", BkT[:, h, :], L[:, 384:512], m_ui, ALU.mult, r=[Lr], w=[R[f"BkT{h}"]])
                    mn, mnr = Mn[slot][lev % 2], R[f"Mn{slot}_{lev % 2}"]
                    wdt = 256 if lev < 5 else 128
                    P.cp("act" if (h + lev) % 2 else "dve", mn[:, 0:wdt], L[:, 0:wdt], r=[Lr], w=[mnr])
                    P.mm(L[:, 256:384], mn[:, 0:128], TT_[:], r=[mnr, TTr], w=[Lr])
                    yield
                    tn = 1 - tcur
                    TTn, TTnr = TTb[slot][tn], R[f"TT{slot}_{tn}"]
                    P.tt("dve", TTn[:], L[:, 256:384], TT_[:], ALU.add, r=[Lr, TTr], w=[TTnr])
                    TT_, TTr, tcur = TTn, TTnr, tn
                    M_, MT_, Mr = mn[:, 0:128], mn[:, 128:256], mnr
                Z2, Z2r = X, Xr
                P.cp("pool", TTf[slot][:], TT_[:], r=[TTr], w=[R[f"TTf{slot}"]])
                TT_, TTr = TTf[slot], R[f"TTf{slot}"]
                P.mm(Z2[:, 0:64], TT_[:], prod["kq"][:, hs], r=[TTr, R["p_kq"]], w=[Z2r])
                P.mm(Z2[:, 64:128], akt[slot][:], prod["v"][:, hs], r=[R[f"akt{slot}"], R["p_v"]], w=[Z2r])
                yield
                kc_, kcr = kcw[slot], R[f"kcw{slot}"]
                P.cp("act", kc_[:], Z2[:, 0:128], r=[Z2r], w=[kcr])
                P.mm(Z2[:, 128:192], TT_[:], kc_[:, 64:128], r=[TTr, kcr], w=[Z2r])
                P.mm(Z2[0:64, 192:256], kc_[:, 0:64], prod["bE"][:, hs], r=[kcr, R["p_bE"]], w=[Z2r])
                P.mm(Z2[0:64, 256:384], kc_[:, 0:64], BbT[:, h, :], r=[kcr, R[f"BbT{h}"]], w=[Z2r])
                yield
                P.ts("dve", Uv[:, h, :], Z2[:, 128:192], -1.0, None, op0=ALU.mult, r=[Z2r], w=[R[f"Uv{h}"]])
                P.tt("pool", dg[slot][:], ident_f[0:64, 0:64], gC[:, hs], ALU.mult, r=[R["gC"]], w=[R[f"dg{slot}"]])
                P.tt("dve", GT[:, h, :], dg[slot][:], Z2[0:64, 192:256], ALU.subtract, r=[R[f"dg{slot}"], Z2r], w=[R[f"GT{h}"]])
                P.tt("dve", RcT[:, h, :], rqT, Z2[0:64, 256:384], ALU.subtract, r=[R["xt_rq"], Z2r], w=[R[f"RcT{h}"]])
                P.mm(Z2[0:64, 384:448], prod["bE"][:, hs], Uv[:, h, :], start=True, stop=False,
                     r=[R["p_bE"], R[f"Uv{h}"]], w=[Z2r])
                P.mm(Z2[0:64, 384:448], prod["kE"][:, hs], prod["v"][:, hs], start=False, stop=True,
                     r=[R["p_kE"], R["p_v"]], w=[Z2r])
                yield
                P.cp("act", H_a[:, h, :], Z2[0:64, 384:448], r=[Z2r], w=[R[f"H{h}"]])

            pending = list(range(16))
            active = []
            _hc = {}
            free_slots = list(range(G))
            while pending or active:
                while pending and free_slots:
                    s_ = free_slots.pop(0)
                    active.append((s_, head(s_, pending.pop(0))))
                nxt = []
                for (s_, g_) in active:
                    try:
                        next(g_)
                        _hc[s_] = _hc.get(s_, 0) + 1
                        if _hc[s_] >= int(_os.environ.get("RW_HSTOP", "999")):
                            _hc[s_] = 0
                            g_.close()
                            raise StopIteration
                        nxt.append((s_, g_))
                    except StopIteration:
                        _hc[s_] = 0
                        free_slots.append(s_)
                active = nxt
            if _stop == "B":
                continue
            for h in range(16):
                hs = slice(h * 64, (h + 1) * 64)
                P.mm(PW[:, hs], RcT[:, h, :], st_b[:, h, :], start=True, stop=False,
                     r=[R[f"RcT{h}"], R["st_b"]], w=[R["PW"]])
                P.mm(PW[:, hs], BbT[:, h, :], Uv[:, h, :], start=False, stop=False,
                     r=[R[f"BbT{h}"], R[f"Uv{h}"]], w=[R["PW"]])
                P.mm(PW[:, hs], BkT[:, h, :], prod["v"][:, hs], start=False, stop=True,
                     r=[R[f"BkT{h}"], R["p_v"]], w=[R["PW"]])
            P.cp("act", ysb[:], PW[:], r=[R["PW"]], w=[R["ysb"]])
            for h in range(16):
                hs = slice(h * 64, (h + 1) * 64)
                P.mm(PW[0:64, hs], GT[:, h, :], st_f[:, h, :], r=[R[f"GT{h}"], R["st_f"]], w=[R["PW"]])
            P.tt("dve", st_f[:].rearrange("p h j -> p (h j)"), PW[0:64, :], H_a[:].rearrange("p h j -> p (h j)"), ALU.add,
                 r=[R["PW"]] + [R[f"H{h}"] for h in range(16)], w=[R["st_f"]])
            P.cp("act", st_b[:], st_f[:], r=[R["st_f"]], w=[R["st_b"]])
            P.op("dve", lambda e: e.tensor_reduce(out=small[:, 2, :], in_=hv(ysb[:]), axis=AX.X, op=ALU.add),
                 r=[R["ysb"]], w=[R["sm2"]])
            P.tt("pool", tmp[:], ysb[:], ysb[:], ALU.mult, r=[R["ysb"], R["tmp"]], w=[R["tmp"]])
            P.op("dve", lambda e: e.tensor_reduce(out=small[:, 3, :], in_=hv(tmp[:]), axis=AX.X, op=ALU.add),
                 r=[R["tmp"]], w=[R["sm3"]])
            P.ts("dve", small[:, 4, :], small[:, 2, :], 1.0 / 64, None, op0=ALU.mult, r=[R["sm2"]], w=[R["sm4"]])
            P.tt("dve", small[:, 5, :], small[:, 4, :], small[:, 4, :], ALU.mult, r=[R["sm4"]], w=[R["sm5"]])
            P.stt(small[:, 6, :], small[:, 3, :], 1.0 / 64, small[:, 5, :], ALU.mult, ALU.subtract,
                  r=[R["sm3"], R["sm5"]], w=[R["sm6"]])
            P.act(small[:, 6, :], small[:, 6, :], AF.Ln, bias=epsg[:], r=[R["sm6"], R["epsg"]], w=[R["sm6"]])
            P.act(small[:, 6, :], small[:, 6, :], AF.Exp, scale=-0.5, r=[R["sm6"]], w=[R["sm6"]])
            for h in range(16):
                hs = slice(h * 64, (h + 1) * 64)
                P.ts("dve" if h % 2 else "pool", ysb[:, hs], ysb[:, hs], small[:, 4, h:h + 1], small[:, 6, h:h + 1],
                     op0=ALU.subtract, op1=ALU.mult, r=[R["ysb"], R["sm4"], R["sm6"]], w=[R["ysb"]])
            P.tt("pool", ysb[:], ysb[:], rLW, ALU.mult, r=[R["ysb"], R["rwv"]], w=[R["ysb"]])
            P.tt("dve", ysb[:], ysb[:], rLB, ALU.add, r=[R["ysb"], R["rwv"]], w=[R["ysb"]])
            for h in range(16):
                hs = slice(h * 64, (h + 1) * 64)
                P.stt(ysb[:, hs], v_[:, hs], small[:, 1, h:h + 1], ysb[:, hs], ALU.mult, ALU.add,
                      r=[R["T1"], R["sm1"], R["ysb"]], w=[R["ysb"]])
            P.tt("pool", ysb[:], ysb[:], gate[:], ALU.mult, r=[R["ysb"], R["gate"]], w=[R["ysb"]])
            for k in range(8):
                P.tr(PW[:, k * 128:(k + 1) * 128], ysb[:, k * 128:(k + 1) * 128], ident_f, r=[R["ysb"]], w=[R["PW"]])
            P.cp("act", ycs[:].rearrange("p k t -> p (k t)"), PW[:], r=[R["PW"]], w=[R["ycs"]])
            P.dma(ycv[:, :, t0:t0 + 128], ycs[:], r=[R["ycs"]], w=[P.region()])
    P.end_phase()
def phase_merge(C, l, cst, Wl, x_in, per, scr):
    P, nc, S = C.P, C.nc, C.S
    TT = 256
    with contextlib.ExitStack() as st:
        C.stack = st
        R = C.regs("mg_")
        wts = {n: C.sb("mg_" + n, [128, 8, 1024], BF16) for n in ("p_lru", "p_sb", "p_rwkv", "w_out")}
        wr = C.sb("mg_wr", [128, 8, 32])
        brt = C.sb("mg_br", [128, 32])
        pt = per["t"]
        with contextlib.ExitStack() as st2:
            C.stack = st2
            wst = C.sb("mg_wst", [128, 8, 1024])
            for i, n in enumerate(("p_lru", "p_sb", "p_rwkv", "w_out")):
                P.dma(wst[:], Wl[n].rearrange("(k p) j -> p k j", p=128), w=[R["wst"]])
                P.cp("pool" if i % 2 else "dve", wts[n][:], wst[:], r=[R["wst"]], w=[R["w_" + n]])
            P.end_phase()
        C.stack = st
        P.dma(wr[:], Wl["w_router"].rearrange("(k p) j -> p k j", p=128), w=[R["wr"]])
        P.dma(brt[:], Wl["b_router"][:, :], w=[R["br"]])
        yt = {n: C.sb("mg_y" + n, [128, 8, TT], BF16) for n in ("a", "b", "c")}
        gt = [C.sb(f"mg_g{i}", [128, 3, TT]) for i in range(2)]
        mrg = C.sb("mg_mrg", [128, 8, TT], BF16)
        m1 = C.sb("mg_m1", [128, TT])
        m2 = C.sb("mg_m2", [128, TT])
        xt = C.sb("mg_xt", [128, 8, TT])
        x1 = C.sb("mg_x1", [128, 8, TT])
        h2f = C.sb("mg_h2f", [128, 8, TT])
        h2b = C.sb("mg_h2b", [128, 8, TT], BF16)
        sq = C.sb("mg_sq", [128, 8, TT])
        tmp = C.sb("mg_tmp", [128, 8, TT])
        rs = C.sb("mg_rs", [128, TT])
        lg = C.sb("mg_lg", [128, 32])
        m8 = C.sb("mg_m8", [128, 8])
        nmx = C.sb("mg_nmx", [128, 1])
        msk = C.sb("mg_msk", [128, 32])
        ex = C.sb("mg_ex", [128, 32])
        ssum = C.sb("mg_ssum", [128, 1])
        rwo = [C.sb(f"mg_rwo{i}", [128, 32]) for i in range(2)]
        psb = [C.ps(f"mg_ps{i}", [128, 512]) for i in range(3)]
        psm = C.ps("mg_psm", [128, 512])
        ps_s = C.ps("mg_pss", [128, 512])
        psl = C.ps("mg_psl", [128, 512])
        for k_ in ("ps0", "ps1", "ps2", "psm", "ps_s", "psl"):
            R[k_].excl = True
        xv = x_in.rearrange("(k p) s -> p k s", p=128)
        yv = {n: scr["y%sT" % n].rearrange("(k p) s -> p k s", p=128) for n in ("a", "b", "c")}
        gv = scr["gatesT"].rearrange("(g k p) s -> p g k s", p=128, k=8)
        x1v = scr["x1T"].rearrange("(k p) s -> p k s", p=128)
        h2v = scr["h2T"].rearrange("(k p) s -> p k s", p=128)
        pw = (("a", "p_lru"), ("b", "p_sb"), ("c", "p_rwkv"))
        gi = 0
        for ti in range(S // TT):
            ts_ = slice(ti * TT, (ti + 1) * TT)
            for n in ("a", "b", "c"):
                P.dma(yt[n][:], yv[n][:, :, ts_], w=[R["y" + n]])
            P.dma(xt[:], xv[:, :, ts_], w=[R["xt"]])
            for oc in range(8):
                g_ = gt[gi % 2]
                gr = R[f"g{gi % 2}"]
                gi += 1
                P.dma(g_[:], gv[:, :, oc, ts_], w=[gr])
                for bi, (yn, wn) in enumerate(pw):
                    for k in range(8):
                        P.mm(psb[bi][:, :TT], wts[wn][:, k, oc * 128:(oc + 1) * 128], yt[yn][:, k, :],
                             start=(k == 0), stop=(k == 7), r=[R["w_" + wn], R["y" + yn]], w=[R[f"ps{bi}"]])
                P.tt("dve", m1[:], psb[0][:, :TT], g_[:, 0, :], ALU.mult, r=[R["ps0"], gr], w=[R["m1"]])
                P.tt("dve", m2[:], psb[1][:, :TT], g_[:, 1, :], ALU.mult, r=[R["ps1"], gr], w=[R["m2"]])
                P.tt("pool", m1[:], m1[:], m2[:], ALU.add, r=[R["m1"], R["m2"]], w=[R["m1"]])
                P.tt("dve", m2[:], psb[2][:, :TT], g_[:, 2, :], ALU.mult, r=[R["ps2"], gr, R["m2"]], w=[R["m2"]])
                P.tt("pool", mrg[:, oc, :], m1[:], m2[:], ALU.add, r=[R["m1"], R["m2"]], w=[R["mrg"]])
            for oc in range(8):
                for k in range(8):
                    P.mm(psm[:, :TT], wts["w_out"][:, k, oc * 128:(oc + 1) * 128], mrg[:, k, :],
                         start=(k == 0), stop=(k == 7), r=[R["w_w_out"], R["mrg"]], w=[R["psm"]])
                P.stt(x1[:, oc, :], psm[:, :TT], pt[:, 2, oc:oc + 1], xt[:, oc, :], ALU.mult, ALU.add,
                      r=[R["psm"], R["xt"]], w=[R["xt1"]])
            P.dma(x1v[:, :, ts_], x1[:], r=[R["xt1"]], w=[P.region()])
            emit_norm_tile(C, R, x1[:], lambda k: h2f[:, k, :], pt[:, 3, :], pt[:, 4, :], cst["ones"], sq, rs, tmp,
                           ps_s, TT, cst["eps"], "1")
            P.cp("pool", h2b[:], h2f[:], r=[R["hT"]], w=[R["h2b"]])
            P.dma(h2v[:, :, ts_], h2b[:], r=[R["h2b"]], w=[P.region()])
            for sub in range(TT // 128):
                for k in range(8):
                    P.mm(psl[:, 0:32], h2f[:, k, sub * 128:(sub + 1) * 128], wr[:, k, :], start=(k == 0), stop=(k == 7),
                         r=[R["hT"], R["wr"]], w=[R["psl"]])
                P.tt("dve", lg[:], psl[:, 0:32], brt[:], ALU.add, r=[R["psl"], R["br"]], w=[R["lg"]])
                P.op("dve", lambda e: e.max(out=m8[:], in_=lg[:]), r=[R["lg"]], w=[R["m8"]])
                P.ts("dve", nmx[:], m8[:, 0:1], -1.0, None, op0=ALU.mult, r=[R["m8"]], w=[R["nmx"]])
                P.ts("dve", msk[:], lg[:], m8[:, 3:4], None, op0=ALU.is_ge, r=[R["lg"], R["m8"]], w=[R["msk"]])
                P.act(ex[:], lg[:], AF.Exp, bias=nmx[:], r=[R["lg"], R["nmx"]], w=[R["ex"]])
                P.tt("dve", ex[:], ex[:], msk[:], ALU.mult, r=[R["ex"], R["msk"]], w=[R["ex"]])
                P.op("dve", lambda e: e.tensor_reduce(out=ssum[:], in_=ex[:], axis=AX.X, op=ALU.add),
                     r=[R["ex"]], w=[R["ssum"]])
                P.op("dve", lambda e: e.reciprocal(out=ssum[:], in_=ssum[:]), r=[R["ssum"]], w=[R["ssum"]])
                ro = rwo[sub % 2]
                P.ts("dve", ro[:], ex[:], ssum[:, 0:1], None, op0=ALU.mult, r=[R["ex"], R["ssum"]], w=[R[f"rwo{sub % 2}"]])
                t0 = ti * TT + sub * 128
                P.dma(scr["rwt"][t0:t0 + 128, :], ro[:], r=[R[f"rwo{sub % 2}"]], w=[P.region()])
    P.end_phase()


def phase_moe(C, l, cst, Wl, per, scr, nfin_d, out_d):
    P, nc, S = C.P, C.nc, C.S
    import os
    NE = int(os.environ.get("MOE_NE", "32"))
    ST = min(int(os.environ.get("MOE_ST", "512")), S)
    NTB = ST // 128
    with contextlib.ExitStack() as st:
        C.stack = st
        R = C.regs("moe_")
        pt = per["t"]
        gub = scr["gub"]
        dnb = scr["dnb"]
        for e in range(NE):
            P.dma(gub[e], Wl["w_gu"][e], w=[P.region()], q="pool")
            P.dma(dnb[e], Wl["w_down"][e], w=[P.region()], q="pool")
        P.end_phase()
        C.stack = st
        h2 = C.sb("moe_h2", [128, 8, ST], BF16)
        yacc = C.sb("moe_yacc", [128, NTB, 1024])
        wgu = [C.sb(f"moe_wgu{i}", [128, 8, 2048], BF16) for i in range(2)]
        wdn = [C.sb(f"moe_wdn{i}", [128, 8, 1024], BF16) for i in range(2)]
        actT = [C.sb(f"moe_act{i}", [128, 8, 512], BF16) for i in range(2)]
        bgu = C.sb("moe_bgu", [128, 32, 16])
        bdn = C.sb("moe_bdn", [32, 1024])
        rwt = C.sb("moe_rwt", [128, NTB, 32])
        rwT = C.sb("moe_rwT", [32, 128])
        g_t = [C.sb(f"moe_g{i}", [128, 512]) for i in range(2)]
        u_t = [C.sb(f"moe_u{i}", [128, 512]) for i in range(2)]
        s_t = [C.sb(f"moe_s{i}", [128, 512]) for i in range(2)]
        x1t = C.sb("moe_x1", [128, 8, 128])
        x2t = C.sb("moe_x2", [128, 8, 128])
        nfin = C.sb("moe_nfin", [128, 8])
        sq = C.sb("moe_sq", [128, 8, 128])
        rs = C.sb("moe_rs", [128, 128])
        o_t = sq
        psg = [C.ps(f"moe_psg{i}", [128, 512]) for i in range(2)]
        psu = [C.ps(f"moe_psu{i}", [128, 512]) for i in range(2)]
        psd = [C.ps(f"moe_psd{i}", [128, 512]) for i in range(2)]
        psx = C.ps("moe_psx", [128, 1024])
        for k_ in ("psg0", "psg1", "psu0", "psu1", "psd0", "psd1", "psx"):
            R[k_].excl = True
        P.dma(bgu[:], Wl["b_gu"][:, :, :], w=[R["bgu"]])
        P.dma(bdn[:], Wl["b_down"][:, :], w=[R["bdn"]])
        if nfin_d is not None:
            P.dma(nfin[:], nfin_d[:, :], w=[R["nfin"]])
        h2v = scr["h2T"].rearrange("(k p) s -> p k s", p=128)
        x1v = scr["x1T"].rearrange("(k p) s -> p k s", p=128)
        ov = out_d.rearrange("(k p) s -> p k s", p=128)
        rwv_ = scr["rwt"].rearrange("(n p) e -> p n e", p=128)
        wi = 0
        ci = 0
        for si in range(S // ST):
            s0 = si * ST
            P.dma(h2[:], h2v[:, :, s0:s0 + ST], w=[R["h2"]])
            P.dma(rwt[:], rwv_[:, si * NTB:(si + 1) * NTB, :], w=[R["rwt"]])
            for tb in range(NTB):
                P.tr(psx[0:32, 0:128], rwt[:, tb, :], cst["ident"], r=[R["rwt"]], w=[R["psx"]])
                P.cp("dve", rwT[:], psx[0:32, 0:128], r=[R["psx"]], w=[R["rwT"]])
                for dh in range(2):
                    P.mm(psd[dh][:], rwT[:], bdn[:, dh * 512:(dh + 1) * 512], r=[R["rwT"], R["bdn"]], w=[R[f"psd{dh}"]])
                    P.cp("act", yacc[:, tb, dh * 512:(dh + 1) * 512], psd[dh][:], r=[R[f"psd{dh}"]], w=[R[f"yacc{tb}"]])
            for e in range(NE):
                wg, wd = wgu[wi % 2], wdn[wi % 2]
                wgr, wdr = R[f"wgu{wi % 2}"], R[f"wdn{wi % 2}"]
                wi += 1
                P.dma(wg[:], gub[e].rearrange("(k p) j -> p k j", p=128), w=[wgr])
                P.dma(wd[:], dnb[e].rearrange("(k p) j -> p k j", p=128), w=[wdr])
                for tt_ in range(ST // 512):
                    tsl = slice(tt_ * 512, (tt_ + 1) * 512)
                    at, atr = actT[ci % 2], R[f"act{ci % 2}"]
                    ci += 1
                    def fin(fc_):
                        j2 = fc_ % 2
                        P.stt(at[:, fc_, :], s_t[j2][:], 1.0 / 1.702, u_t[j2][:], ALU.mult, ALU.mult,
                              r=[R[f"s{j2}"], R[f"u{j2}"]], w=[atr])
                    for fc in range(8):
                        i2 = fc % 2
                        for k in range(8):
                            P.mm(psg[i2][:], wg[:, k, fc * 128:(fc + 1) * 128], h2[:, k, tsl], start=(k == 0), stop=(k == 7),
                                 r=[wgr, R["h2"]], w=[R[f"psg{i2}"]])
                        for k in range(8):
                            P.mm(psu[i2][:], wg[:, k, 1024 + fc * 128:1024 + (fc + 1) * 128], h2[:, k, tsl],
                                 start=(k == 0), stop=(k == 7), r=[wgr, R["h2"]], w=[R[f"psu{i2}"]])
                        P.ts("dve", g_t[i2][:], psg[i2][:], bgu[:, e, fc:fc + 1], 7.0, op0=ALU.add, op1=ALU.min,
                             r=[R[f"psg{i2}"], R["bgu"]], w=[R[f"g{i2}"]])
                        P.act(s_t[i2][:], g_t[i2][:], AF.Silu, scale=1.702, r=[R[f"g{i2}"]], w=[R[f"s{i2}"]])
                        P.ts("dve", u_t[i2][:], psu[i2][:], bgu[:, e, 8 + fc:9 + fc], 7.0, op0=ALU.add, op1=ALU.min,
                             r=[R[f"psu{i2}"], R["bgu"]], w=[R[f"u{i2}"]])
                        P.ts("dve", u_t[i2][:], u_t[i2][:], -7.0, 1.0, op0=ALU.max, op1=ALU.add, r=[R[f"u{i2}"]], w=[R[f"u{i2}"]])
                        if fc >= 1:
                            fin(fc - 1)
                    fin(7)
                    for sub in range(4):
                        tb = tt_ * 4 + sub
                        for dh in range(2):
                            for fc in range(8):
                                P.mm(psd[dh][:], at[:, fc, sub * 128:(sub + 1) * 128], wd[:, fc, dh * 512:(dh + 1) * 512],
                                     start=(fc == 0), stop=(fc == 7), r=[atr, wdr], w=[R[f"psd{dh}"]])
                            P.stt(yacc[:, tb, dh * 512:(dh + 1) * 512], psd[dh][:], rwt[:, tb, e:e + 1],
                                  yacc[:, tb, dh * 512:(dh + 1) * 512], ALU.mult, ALU.add,
                                  r=[R[f"psd{dh}"], R["rwt"], R[f"yacc{tb}"]], w=[R[f"yacc{tb}"]])
            for tb in range(NTB):
                t0 = s0 + tb * 128
                P.dma(x1t[:], x1v[:, :, t0:t0 + 128], w=[R["x1t"]])
                for k in range(8):
                    P.tr(psx[:, k * 128:(k + 1) * 128], yacc[:, tb, k * 128:(k + 1) * 128], cst["ident"],
                         r=[R[f"yacc{tb}"]], w=[R["psx"]])
                for k in range(8):
                    P.stt(x2t[:, k, :], psx[:, k * 128:(k + 1) * 128], pt[:, 5, k:k + 1], x1t[:, k, :], ALU.mult, ALU.add,
                          r=[R["psx"], R["x1t"]], w=[R["x2t"]])
                if nfin_d is None:
                    P.dma(ov[:, :, t0:t0 + 128], x2t[:], r=[R["x2t"]], w=[P.region()])
                else:
                    P.act(sq[:], x2t[:], AF.Square, r=[R["x2t"]], w=[R["sq"]])
                    for k in range(8):
                        P.mm(psd[0][:, :128], cst["ones"], sq[:, k, :], start=(k == 0), stop=(k == 7), r=[R["sq"]], w=[R["psd0"]])
                    P.act(rs[:], psd[0][:, :128], AF.Ln, scale=1.0 / 1024, bias=cst["eps"], r=[R["psd0"]], w=[R["rs"]])
                    P.act(rs[:], rs[:], AF.Exp, scale=-0.5, r=[R["rs"]], w=[R["rs"]])
                    for k in range(8):
                        P.stt(o_t[:, k, :], x2t[:, k, :], nfin[:, k:k + 1], rs[:], ALU.mult, ALU.mult,
                              r=[R["x2t"], R["rs"], R["nfin"], R["sq"]], w=[R["sq"]])
                    P.dma(ov[:, :, t0:t0 + 128], o_t[:], r=[R["sq"]], w=[P.region()], is_out=True)
    P.end_phase()
from concourse.bass_utils import run_bass_kernel_spmd

LAYER_W = ["w_ada", "b_ada", "norm_mix", "norm_moe", "w_in", "conv_w", "conv_b", "lru_wa", "lru_ba", "lru_wx",
           "lru_bx", "lru_lambda", "rw_mu", "rw_w0", "rw_w_up", "rw_a0", "rw_a_up", "rw_g_up", "rw_k_k", "rw_k_a",
           "rw_r_k", "rw_lnx_w", "rw_lnx_b", "p_lru", "p_sb", "p_rwkv", "w_out", "w_router", "b_router",
           "w_gu", "b_gu", "w_down", "b_down"]


def fm(v):
    v = np.asarray(v, np.float32).reshape(-1, 128)
    return np.ascontiguousarray(v.T)


def bc(v):
    v = np.asarray(v, np.float32).reshape(1, -1)
    return np.ascontiguousarray(np.broadcast_to(v, (128, v.shape[1])))


def layer_inputs(inp, l):
    g = lambda n: np.asarray(inp[n][l], np.float32)
    d = {}
    d["w_ada"] = g("w_ada")
    d["b_ada"] = fm(g("b_ada"))
    d["norm_mix"] = fm(g("norm_mix"))
    d["norm_moe"] = fm(g("norm_moe"))
    d["w_in"] = g("w_in")
    d["mu"] = bc(g("rw_mu"))
    cw = g("conv_w")
    vecs = [cw[0], cw[1], cw[2], cw[3], g("conv_b"), g("lru_ba"), g("lru_bx"), g("lru_lambda")]
    d["lruv"] = np.ascontiguousarray(np.stack([fm(v) for v in vecs], axis=1))
    for nm in ("lru_wa", "lru_wx"):
        w = g(nm)
        bd = np.zeros((128, 8, 128), np.float32)
        for c in range(8):
            bd[0:64, c, 0:64] = w[2 * c]
            bd[64:128, c, 64:128] = w[2 * c + 1]
        d[nm] = bd
    d["rwv"] = np.ascontiguousarray(np.stack(
        [bc(g(n).reshape(-1)) for n in ("rw_w0", "rw_a0", "rw_k_k", "rw_k_a", "rw_r_k", "rw_lnx_w", "rw_lnx_b")], axis=1))
    d["rw_w_up"] = g("rw_w_up")
    d["rw_a_up"] = g("rw_a_up")
    d["rw_g_up"] = g("rw_g_up")
    for nm in ("p_lru", "p_sb", "p_rwkv", "w_out", "w_router", "w_gu", "w_down", "b_down"):
        d[nm] = g(nm)
    d["b_router"] = bc(g("b_router"))
    bg = g("b_gu")
    d["b_gu"] = np.ascontiguousarray(bg.reshape(32, 16, 128).transpose(2, 0, 1))
    return d


LAYER_SHAPES = {
    "w_ada": (1024, 6144), "b_ada": (128, 48), "norm_mix": (128, 8), "norm_moe": (128, 8), "w_in": (1024, 11520),
    "mu": (128, 3328), "lruv": (128, 8, 8), "lru_wa": (128, 8, 128), "lru_wx": (128, 8, 128),
    "rwv": (128, 7, 1024), "rw_w_up": (64, 1024), "rw_a_up": (64, 1024), "rw_g_up": (128, 1024),
    "p_lru": (1024, 1024), "p_sb": (1024, 1024), "p_rwkv": (1024, 1024), "w_out": (1024, 1024),
    "w_router": (1024, 32), "w_gu": (32, 1024, 2048), "w_down": (32, 1024, 1024), "b_down": (32, 1024),
    "b_router": (128, 32), "b_gu": (128, 32, 16),
}


def build(S, depth, debug=False, phases=None):
    nc = bass.Bass("TRN2", target_bir_lowering=False)
    P = Prog(nc)
    C = Ctx(nc, P, S, debug)
    cst_np, ccols = make_consts()
    NCC = cst_np.shape[1]
    xT_d = C.din("xT", [1024, S])
    cvec_d = C.din("cvec", [128, 8])
    nfin_d = C.din("norm_final", [128, 8])
    cst_d = C.din("consts", [128, NCC])
    class LazyW(dict):
        def __init__(self, l):
            super().__init__()
            self.l = l

        def __missing__(self, n):
            ap = C.din(f"{n}_{self.l}", LAYER_SHAPES[n])
            self[n] = ap
            C.declared.add(f"{n}_{self.l}")
            return ap
    C.declared = set()
    W = [LazyW(l) for l in range(depth)]
    out_d = C.dout("outT", [1024, S])
    scr = {
        "lruT": C.dscr("s_lruT", [2048, S], F32),
        "qT": C.dscr("s_qT", [1024, S], BF16),
        "kT": C.dscr("s_kT", [1024, S], BF16),
        "v": C.dscr("s_v", [S, 1024], BF16),
        "rkv": C.dscr("s_rkv", [S, 3072], F32),
        "loraT": C.dscr("s_loraT", [256, S], BF16),
        "gatesT": C.dscr("s_gatesT", [3072, S], F32),
        "yaT": C.dscr("s_yaT", [1024, S], BF16),
        "ybT": C.dscr("s_ybT", [1024, S], BF16),
        "ycT": C.dscr("s_ycT", [1024, S], BF16),
        "x1T": C.dscr("s_x1T", [1024, S], F32),
        "x2T": C.dscr("s_x2T", [1024, S], F32),
        "h2T": C.dscr("s_h2T", [1024, S], BF16),
        "rwt": C.dscr("s_rwt", [S, 32], F32),
        "gub": nc.dram_tensor("s_gub", [32, 1024, 2048], BF16, kind="Internal").ap(),
        "dnb": nc.dram_tensor("s_dnb", [32, 1024, 1024], BF16, kind="Internal").ap(),
    }
    with contextlib.ExitStack() as top:
        C.stack = top
        so, sw = ccols.pop("__sbm__")
        NCA = so
        cst_t = top.enter_context(nc.sbuf_tensor("cst", [128, NCA], F32))
        cstb_t = top.enter_context(nc.sbuf_tensor("cstb", [128, NCA], BF16))
        sbm_t = top.enter_context(nc.sbuf_tensor("sbm", [128, sw], BF16))
        eps_t = top.enter_context(nc.sbuf_tensor("eps", [128, 1], F32))
        per_t = top.enter_context(nc.sbuf_tensor("per", [128, 6, 8], F32))
        R0 = C.regs("init_")
        with contextlib.ExitStack() as st0:
            tmpc = st0.enter_context(nc.sbuf_tensor("cst_tmp", [128, sw], F32))
            P.dma(cst_t[:], cst_d[:, 0:NCA], w=[R0["cst"]])
            P.cp("dve", cstb_t[:], cst_t[:], r=[R0["cst"]], w=[R0["cstb"]])
            P.dma(tmpc[:], cst_d[:, so:so + sw], w=[R0["tmpc"]])
            P.cp("dve", sbm_t[:], tmpc[:], r=[R0["tmpc"]], w=[R0["sbm"]])
            P.memset("pool", eps_t[:], EPS, w=[R0["eps"]])
            P.end_phase()
        cst = {"eps": eps_t[:], "sbmask_b": sbm_t[:]}
        for n, (o, wd) in ccols.items():
            cst[n] = cst_t[:, o:o + wd]
            cst[n + "_b"] = cstb_t[:, o:o + wd]
        per = {"t": per_t, "reg": P.region("per", persistent=True)}
        x_in = xT_d
        for l in range(depth):
            Wl = W[l]
            if phases is None or "ada" in phases:
                phase_adaln(C, l, cst, cvec_d, Wl["w_ada"], Wl["b_ada"], Wl["norm_mix"], Wl["norm_moe"], per)
            if phases is None or "proj" in phases:
                phase_proj(C, l, cst, x_in, Wl["w_in"], Wl["mu"], per, scr)
            if phases is None or "lru" in phases:
                phase_lru(C, l, cst, Wl, scr)
            if phases is None or "sb" in phases:
                phase_sb(C, l, cst, scr)
            if phases is None or "rwkv" in phases:
                phase_rwkv(C, l, cst, Wl, scr)
            if phases is None or "merge" in phases:
                phase_merge(C, l, cst, Wl, x_in, per, scr)
            if phases is None or "moe" in phases:
                last = (l == depth - 1)
                phase_moe(C, l, cst, Wl, per, scr, nfin_d if last else None, out_d if last else scr["x2T"])
            x_in = scr["x2T"]
        if phases is not None and "moe" not in phases:
            with contextlib.ExitStack() as st:
                C.stack = st
                z = C.sb("zz", [128, 8])
                rz = P.region()
                P.memset("pool", z[:], 0.0, w=[rz])
                P.dma(out_d[0:128, 0:8], z[:], r=[rz], w=[P.region()], is_out=True)
        P.emit()
    nc.declared_inputs = set(C.declared)
    return nc, cst_np


def core_inputs(inp, b, S, depth, cst_np, layer_cache, declared=None):
    m = {"xT": np.ascontiguousarray(np.asarray(inp["x"][b, :S], np.float32).T),
         "cvec": fm(np.asarray(inp["c"][b], np.float32)),
         "norm_final": fm(np.asarray(inp["norm_final"], np.float32)),
         "consts": cst_np}
    for l in range(depth):
        for n, a in layer_cache[l].items():
            if declared is None or f"{n}_{l}" in declared:
                m[f"{n}_{l}"] = a
    return m


N_ACTIVE = 4


def kernel(**inputs):
    S, depth, B = 8192, 2, 4
    nc, cst_np = build(S, depth)
    lc = [layer_inputs(inputs, l) for l in range(depth)]
    in_maps = [core_inputs(inputs, b, S, depth, cst_np, lc) for b in range(B)]
    res = run_bass_kernel_spmd(nc, in_maps, core_ids=list(range(B)))
    out = np.stack([np.asarray(res.results[b]["outT"], np.float32).T for b in range(B)], axis=0)
    return np.ascontiguousarray(out)
prog_extend(Prog)
```

```python
import concourse.bass as bass
import concourse.mybir as mybir

F32 = mybir.dt.float32
BF16 = mybir.dt.bfloat16
ALU = mybir.AluOpType
AF = mybir.ActivationFunctionType
AX = mybir.AxisListType


class Region:
    __slots__ = ("w", "rs", "name", "excl")

    def __init__(self, name=""):
        self.w = None
        self.rs = {}
        self.name = name
        self.excl = False


class Ins:
    __slots__ = ("eng", "fn", "deps", "sig", "val", "dma", "dsem", "dval", "dprev")

    def __init__(self, eng, fn, dma=False):
        self.eng = eng
        self.fn = fn
        self.deps = []
        self.sig = False
        self.val = 0
        self.dma = dma
        self.dsem = None
        self.dval = 0
        self.dprev = 0


class Prog:
    ENGS = ("pe", "act", "dve", "pool", "sp")
    NDMA = 10

    def __init__(self, nc):
        self.nc = nc
        self.q = {e: [] for e in self.ENGS}
        self.dcount = {"sp": 0, "pool": 0, "act": 0}
        self.out_dmas = []

    def op(self, eng, fn, r=(), w=(), dma=False):
        ins = Ins(eng, fn, dma)
        deps = {}

        def add(d):
            if d is None or d is ins:
                return
            if (not d.dma) and (not dma) and d.eng == "pe" and eng == "pe":
                return
            deps[id(d)] = d

        for reg in r:
            add(reg.w)
            if reg.excl:
                for k, v in reg.rs.items():
                    if k != eng and k != "dma":
                        add(v)
        for reg in w:
            add(reg.w)
            for k, v in reg.rs.items():
                if k == "dma":
                    for d in v:
                        add(d)
                else:
                    add(v)
        ins.deps = list(deps.values())
        for d in ins.deps:
            d.sig = True
        for reg in r:
            if dma:
                reg.rs.setdefault("dma", []).append(ins)
            else:
                reg.rs[eng] = ins
        for reg in w:
            reg.w = ins
            reg.rs = {}
        if dma:
            i = self.dcount[eng]
            self.dcount[eng] = i + 1
            ins.dsem = (eng, i % self.NDMA)
            ins.dval = 16 * (i // self.NDMA + 1)
            ins.dprev = 16 * (i // self.NDMA)
        self.q[eng].append(ins)
        return ins

    def mm(self, out, lhsT, rhs, start=True, stop=True, r=(), w=(), **kw):
        return self.op("pe", lambda e: e.matmul(out, lhsT, rhs, start=start, stop=stop, **kw), r, w)

    def tr(self, out, in_, ident, r=(), w=()):
        return self.op("pe", lambda e: e.transpose(out, in_, ident), r, w)

    def act(self, out, in_, func, r=(), w=(), **kw):
        return self.op("act", lambda e: e.activation(out, in_, func, **kw), r, w)

    def dma(self, out, in_, r=(), w=(), q="sp", is_out=False, **kw):
        ins = self.op(q, lambda e: e.dma_start(out=out, in_=in_, **kw), r, w, dma=True)
        if is_out:
            self.out_dmas.append(ins)
        return ins

    def emit(self):
        nc = self.nc
        for e in self.ENGS:
            c = 0
            for ins in self.q[e]:
                if ins.sig and not ins.dma:
                    c += 1
                    ins.val = c
        import contextlib
        with contextlib.ExitStack() as st:
            esem = {e: st.enter_context(nc.semaphore("es_" + e)) for e in self.ENGS}
            dsem = {}
            for qn in ("sp", "pool"):
                for i in range(self.NDMA):
                    dsem[(qn, i)] = st.enter_context(nc.semaphore(f"ds_{qn}{i}"))
            block = st.enter_context(nc.Block())
            final = self.out_dmas

            def body(ename, eng):
                waited = {}

                def wait(key, sem, val):
                    if val <= 0:
                        return
                    if waited.get(key, 0) >= val:
                        return
                    eng.wait_ge(sem, val)
                    waited[key] = val

                for ins in self.q[ename]:
                    need = {}
                    for d in ins.deps:
                        if d.dma:
                            key, sem, val = d.dsem, dsem[d.dsem], d.dval
                        else:
                            key, sem, val = d.eng, esem[d.eng], d.val
                        if key not in need or need[key][1] < val:
                            need[key] = (sem, val)
                    for key, (sem, val) in need.items():
                        wait(key, sem, val)
                    if ins.dma:
                        wait(ins.dsem, dsem[ins.dsem], ins.dprev)
                    i = ins.fn(eng)
                    if ins.dma:
                        i.then_inc(dsem[ins.dsem], 16)
                    elif ins.sig:
                        i.then_inc(esem[ename], 1)
                if ename == "sp":
                    for d in final:
                        wait(d.dsem, dsem[d.dsem], d.dval)

            @block.tensor
            def _(e):
                body("pe", e)

            @block.scalar
            def _(e):
                body("act", e)

            @block.vector
            def _(e):
                body("dve", e)

            @block.gpsimd
            def _(e):
                body("pool", e)

            @block.sync
            def _(e):
                body("sp", e)
import contextlib
import numpy as np

D = 1024
KC = 8
N_IN = 11520
EPS = 1e-6


class RegMap(dict):
    def __init__(self, P, name, persistent=False):
        super().__init__()
        self.P = P
        self.name = name
        self.persistent = persistent

    def __missing__(self, k):
        r = self.P.region(f"{self.name}{k}", self.persistent)
        self[k] = r
        return r


class Ctx:
    def __init__(self, nc, P, S, debug):
        self.nc = nc
        self.P = P
        self.S = S
        self.debug = debug
        self.dram_regs = {}
        self.stack = None

    def din(self, name, shape, dt=F32):
        return self.nc.dram_tensor(name, list(shape), dt, kind="ExternalInput").ap()

    def dout(self, name, shape, dt=F32):
        return self.nc.dram_tensor(name, list(shape), dt, kind="ExternalOutput").ap()

    def dscr(self, name, shape, dt):
        kind = "ExternalOutput" if self.debug else "Internal"
        return self.nc.dram_tensor(name, list(shape), dt, kind=kind).ap()

    def sb(self, name, shape, dt=F32):
        self.uid = getattr(self, "uid", 0) + 1
        return self.stack.enter_context(self.nc.sbuf_tensor(f"{name}_{self.uid}", list(shape), dt))

    def ps(self, name, shape, dt=F32):
        self.uid = getattr(self, "uid", 0) + 1
        return self.stack.enter_context(self.nc.psum_tensor(f"{name}_{self.uid}", list(shape), dt))

    def regs(self, name, persistent=False):
        return RegMap(self.P, name, persistent)


def prog_extend(Prog):
    def region(self, name="", persistent=False):
        r = Region(name)
        if not persistent:
            r.w = self.cur_bar
            self.phase_regions.append(r)
        return r

    def end_phase(self):
        bar = self.op("sp", lambda e: e.nop(), w=list(self.phase_regions))
        self.cur_bar = bar
        self.phase_regions = []

    def tt(self, eng, out, in0, in1, op, r=(), w=()):
        return self.op(eng, lambda e: e.tensor_tensor(out=out, in0=in0, in1=in1, op=op), r, w)

    def ts(self, eng, out, in0, s1, s2=None, op0=ALU.mult, op1=None, r=(), w=()):
        if op1 is None:
            return self.op(eng, lambda e: e.tensor_scalar(out=out, in0=in0, scalar1=s1, scalar2=None, op0=op0), r, w)
        return self.op(eng, lambda e: e.tensor_scalar(out=out, in0=in0, scalar1=s1, scalar2=s2, op0=op0, op1=op1), r, w)

    def stt(self, out, in0, scalar, in1, op0, op1, r=(), w=()):
        return self.op("dve", lambda e: e.scalar_tensor_tensor(out=out, in0=in0, scalar=scalar, in1=in1, op0=op0, op1=op1), r, w)

    def cp(self, eng, out, in_, r=(), w=()):
        if eng == "act":
            return self.op(eng, lambda e: e.activation(out, in_, AF.Copy), r, w)
        return self.op(eng, lambda e: e.tensor_copy(out=out, in_=in_), r, w)

    def memset(self, eng, ap, val, r=(), w=()):
        return self.op(eng, lambda e: e.memset(ap, val), r, w)

    Prog.region = region
    Prog.end_phase = end_phase
    Prog.tt = tt
    Prog.ts = ts
    Prog.stt = stt
    Prog.cp = cp
    Prog.memset = memset
    Prog.cur_bar = None
    Prog.phase_regions = []


def make_consts():
    cols = {}
    parts = []
    off = 0

    def add(name, arr):
        nonlocal off
        arr = np.asarray(arr, np.float32).reshape(128, -1)
        cols[name] = (off, arr.shape[1])
        parts.append(arr)
        off += arr.shape[1]

    i = np.arange(128)
    add("ident", np.eye(128))
    add("ones", np.ones((128, 128)))
    add("tri_ge", (i[:, None] >= i[None, :]))
    add("tri_lt", (i[:, None] < i[None, :]))
    add("tri_le", (i[:, None] <= i[None, :]))
    t = np.arange(512)
    m = np.stack([(128 * k + i[:, None] < t[None, :]) for k in range(4)], axis=1)
    sbm = np.asarray(m, np.float32).reshape(128, -1)
    add("m_sl", (i[None, :] < i[:, None]))
    add("m_li", (i[None, :] <= i[:, None]))
    add("m_su", (i[:, None] < i[None, :]))
    add("m_ui", (i[:, None] <= i[None, :]))
    add("mask3", np.concatenate([(i[None, :] < i[:, None]), (i[:, None] < i[None, :]), (i[:, None] < i[None, :])], axis=1))
    cols["__sbm__"] = (off, sbm.shape[1])
    parts.append(sbm)
    return np.concatenate(parts, axis=1), cols


def phase_adaln(C, l, cst, cvec_d, w_ada_d, b_ada_d, nmix_d, nmoe_d, per):
    P, nc = C.P, C.nc
    with contextlib.ExitStack() as st:
        C.stack = st
        R = C.regs("p0_")
        cact = C.sb("cact", [128, 8])
        wst = [C.sb(f"wada{i}", [128, 8, 768]) for i in range(2)]
        ada = C.sb("ada", [128, 48])
        bada = C.sb("bada", [128, 48])
        nm = C.sb("nm", [128, 16])
        psa = C.ps("psa", [128, 48])
        P.dma(cact[:], cvec_d[:, :], w=[R["cact"]])
        P.dma(bada[:], b_ada_d[:, :], w=[R["bada"]])
        P.dma(nm[:, 0:8], nmix_d[:, :], w=[R["nm"]])
        P.dma(nm[:, 8:16], nmoe_d[:, :], w=[R["nm"]])
        P.act(cact[:], cact[:], AF.Silu, r=[R["cact"]], w=[R["cact"]])
        wv = w_ada_d.rearrange("(k p) j -> p k j", p=128)
        for g in range(8):
            wt = wst[g % 2]
            P.dma(wt[:], wv[:, :, g * 768:(g + 1) * 768], w=[R[f"w{g % 2}"]])
            for cc in range(6):
                j = g * 6 + cc
                for k in range(8):
                    P.mm(psa[:, j:j + 1], wt[:, k, cc * 128:(cc + 1) * 128], cact[:, k:k + 1],
                         start=(k == 0), stop=(k == 7), r=[R[f"w{g % 2}"], R["cact"]], w=[R["psa"]])
        P.tt("dve", ada[:], psa[:], bada[:], ALU.add, r=[R["psa"], R["bada"]], w=[R["ada"]])
        Rp = per["reg"]
        pt = per["t"]
        for (dst, sc_i, nofs) in ((0, 1, 0), (3, 4, 8)):
            P.ts("dve", pt[:, dst, :], ada[:, sc_i * 8:(sc_i + 1) * 8], 1.0, None, op0=ALU.add, r=[R["ada"]], w=[Rp])
            P.tt("dve", pt[:, dst, :], pt[:, dst, :], nm[:, nofs:nofs + 8], ALU.mult, r=[Rp, R["nm"]], w=[Rp])
        for (dst, src) in ((1, 0), (2, 2), (4, 3), (5, 5)):
            P.cp("dve", pt[:, dst, :], ada[:, src * 8:(src + 1) * 8], r=[R["ada"]], w=[Rp])
    P.end_phase()


def emit_norm_tile(C, R, xt, hT_out_fn, scale_ap, shift_ap, ones_f, sq, rs, tmp, ps_s, TT, eps_ap, tag):
    P = C.P
    P.act(sq[:], xt, AF.Square, r=[R["xt" + tag]], w=[R["sq"]])
    for k in range(8):
        P.mm(ps_s[:, :TT], ones_f, sq[:, k, :], start=(k == 0), stop=(k == 7), r=[R["sq"]], w=[R["ps_s"]])
    P.act(rs[:], ps_s[:, :TT], AF.Ln, scale=1.0 / 1024, bias=eps_ap, r=[R["ps_s"]], w=[R["rs"]])
    P.act(rs[:], rs[:], AF.Exp, scale=-0.5, r=[R["rs"]], w=[R["rs"]])
    for k in range(8):
        if shift_ap is None:
            P.stt(hT_out_fn(k), xt[:, k, :], scale_ap[:, k:k + 1], rs[:], ALU.mult, ALU.mult,
                  r=[R["xt" + tag], R["rs"]], w=[R["hT"]])
        else:
            P.stt(tmp[:, k, :], xt[:, k, :], scale_ap[:, k:k + 1], rs[:], ALU.mult, ALU.mult,
                  r=[R["xt" + tag], R["rs"]], w=[R["tmp"]])
            P.ts("pool", hT_out_fn(k), tmp[:, k, :], shift_ap[:, k:k + 1], None, op0=ALU.add,
                 r=[R["tmp"]], w=[R["hT"]])


def phase_proj(C, l, cst, xT_d, w_in_d, mu_d, per, scr):
    P, nc, S = C.P, C.nc, C.S
    TT = 256
    with contextlib.ExitStack() as st:
        C.stack = st
        R = C.regs("p1_")
        import os
        TOK = min(int(os.environ.get("PROJ_TOK", "4096")), S)
        hT = C.sb("hT", [128, 8, TOK + 1], BF16)
        off = {"p0": 0}
        xb = [C.sb(f"xb{i}", [128, 8, TT]) for i in range(2)]
        sq = C.sb("sq", [128, 8, TT])
        tmp = C.sb("tmp", [128, 8, TT])
        rs = C.sb("rs", [128, TT])
        ps_s = C.ps("ps_s", [128, 512])
        pt = per["t"]
        xv = xT_d.rearrange("(k p) s -> p k s", p=128)
        def norm_pass(p0):
            if p0 == 0:
                P.memset("pool", hT[:, :, 0:1], 0.0, w=[R["hT"]])
            else:
                P.cp("dve", hT[:, :, 0:1], hT[:, :, TOK:TOK + 1], r=[R["hT"]], w=[R["hT"]])
            for ti in range(TOK // TT):
                xt = xb[ti % 2]
                tag = str(ti % 2)
                P.dma(xt[:], xv[:, :, p0 + ti * TT:p0 + (ti + 1) * TT], w=[R["xt" + tag]])
                emit_norm_tile(C, R, xt[:], lambda k: hT[:, k, 1 + ti * TT:1 + (ti + 1) * TT],
                               pt[:, 0, :], pt[:, 1, :], cst["ones"], sq, rs, tmp, ps_s, TT, cst["eps"], tag)
        wst = [C.sb(f"wst{i}", [128, 8, 512]) for i in range(2)]
        wb = [C.sb(f"wb{i}", [128, 8, 512], BF16) for i in range(2)]
        wb2 = [C.sb(f"wb2{i}", [128, 8, 512], BF16) for i in range(2)]
        mug = [C.sb(f"mug{i}", [128, 512]) for i in range(2)]
        omug = [C.sb(f"omug{i}", [128, 512]) for i in range(2)]
        ev = [C.sb(f"ev{i}", [128, 512]) for i in range(4)]
        evb = [C.sb(f"evb{i}", [128, 512], BF16) for i in range(4)]
        pso = [C.ps(f"pso{i}", [128, 512]) for i in range(6)]
        wv = w_in_d.rearrange("(k p) j -> p k j", p=128)
        cnt = {"g": 0, "ps": 0, "ev": 0}
        NT5 = TOK // 512
        NT1 = TOK // 128

        def load_group(c0, width, rw):
            g = cnt["g"] % 2
            cnt["g"] += 1
            P.dma(wst[g][:, :, :width], wv[:, :, c0:c0 + width], w=[R[f"wst{g}"]])
            if not rw:
                P.cp("pool", wb[g][:, :, :width], wst[g][:, :, :width], r=[R[f"wst{g}"]], w=[R[f"wb{g}"]])
                return wb[g], None, [R[f"wb{g}"]]
            m0 = c0 - 5120
            P.dma(mug[g][:, :width], mu_d[:, m0:m0 + width], w=[R[f"mug{g}"]])
            P.ts("pool", omug[g][:, :width], mug[g][:, :width], -1.0, 1.0, op0=ALU.mult, op1=ALU.add,
                 r=[R[f"mug{g}"]], w=[R[f"omug{g}"]])
            for k in range(8):
                P.tt("pool" if k % 2 else "dve", wb[g][:, k, :width], wst[g][:, k, :width], omug[g][:, :width], ALU.mult,
                     r=[R[f"wst{g}"], R[f"omug{g}"]], w=[R[f"wb{g}"]])
                P.tt("dve" if k % 2 else "pool", wb2[g][:, k, :width], wst[g][:, k, :width], mug[g][:, :width], ALU.mult,
                     r=[R[f"wst{g}"], R[f"mug{g}"]], w=[R[f"wb2{g}"]])
            return wb[g], wb2[g], [R[f"wb{g}"], R[f"wb2{g}"]]

        def next_ps():
            i = cnt["ps"] % 6
            cnt["ps"] += 1
            return pso[i], R[f"pso{i}"]

        def next_ev(bf):
            i = cnt["ev"] % 4
            cnt["ev"] += 1
            return (evb[i], R[f"evb{i}"]) if bf else (ev[i], R[f"ev{i}"])

        def fm_group(c0, width, rw, evac):
            w1, w2, wr = load_group(c0, width, rw)
            for cc in range(width // 128):
                for tt_ in range(NT5):
                    pt_, pr = next_ps()
                    n = 16 if rw else 8
                    for k in range(8):
                        P.mm(pt_[:], w1[:, k, cc * 128:(cc + 1) * 128], hT[:, k, 1 + tt_ * 512:1 + (tt_ + 1) * 512],
                             start=(k == 0), stop=(k == 7 and not rw), r=wr + [R["hT"]], w=[pr])
                    if rw:
                        for k in range(8):
                            P.mm(pt_[:], w2[:, k, cc * 128:(cc + 1) * 128], hT[:, k, tt_ * 512:(tt_ + 1) * 512],
                                 start=False, stop=(k == 7), r=wr + [R["hT"]], w=[pr])
                    evac(c0 + cc * 128, tt_, pt_, pr)

        def tm_group(c0, width, rw, evac):
            w1, w2, wr = load_group(c0, width, rw)
            for t1 in range(NT1):
                pt_, pr = next_ps()
                for k in range(8):
                    P.mm(pt_[:, :width], hT[:, k, 1 + t1 * 128:1 + (t1 + 1) * 128], w1[:, k, :width],
                         start=(k == 0), stop=(k == 7 and not rw), r=wr + [R["hT"]], w=[pr])
                if rw:
                    for k in range(8):
                        P.mm(pt_[:, :width], hT[:, k, t1 * 128:(t1 + 1) * 128], w2[:, k, :width],
                             start=False, stop=(k == 7), r=wr + [R["hT"]], w=[pr])
                evac(c0, t1, pt_, pr)

        def ev_lru(col, tt_, pt_, pr):
            e, er = next_ev(False)
            P.cp("act", e[:], pt_[:], r=[pr], w=[er])
            P.dma(scr["lruT"][col:col + 128, off['p0'] + tt_ * 512:off['p0'] + (tt_ + 1) * 512], e[:], r=[er], w=[P.region()])

        def ev_q(col, tt_, pt_, pr):
            e, er = next_ev(True)
            P.ts("dve", e[:], pt_[:], float(128 ** -0.5), None, op0=ALU.mult, r=[pr], w=[er])
            c = col - 2048
            P.dma(scr["qT"][c:c + 128, off['p0'] + tt_ * 512:off['p0'] + (tt_ + 1) * 512], e[:], r=[er], w=[P.region()])

        def ev_k(col, tt_, pt_, pr):
            e, er = next_ev(True)
            P.cp("act", e[:], pt_[:], r=[pr], w=[er])
            c = col - 3072
            P.dma(scr["kT"][c:c + 128, off['p0'] + tt_ * 512:off['p0'] + (tt_ + 1) * 512], e[:], r=[er], w=[P.region()])

        def ev_v(c0, t1, pt_, pr):
            e, er = next_ev(True)
            P.cp("dve", e[:], pt_[:], r=[pr], w=[er])
            c = c0 - 4096
            P.dma(scr["v"][off['p0'] + t1 * 128:off['p0'] + (t1 + 1) * 128, c:c + 512], e[:], r=[er], w=[P.region()])

        def ev_rkv(c0, t1, pt_, pr):
            e, er = next_ev(False)
            P.cp("act" if t1 % 2 else "dve", e[:], pt_[:], r=[pr], w=[er])
            c = c0 - 5120
            P.dma(scr["rkv"][off['p0'] + t1 * 128:off['p0'] + (t1 + 1) * 128, c:c + 512], e[:], r=[er], w=[P.region()])

        def ev_lora(col, tt_, pt_, pr):
            e, er = next_ev(True)
            if col == 8192:
                P.act(e[0:64, :], pt_[0:64, :], AF.Tanh, r=[pr], w=[er])
                P.cp("dve", e[64:128, :], pt_[64:128, :], r=[pr], w=[er])
            else:
                P.act(e[:], pt_[:], AF.Sigmoid, r=[pr], w=[er])
            c = col - 8192
            P.dma(scr["loraT"][c:c + 128, off['p0'] + tt_ * 512:off['p0'] + (tt_ + 1) * 512], e[:], r=[er], w=[P.region()])

        def ev_gate(col, tt_, pt_, pr):
            e, er = next_ev(False)
            P.act(e[:], pt_[:], AF.Sigmoid, r=[pr], w=[er])
            c = col - 8448
            P.dma(scr["gatesT"][c:c + 128, off['p0'] + tt_ * 512:off['p0'] + (tt_ + 1) * 512], e[:], r=[er], w=[P.region()])

        for p0 in range(0, S, TOK):
          off["p0"] = p0
          norm_pass(p0)
          for c0 in range(0, 2048, 512):
            fm_group(c0, 512, False, ev_lru)
          for c0 in range(2048, 3072, 512):
              fm_group(c0, 512, False, ev_q)
          for c0 in range(3072, 4096, 512):
              fm_group(c0, 512, False, ev_k)
          for c0 in range(4096, 5120, 512):
              tm_group(c0, 512, False, ev_v)
          for c0 in range(5120, 8192, 512):
              tm_group(c0, 512, True, ev_rkv)
          fm_group(8192, 256, True, ev_lora)
          for c0 in range(8448, 11520, 512):
              fm_group(c0, 512, False, ev_gate)
    P.end_phase()
GELU_K = 1.5957691216057308


def phase_lru(C, l, cst, Wl, scr):
    P, nc, S = C.P, C.nc, C.S
    import os
    TL = min(int(os.environ.get("LRU_TL", "2048")), S)
    with contextlib.ExitStack() as st:
        C.stack = st
        R = C.regs("lru_")
        lv = C.sb("lv", [128, 8, 8])
        wa = C.sb("wa", [128, 8, 128])
        wx = C.sb("wx", [128, 8, 128])
        c12 = C.sb("c12", [128, 2, 8])
        tiny = C.sb("tiny", [128, 1])
        carry = C.sb("carry", [128, 1])
        xin = [C.sb(f"xin{i}", [128, TL + 3]) for i in range(2)]
        gin = [C.sb(f"gin{i}", [128, TL]) for i in range(2)]
        xc = C.sb("xc", [128, TL])
        rg = C.sb("rg", [128, TL])
        ig = C.sb("ig", [128, TL])
        a_t = C.sb("a_t", [128, TL])
        e2 = C.sb("e2", [128, TL])
        b_t = C.sb("b_t", [128, TL])
        h_t = C.sb("h_t", [128, TL])
        u_t = C.sb("u_t", [128, TL])
        y_t = [C.sb(f"y_t{i}", [128, TL], BF16) for i in range(2)]
        psr = [C.ps(f"psr{i}", [128, 512]) for i in range(2)]
        psi = [C.ps(f"psi{i}", [128, 512]) for i in range(2)]
        P.dma(lv[:], Wl["lruv"][:, :, :], w=[R["lv"]])
        P.dma(wa[:], Wl["lru_wa"][:, :, :], w=[R["wa"]])
        P.dma(wx[:], Wl["lru_wx"][:, :, :], w=[R["wx"]])
        P.memset("pool", tiny[:], 1e-20, w=[R["tiny"]])
        P.act(c12[:, 0, :], lv[:, 7, :], AF.Exp, scale=-1.0, r=[R["lv"]], w=[R["c12"]])
        P.act(c12[:, 0, :], c12[:, 0, :], AF.Ln, bias=1.0, r=[R["c12"]], w=[R["c12"]])
        P.ts("dve", c12[:, 1, :], c12[:, 0, :], -16.0, None, op0=ALU.mult, r=[R["c12"]], w=[R["c12b"]])
        P.ts("dve", c12[:, 0, :], c12[:, 0, :], -8.0, None, op0=ALU.mult, r=[R["c12"], R["c12b"]], w=[R["c12"]])
        it = 0
        for c in range(8):
            rows = slice(c * 128, (c + 1) * 128)
            grows = slice(1024 + c * 128, 1024 + (c + 1) * 128)
            for ti in range(S // TL):
                t0 = ti * TL
                xi, gi = xin[it % 2], gin[it % 2]
                xr, gr = R[f"xin{it % 2}"], R[f"gin{it % 2}"]
                if ti == 0:
                    P.memset("pool", xi[:, 0:3], 0.0, w=[xr])
                    P.dma(xi[:, 3:3 + TL], scr["lruT"][rows, 0:TL], w=[xr])
                else:
                    P.dma(xi[:, 0:3 + TL], scr["lruT"][rows, t0 - 3:t0 + TL], w=[xr])
                P.dma(gi[:], scr["lruT"][grows, t0:t0 + TL], w=[gr])
                cw = lambda k: lv[:, k, c:c + 1]
                P.ts("dve", xc[:], xi[:, 3:3 + TL], cw(0), cw(4), op0=ALU.mult, op1=ALU.add, r=[xr, R["lv"]], w=[R["xc"]])
                for k in (1, 2, 3):
                    P.stt(xc[:], xi[:, 3 - k:3 - k + TL], cw(k), xc[:], ALU.mult, ALU.add, r=[xr, R["xc"]], w=[R["xc"]])
                for j in range(TL // 512):
                    sl = slice(j * 512, (j + 1) * 512)
                    P.mm(psr[j % 2][:], wa[:, c, :], xc[:, sl], r=[R["wa"], R["xc"]], w=[R[f"psr{j % 2}"]])
                    P.mm(psi[j % 2][:], wx[:, c, :], xc[:, sl], r=[R["wx"], R["xc"]], w=[R[f"psi{j % 2}"]])
                    P.act(rg[:, sl], psr[j % 2][:], AF.Sigmoid, bias=lv[:, 5, c:c + 1], r=[R[f"psr{j % 2}"]], w=[R["rg"]])
                    P.act(ig[:, sl], psi[j % 2][:], AF.Sigmoid, bias=lv[:, 6, c:c + 1], r=[R[f"psi{j % 2}"]], w=[R["ig"]])
                P.act(a_t[:], rg[:], AF.Exp, scale=c12[:, 0, c:c + 1], r=[R["rg"], R["c12"]], w=[R["a"]])
                P.act(e2[:], rg[:], AF.Exp, scale=c12[:, 1, c:c + 1], r=[R["rg"], R["c12b"]], w=[R["e2"]])
                P.ts("pool", e2[:], e2[:], -1.0, 1.0, op0=ALU.mult, op1=ALU.add, r=[R["e2"]], w=[R["e2"]])
                P.act(e2[:], e2[:], AF.Ln, bias=tiny[:], r=[R["e2"], R["tiny"]], w=[R["e2"]])
                P.act(e2[:], e2[:], AF.Exp, scale=0.5, r=[R["e2"]], w=[R["e2"]])
                P.tt("dve", b_t[:], e2[:], ig[:], ALU.mult, r=[R["e2"], R["ig"]], w=[R["b"]])
                P.tt("pool", b_t[:], b_t[:], xc[:], ALU.mult, r=[R["b"], R["xc"]], w=[R["b"]])
                init = 0.0 if ti == 0 else carry[:, 0:1]
                P.op("dve", (lambda init=init: (lambda e: e.tensor_tensor_scan(out=h_t[:], data0=a_t[:], data1=b_t[:],
                                                                             initial=init, op0=ALU.mult, op1=ALU.add)))(),
                     r=[R["a"], R["b"], R["carry"]], w=[R["h"]])
                P.cp("pool", carry[:], h_t[:, TL - 1:TL], r=[R["h"]], w=[R["carry"]])
                P.tt("pool", u_t[:], gi[:], gi[:], ALU.mult, r=[gr], w=[R["u"]])
                P.ts("pool", u_t[:], u_t[:], 0.044715, 1.0, op0=ALU.mult, op1=ALU.add, r=[R["u"]], w=[R["u"]])
                P.tt("dve", u_t[:], u_t[:], gi[:], ALU.mult, r=[R["u"], gr], w=[R["u"]])
                P.act(u_t[:], u_t[:], AF.Sigmoid, scale=GELU_K, r=[R["u"]], w=[R["u"]])
                P.tt("pool", u_t[:], u_t[:], gi[:], ALU.mult, r=[R["u"], gr], w=[R["u"]])
                yt, yr = y_t[it % 2], R[f"y{it % 2}"]
                P.tt("dve", yt[:], u_t[:], h_t[:], ALU.mult, r=[R["u"], R["h"]], w=[yr])
                P.dma(scr["yaT"][rows, t0:t0 + TL], yt[:], r=[yr], w=[P.region()])
                it += 1
    P.end_phase()


def phase_sb(C, l, cst, scr):
    P, nc, S = C.P, C.nc, C.S
    NQ = S // 512
    NB = S // 128
    with contextlib.ExitStack() as st:
        C.stack = st
        R = C.regs("sb_")
        qh = [C.sb(f"qh{i}", [128, S], BF16) for i in range(2)]
        kh = [C.sb(f"kh{i}", [128, S], BF16) for i in range(2)]
        vh = [C.sb(f"vh{i}", [128, NB, 128], BF16) for i in range(2)]
        NS = 2
        e_b = [[C.sb(f"e{s}_{i}", [128, 512]) for i in range(2)] for s in range(NS)]
        sp_b = [[C.sb(f"sp{s}_{i}", [128, 512], BF16) for i in range(2)] for s in range(NS)]
        d_b = [[C.sb(f"d{s}_{i}", [128, 512]) for i in range(2)] for s in range(NS)]
        at_b = [[C.sb(f"at{s}_{i}", [128, 512], BF16) for i in range(2)] for s in range(NS)]
        yo = [C.sb(f"yo{s}", [128, 512], BF16) for s in range(NS)]
        pz = [[C.ps(f"pz{s}_{i}", [128, 512]) for i in range(2)] for s in range(NS)]
        pc = [C.ps(f"pc{s}", [128, 512]) for s in range(NS)]
        po = [C.ps(f"po{s}", [128, 512]) for s in range(NS)]
        tri_ge, tri_lt, mask = cst["tri_ge_b"], cst["tri_lt_b"], cst["sbmask_b"]
        vv = scr["v"].rearrange("(n p) d -> p n d", p=128)

        def stream(s, hd, qt, hb):
            q_t, k_t, v_t = qh[hb], kh[hb], vh[hb]
            hr = [R[f"q{hb}"], R[f"k{hb}"], R[f"v{hb}"]]
            kbs = list(range(4 * qt + 3, -1, -1))
            pcr, por = R[f"pc{s}"], R[f"po{s}"]

            def front(idx):
                kb = kbs[idx]
                i2 = idx % 2
                z, zr = pz[s][i2], R[f"pz{s}_{i2}"]
                et, er = e_b[s][i2], R[f"e{s}_{i2}"]
                spt, spr = sp_b[s][i2], R[f"sp{s}_{i2}"]
                P.mm(z[:], k_t[:, kb * 128:(kb + 1) * 128], q_t[:, qt * 512:(qt + 1) * 512], r=hr[0:2], w=[zr])
                P.act(et[:], z[:], AF.Exp, r=[zr], w=[er])
                P.act(spt[:], et[:], AF.Ln, bias=1.0, r=[er], w=[spr])
                mi = kb - 4 * qt
                if mi >= 0:
                    P.tt("dve", spt[:], spt[:], mask[:, mi * 512:(mi + 1) * 512], ALU.mult, r=[spr], w=[spr])

            def back1(idx):
                last = idx == len(kbs) - 1
                i2 = idx % 2
                spt, spr = sp_b[s][i2], R[f"sp{s}_{i2}"]
                dt_, dr = d_b[s][i2], R[f"d{s}_{i2}"]
                P.mm(pc[s][:], tri_ge, spt[:], start=(idx == 0), stop=last, r=[spr], w=[pcr], skip_group_check=True)
                P.act(dt_[:], pc[s][:], AF.Exp, scale=-1.0, r=[pcr], w=[dr])

            def back2(idx):
                kb = kbs[idx]
                last = idx == len(kbs) - 1
                i2 = idx % 2
                et, er = e_b[s][i2], R[f"e{s}_{i2}"]
                spt, spr = sp_b[s][i2], R[f"sp{s}_{i2}"]
                dt_, dr = d_b[s][i2], R[f"d{s}_{i2}"]
                att, atr = at_b[s][i2], R[f"at{s}_{i2}"]
                if not last:
                    P.mm(pc[s][:], tri_lt, spt[:], start=False, stop=False, r=[spr], w=[pcr], skip_group_check=True)
                P.tt("dve", att[:], et[:], dt_[:], ALU.mult, r=[er, dr], w=[atr])
                mi = kb - 4 * qt
                if mi >= 0:
                    P.tt("dve", att[:], att[:], mask[:, mi * 512:(mi + 1) * 512], ALU.mult, r=[atr], w=[atr])

            def av(idx):
                kb = kbs[idx]
                last = idx == len(kbs) - 1
                i2 = idx % 2
                att, atr = at_b[s][i2], R[f"at{s}_{i2}"]
                P.mm(po[s][:], v_t[:, kb, :], att[:], start=(idx == 0), stop=last, r=[atr, hr[2]], w=[por])

            front(0)
            for idx in range(len(kbs)):
                if idx + 1 < len(kbs):
                    front(idx + 1)
                back1(idx)
                yield
                back2(idx)
                if idx >= 1:
                    av(idx - 1)
                yield
            av(len(kbs) - 1)
            P.cp("dve", yo[s][:], po[s][:], r=[R[f"po{s}"]], w=[R[f"yo{s}"]])
            P.dma(scr["ybT"][hd * 128:(hd + 1) * 128, qt * 512:(qt + 1) * 512], yo[s][:], r=[R[f"yo{s}"]], w=[P.region()])

        for hd in range(8):
            hb = hd % 2
            rows = slice(hd * 128, (hd + 1) * 128)
            P.dma(qh[hb][:], scr["qT"][rows, :], w=[R[f"q{hb}"]])
            P.dma(kh[hb][:], scr["kT"][rows, :], w=[R[f"k{hb}"]])
            for n0 in range(0, NB, 16):
                n1 = min(NB, n0 + 16)
                P.dma(vh[hb][:, n0:n1, :], vv[:, n0:n1, rows], w=[R[f"v{hb}"]])
            order = []
            lo, hi = 0, NQ - 1
            while lo <= hi:
                order.append(hi)
                if lo != hi:
                    order.append(lo)
                lo += 1
                hi -= 1
            pending = list(order)
            active = []
            free_slots = list(range(NS))
            while pending or active:
                while pending and free_slots:
                    s = free_slots.pop(0)
                    active.append((s, stream(s, hd, pending.pop(0), hb)))
                nxt = []
                for (s, g) in active:
                    try:
                        next(g)
                        nxt.append((s, g))
                    except StopIteration:
                        free_slots.append(s)
                active = nxt
    P.end_phase()


RW_C0 = 0.6065306597126334


def phase_rwkv(C, l, cst, Wl, scr):
    P, nc, S = C.P, C.nc, C.S
    NCH = S // 128
    with contextlib.ExitStack() as st:
        C.stack = st
        R = C.regs("rw_")
        rwv = C.sb("rwv", [128, 7, 1024])
        wst = C.sb("rw_wst", [128, 1024])
        waup = C.sb("waup", [128, 1024], BF16)
        gup = C.sb("gup", [128, 1024], BF16)
        epsg = C.sb("epsg", [128, 1])
        T1 = C.sb("T1", [128, 3, 1024])
        lt = C.sb("lt", [128, 2, 128], BF16)
        lta = C.sb("lta", [64, 128], BF16)
        aup = C.sb("aup", [64, 1024], BF16)
        sgw = C.sb("sgw", [128, 1024])
        a_t = C.sb("rwa", [128, 1024])
        kk = C.sb("kk", [128, 1024])
        b_t = C.sb("rwb", [128, 1024])
        clsb = C.sb("clsb", [128, 1024])
        tmp = C.sb("rwtmp", [128, 1024])
        E = [C.sb(f"rwE{i}", [128, 1024]) for i in range(2)]
        gC = C.sb("gC", [64, 1024])
        gate = C.sb("rwgate", [128, 1024])
        small = C.sb("rwsmall", [128, 8, 16])
        prod = {n: C.sb("pr_" + n, [128, 1024], BF16) for n in ("kq", "bh", "kh", "rq", "bE", "kE", "v")}
        XT = {n: C.sb("xt_" + n, [64, 16, 128], BF16) for n in ("kq", "bh", "kh", "rq")}
        G = 4
        xs = [C.sb(f"xs{i}", [128, 256]) for i in range(G)]
        akt = [C.sb(f"akt{i}", [128, 128], BF16) for i in range(G)]
        Mn = [[C.sb(f"Mn{i}_{j}", [128, 256]) for j in range(2)] for i in range(G)]
        TTb = [[C.sb(f"TT{i}_{j}", [128, 128]) for j in range(2)] for i in range(G)]
        TTf = [C.sb(f"TTf{i}", [128, 128], BF16) for i in range(G)]
        kcw = [C.sb(f"kcw{i}", [128, 128], BF16) for i in range(G)]
        dg = [C.sb(f"dg{i}", [64, 64]) for i in range(G)]
        BbT = C.sb("BbT", [128, 16, 128], BF16)
        BkT = C.sb("BkT", [128, 16, 128], BF16)
        Uv = C.sb("Uv", [128, 16, 64], BF16)
        RcT = C.sb("RcT", [64, 16, 128], BF16)
        GT = C.sb("GT", [64, 16, 64])
        H_a = C.sb("H_a", [64, 16, 64])
        st_f = C.sb("st_f", [64, 16, 64])
        st_b = C.sb("st_b", [64, 16, 64], BF16)
        ysb = C.sb("ysb", [128, 1024])
        ycs = C.sb("ycs", [128, 8, 128], BF16)
        PW = C.ps("PW", [128, 1024])
        PT = C.ps("PT", [128, 2048], BF16)
        banks = [C.ps(f"rwbk{i}", [128, 512]) for i in range(4)]
        for _k in ("PW", "PT", "bk0", "bk1", "bk2", "bk3"):
            R[_k].excl = True
        bcnt = [0]

        def bank():
            i = bcnt[0] % 4
            bcnt[0] += 1
            return banks[i], R[f"bk{i}"]

        ident_f, ident_b, ones_f, tri_le = cst["ident"], cst["ident_b"], cst["ones"], cst["tri_le"]
        mask3, m_ui = cst["mask3"], cst["m_ui"]
        P.dma(rwv[:], Wl["rwv"][:, :, :], w=[R["rwv"]])
        P.dma(wst[0:64, :], Wl["rw_w_up"][:, :], w=[R["wst"]])
        P.dma(wst[64:128, :], Wl["rw_a_up"][:, :], w=[R["wst"]])
        P.cp("dve", waup[:], wst[:], r=[R["wst"]], w=[R["waup"]])
        P.dma(wst[0:64, :], Wl["rw_a_up"][:, :], w=[R["wst"]])
        P.cp("dve", aup[:], wst[0:64, :], r=[R["wst"]], w=[R["aup"]])
        P.dma(wst[:], Wl["rw_g_up"][:, :], w=[R["wst"]])
        P.cp("dve", gup[:], wst[:], r=[R["wst"]], w=[R["gup"]])
        P.memset("pool", epsg[:], 64e-5, w=[R["epsg"]])
        P.memset("pool", st_f[:], 0.0, w=[R["st_f"]])
        P.memset("pool", st_b[:], 0.0, w=[R["st_b"]])
        ycv = scr["ycT"].rearrange("(k p) s -> p k s", p=128)
        hv = lambda t: t.rearrange("p (h j) -> p h j", j=64)
        rW, rA, rKK, rKA, rRK, rLW, rLB = (rwv[:, i, :] for i in range(7))

        for n in range(NCH):
            t0 = n * 128
            r_, k_, v_ = T1[:, 0, :], T1[:, 1, :], T1[:, 2, :]
            P.dma(T1[:], scr["rkv"][t0:t0 + 128, :].rearrange("p (a c) -> p a c", a=3), w=[R["T1"]])
            P.dma(lt[:, 0, :], scr["loraT"][0:128, t0:t0 + 128], w=[R["lt"]])
            P.dma(lt[:, 1, :], scr["loraT"][128:256, t0:t0 + 128], w=[R["lt"]])
            P.dma(lta[:], scr["loraT"][64:128, t0:t0 + 128], w=[R["lta"]])
            for hf in range(2):
                sl = slice(hf * 512, (hf + 1) * 512)
                P.mm(PW[:, sl], lt[0:64, 0, :], waup[0:64, sl], r=[R["lt"], R["waup"]], w=[R["PW"]])
            P.tt("dve", sgw[:], PW[:], rW, ALU.add, r=[R["PW"], R["rwv"]], w=[R["sgw"]])
            P.act(sgw[:], sgw[:], AF.Sigmoid, r=[R["sgw"]], w=[R["sgw"]])
            for hf in range(2):
                sl = slice(hf * 512, (hf + 1) * 512)
                P.mm(PW[:, sl], lta[:], aup[:, sl], r=[R["lta"], R["aup"]], w=[R["PW"]])
            P.tt("dve", a_t[:], PW[:], rA, ALU.add, r=[R["PW"], R["rwv"]], w=[R["a"]])
            P.act(a_t[:], a_t[:], AF.Sigmoid, r=[R["a"]], w=[R["a"]])
            for hf in range(2):
                sl = slice(hf * 512, (hf + 1) * 512)
                P.mm(PW[:, sl], lt[:, 1, :], gup[:, sl], r=[R["lt"], R["gup"]], w=[R["PW"]])
            P.cp("act", gate[:], PW[:], r=[R["PW"]], w=[R["gate"]])
            P.tt("dve", kk[:], k_, rKK, ALU.mult, r=[R["T1"], R["rwv"]], w=[R["kk"]])
            P.tt("pool", tmp[:], kk[:], kk[:], ALU.mult, r=[R["kk"]], w=[R["tmp"]])
            P.op("dve", lambda e: e.tensor_reduce(out=small[:, 0, :], in_=hv(tmp[:]), axis=AX.X, op=ALU.add),
                 r=[R["tmp"]], w=[R["sm0"]])
            P.ts("dve", small[:, 0, :], small[:, 0, :], 1e-24, None, op0=ALU.max, r=[R["sm0"]], w=[R["sm0"]])
            P.act(small[:, 0, :], small[:, 0, :], AF.Ln, r=[R["sm0"]], w=[R["sm0"]])
            P.act(small[:, 0, :], small[:, 0, :], AF.Exp, scale=-0.5, r=[R["sm0"]], w=[R["sm0"]])
            for h in range(16):
                hs = slice(h * 64, (h + 1) * 64)
                P.ts("dve" if h % 2 else "pool", kk[:, hs], kk[:, hs], small[:, 0, h:h + 1], None, op0=ALU.mult,
                     r=[R["kk"], R["sm0"]], w=[R["kk"]])
            P.stt(tmp[:], a_t[:], -1.0, rKA, ALU.add, ALU.mult, r=[R["a"], R["rwv"], R["tmp"]], w=[R["tmp"]])
            P.stt(k_, tmp[:], 1.0, k_, ALU.add, ALU.mult, r=[R["tmp"], R["T1"], R["kk"]], w=[R["T1"]])
            P.tt("pool", b_t[:], kk[:], a_t[:], ALU.mult, r=[R["kk"], R["a"]], w=[R["b"]])
            P.tt("pool", tmp[:], r_, k_, ALU.mult, r=[R["T1"]], w=[R["tmp"]])
            P.tt("pool", tmp[:], tmp[:], rRK, ALU.mult, r=[R["tmp"], R["rwv"]], w=[R["tmp"]])
            P.op("dve", lambda e: e.tensor_reduce(out=small[:, 1, :], in_=hv(tmp[:]), axis=AX.X, op=ALU.add),
                 r=[R["tmp"]], w=[R["sm1"]])
            for hf in range(2):
                sl = slice(hf * 512, (hf + 1) * 512)
                P.mm(PW[:, sl], tri_le, sgw[:, sl], r=[R["sgw"]], w=[R["PW"]])
            P.cp("act", clsb[:], PW[:], r=[R["PW"]], w=[R["clsb"]])
            for hf in range(2):
                sl = slice(hf * 512, (hf + 1) * 512)
                P.mm(PW[:, sl], ones_f, sgw[:, sl], r=[R["sgw"]], w=[R["PW"]])
            E0, E1 = E[0], E[1]
            P.act(E0[:], clsb[:], AF.Exp, scale=-RW_C0, r=[R["clsb"]], w=[R["E0"]])
            P.tt("dve", prod["rq"][:], r_, E0[:], ALU.mult, r=[R["T1"], R["E0"]], w=[R["p_rq"]])
            P.act(E1[:], clsb[:], AF.Exp, scale=RW_C0, r=[R["clsb"]], w=[R["E1"]])
            P.tt("pool", prod["bh"][:], b_t[:], E1[:], ALU.mult, r=[R["b"], R["E1"]], w=[R["p_bh"]])
            P.tt("dve", prod["kh"][:], k_, E1[:], ALU.mult, r=[R["T1"], R["E1"]], w=[R["p_kh"]])
            P.tt("pool", tmp[:], clsb[:], sgw[:], ALU.subtract, r=[R["clsb"], R["sgw"], R["tmp"]], w=[R["tmp"]])
            P.act(E0[:], tmp[:], AF.Exp, scale=-RW_C0, r=[R["tmp"], R["E0"]], w=[R["E0"]])
            P.tt("dve", prod["kq"][:], kk[:], E0[:], ALU.mult, r=[R["kk"], R["E0"]], w=[R["p_kq"]])
            P.tt("dve", tmp[:], PW[:], clsb[:], ALU.subtract, r=[R["PW"], R["clsb"], R["tmp"]], w=[R["tmp"]])
            P.act(E1[:], tmp[:], AF.Exp, scale=-RW_C0, r=[R["tmp"], R["E1"]], w=[R["E1"]])
            P.tt("pool", prod["bE"][:], b_t[:], E1[:], ALU.mult, r=[R["b"], R["E1"]], w=[R["p_bE"]])
            P.tt("dve", prod["kE"][:], k_, E1[:], ALU.mult, r=[R["T1"], R["E1"]], w=[R["p_kE"]])
            P.act(gC[:], PW[0:64, :], AF.Exp, scale=-RW_C0, r=[R["PW"]], w=[R["gC"]])
            P.cp("pool", prod["v"][:], v_, r=[R["T1"]], w=[R["p_v"]])
            import os as _os
            _stop = _os.environ.get("RW_STOP", "")
            if _stop == "A":
                continue
            PTv = PT[0:64, :].rearrange("p (h t) -> p h t", t=128)
            for qi, qn in enumerate(("kq", "bh", "kh", "rq")):
                for h in range(16):
                    P.tr(PTv[:, h, :], prod[qn][:, h * 64:(h + 1) * 64], ident_b, r=[R["p_" + qn]], w=[R["PT"]])
                P.cp("act" if qi % 2 else "dve", XT[qn][:], PTv, r=[R["PT"]], w=[R["xt_" + qn]])

            if _stop == "T":
                continue
            def head(slot, h):
                hs = slice(h * 64, (h + 1) * 64)
                kqT, bhT, khT, rqT = (XT[q][:, h, :] for q in ("kq", "bh", "kh", "rq"))
                xr = [R["xt_kq"], R["xt_bh"], R["xt_kh"], R["xt_rq"]]
                X, Xr = banks[slot], R[f"bk{slot}"]
                P.mm(X[:, 0:128], kqT, bhT, r=xr, w=[Xr])
                P.mm(X[:, 128:256], bhT, kqT, r=xr, w=[Xr])
                P.mm(X[:, 256:384], khT, kqT, r=xr, w=[Xr])
                P.mm(X[:, 384:512], bhT, rqT, r=xr, w=[Xr])
                yield
                xs_, xsr = xs[slot], R[f"xs{slot}"]
                P.tt("dve", xs_[:], X[:, 0:256], mask3[:, 0:256], ALU.mult, r=[Xr], w=[xsr])
                P.tt("dve", akt[slot][:], X[:, 256:384], mask3[:, 256:384], ALU.mult, r=[Xr], w=[R[f"akt{slot}"]])
                P.tt("dve", BbT[:, h, :], X[:, 384:512], m_ui, ALU.mult, r=[Xr], w=[R[f"BbT{h}"]])
                tcur = 0
                TT_, TTr = TTb[slot][tcur], R[f"TT{slot}_{tcur}"]
                P.tt("pool", TT_[:], ident_f, xs_[:, 128:256], ALU.subtract, r=[xsr], w=[TTr])
                M_, MT_, Mr = xs_[:, 0:128], xs_[:, 128:256], xsr
                yield
                for lev in range(6):
                    L, Lr = X, Xr
                    P.mm(L[:, 0:128], MT_, M_, r=[Mr], w=[Lr])
                    if lev < 5:
                        P.mm(L[:, 128:256], M_, MT_, r=[Mr], w=[Lr])
                    if lev == 0:
                        P.mm(L[:, 384:512], khT, rqT, r=xr, w=[Lr])
                    yield
                    if lev == 0:
                        P.tt("dve", BkT[:, h, :], L[:, 384:512], m_ui, ALU.mult, r=[Lr], w=[R[f"BkT{h}"]])
                    mn, mnr = Mn[slot][lev % 2], R[f"Mn{slot}_{lev % 2}"]
                    wdt = 256 if lev < 5 else 128
                    P.cp("act" if (h + lev) % 2 else "dve", mn[:, 0:wdt], L[:, 0:wdt], r=[Lr], w=[mnr])
                    P.mm(L[:, 256:384], mn[:, 0:128], TT_[:], r=[mnr, TTr], w=[Lr])
                    yield
                    tn = 1 - tcur
                    TTn, TTnr = TTb[slot][tn], R[f"TT{slot}_{tn}"]
                    P.tt("dve", TTn[:], L[:, 256:384], TT_[:], ALU.add, r=[Lr, TTr], w=[TTnr])
                    TT_, TTr, tcur = TTn, TTnr, tn
                    M_, MT_, Mr = mn[:, 0:128], mn[:, 128:256], mnr
                Z2, Z2r = X, Xr
                P.cp("pool", TTf[slot][:], TT_[:], r=[TTr], w=[R[f"TTf{slot}"]])
                TT_, TTr = TTf[slot], R[f"TTf{slot}"]
                P.mm(Z2[:, 0:64], TT_[:], prod["kq"][:, hs], r=[TTr, R["p_kq"]], w=[Z2r])
                P.mm(Z2[:, 64:128], akt[slot][:], prod["v"][:, hs], r=[R[f"akt{slot}"], R["p_v"]], w=[Z2r])
                yield
                kc_, kcr = kcw[slot], R[f"kcw{slot}"]
                P.cp("act", kc_[:], Z2[:, 0:128], r=[Z2r], w=[kcr])
                P.mm(Z2[:, 128:192], TT_[:], kc_[:, 64:128], r=[TTr, kcr], w=[Z2r])
                P.mm(Z2[0:64, 192:256], kc_[:, 0:64], prod["bE"][:, hs], r=[kcr, R["p_bE"]], w=[Z2r])
                P.mm(Z2[0:64, 256:384], kc_[:, 0:64], BbT[:, h, :], r=[kcr, R[f"BbT{h}"]], w=[Z2r])
                yield
                P.ts("dve", Uv[:, h, :], Z2[:, 128:192], -1.0, None, op0=ALU.mult, r=[Z2r], w=[R[f"Uv{h}"]])
                P.tt("pool", dg[slot][:], ident_f[0:64, 0:64], gC[:, hs], ALU.mult, r=[R["gC"]], w=[R[f"dg{slot}"]])
                P.tt("dve", GT[:, h, :], dg[slot][:], Z2[0:64, 192:256], ALU.subtract, r=[R[f"dg{slot}"], Z2r], w=[R[f"GT{h}"]])
                P.tt("dve", RcT[:, h, :], rqT, Z2[0:64, 256:384], ALU.subtract, r=[R["xt_rq"], Z2r], w=[R[f"RcT{h}"]])
                P.mm(Z2[0:64, 384:448], prod["bE"][:, hs], Uv[:, h, :], start=True, stop=False,
                     r=[R["p_bE"], R[f"Uv{h}"]], w=[Z2r])
                P.mm(Z2[0:64, 384:448], prod["kE"][:, hs], prod["v"][:, hs], start=False, stop=True,
                     r=[R["p_kE"], R["p_v"]], w=[Z2r])
                yield
                P.cp("act", H_a[:, h, :], Z2[0:64, 384:448], r=[Z2r], w=[R[f"H{h}"]])

            pending = list(range(16))
            active = []
            _hc = {}
            free_slots = list(range(G))
            while pending or active:
                while pending and free_slots:
                    s_ = free_slots.pop(0)
                    active.append((s_, head(s_, pending.pop(0))))
                nxt = []
                for (s_, g_) in active:
                    try:
                        next(g_)
                        _hc[s_] = _hc.get(s_, 0) + 1
                        if _hc[s_] >= int(_os.environ.get("RW_HSTOP", "999")):
                            _hc[s_] = 0
                            g_.close()
                            raise StopIteration
                        nxt.append((s_, g_))
                    except StopIteration:
                        _hc[s_] = 0
                        free_slots.append(s_)
                active = nxt
            if _stop == "B":
                continue
            for h in range(16):
                hs = slice(h * 64, (h + 1) * 64)
                P.mm(PW[:, hs], RcT[:, h, :], st_b[:, h, :], start=True, stop=False,
                     r=[R[f"RcT{h}"], R["st_b"]], w=[R["PW"]])
                P.mm(PW[:, hs], BbT[:, h, :], Uv[:, h, :], start=False, stop=False,
                     r=[R[f"BbT{h}"], R[f"Uv{h}"]], w=[R["PW"]])
                P.mm(PW[:, hs], BkT[:, h, :], prod["v"][:, hs], start=False, stop=True,
                     r=[R[f"BkT{h}"], R["p_v"]], w=[R["PW"]])
            P.cp("act", ysb[:], PW[:], r=[R["PW"]], w=[R["ysb"]])
            for h in range(16):
                hs = slice(h * 64, (h + 1) * 64)
                P.mm(PW[0:64, hs], GT[:, h, :], st_f[:, h, :], r=[R[f"GT{h}"], R["st_f"]], w=[R["PW"]])
            P.tt("dve", st_f[:].rearrange("p h j -> p (h j)"), PW[0:64, :], H_a[:].rearrange("p h j -> p (h j)"), ALU.add,
                 r=[R["PW"]] + [R[f"H{h}"] for h in range(16)], w=[R["st_f"]])
            P.cp("act", st_b[:], st_f[:], r=[R["st_f"]], w=[R["st_b"]])
            P.op("dve", lambda e: e.tensor_reduce(out=small[:, 2, :], in_=hv(ysb[:]), axis=AX.X, op=ALU.add),
                 r=[R["ysb"]], w=[R["sm2"]])
            P.tt("pool", tmp[:], ysb[:], ysb[:], ALU.mult, r=[R["ysb"], R["tmp"]], w=[R["tmp"]])
            P.op("dve", lambda e: e.tensor_reduce(out=small[:, 3, :], in_=hv(tmp[:]), axis=AX.X, op=ALU.add),
                 r=[R["tmp"]], w=[R["sm3"]])
            P.ts("dve", small[:, 4, :], small[:, 2, :], 1.0 / 64, None, op0=ALU.mult, r=[R["sm2"]], w=[R["sm4"]])
            P.tt("dve", small[:, 5, :], small[:, 4, :], small[:, 4, :], ALU.mult, r=[R["sm4"]], w=[R["sm5"]])
            P.stt(small[:, 6, :], small[:, 3, :], 1.0 / 64, small[:, 5, :], ALU.mult, ALU.subtract,
                  r=[R["sm3"], R["sm5"]], w=[R["sm6"]])
            P.act(small[:, 6, :], small[:, 6, :], AF.Ln, bias=epsg[:], r=[R["sm6"], R["epsg"]], w=[R["sm6"]])
            P.act(small[:, 6, :], small[:, 6, :], AF.Exp, scale=-0.5, r=[R["sm6"]], w=[R["sm6"]])
            for h in range(16):
                hs = slice(h * 64, (h + 1) * 64)
                P.ts("dve" if h % 2 else "pool", ysb[:, hs], ysb[:, hs], small[:, 4, h:h + 1], small[:, 6, h:h + 1],
                     op0=ALU.subtract, op1=ALU.mult, r=[R["ysb"], R["sm4"], R["sm6"]], w=[R["ysb"]])
            P.tt("pool", ysb[:], ysb[:], rLW, ALU.mult, r=[R["ysb"], R["rwv"]], w=[R["ysb"]])
            P.tt("dve", ysb[:], ysb[:], rLB, ALU.add, r=[R["ysb"], R["rwv"]], w=[R["ysb"]])
            for h in range(16):
                hs = slice(h * 64, (h + 1) * 64)
                P.stt(ysb[:, hs], v_[:, hs], small[:, 1, h:h + 1], ysb[:, hs], ALU.mult, ALU.add,
                      r=[R["T1"], R["sm1"], R["ysb"]], w=[R["ysb"]])
            P.tt("pool", ysb[:], ysb[:], gate[:], ALU.mult, r=[R["ysb"], R["gate"]], w=[R["ysb"]])
            for k in range(8):
                P.tr(PW[:, k * 128:(k + 1) * 128], ysb[:, k * 128:(k + 1) * 128], ident_f, r=[R["ysb"]], w=[R["PW"]])
            P.cp("act", ycs[:].rearrange("p k t -> p (k t)"), PW[:], r=[R["PW"]], w=[R["ycs"]])
            P.dma(ycv[:, :, t0:t0 + 128], ycs[:], r=[R["ycs"]], w=[P.region()])
    P.end_phase()
def phase_merge(C, l, cst, Wl, x_in, per, scr):
    P, nc, S = C.P, C.nc, C.S
    TT = 256
    with contextlib.ExitStack() as st:
        C.stack = st
        R = C.regs("mg_")
        wts = {n: C.sb("mg_" + n, [128, 8, 1024], BF16) for n in ("p_lru", "p_sb", "p_rwkv", "w_out")}
        wr = C.sb("mg_wr", [128, 8, 32])
        brt = C.sb("mg_br", [128, 32])
        pt = per["t"]
        with contextlib.ExitStack() as st2:
            C.stack = st2
            wst = C.sb("mg_wst", [128, 8, 1024])
            for i, n in enumerate(("p_lru", "p_sb", "p_rwkv", "w_out")):
                P.dma(wst[:], Wl[n].rearrange("(k p) j -> p k j", p=128), w=[R["wst"]])
                P.cp("pool" if i % 2 else "dve", wts[n][:], wst[:], r=[R["wst"]], w=[R["w_" + n]])
            P.end_phase()
        C.stack = st
        P.dma(wr[:], Wl["w_router"].rearrange("(k p) j -> p k j", p=128), w=[R["wr"]])
        P.dma(brt[:], Wl["b_router"][:, :], w=[R["br"]])
        yt = {n: C.sb("mg_y" + n, [128, 8, TT], BF16) for n in ("a", "b", "c")}
        gt = [C.sb(f"mg_g{i}", [128, 3, TT]) for i in range(2)]
        mrg = C.sb("mg_mrg", [128, 8, TT], BF16)
        m1 = C.sb("mg_m1", [128, TT])
        m2 = C.sb("mg_m2", [128, TT])
        xt = C.sb("mg_xt", [128, 8, TT])
        x1 = C.sb("mg_x1", [128, 8, TT])
        h2f = C.sb("mg_h2f", [128, 8, TT])
        h2b = C.sb("mg_h2b", [128, 8, TT], BF16)
        sq = C.sb("mg_sq", [128, 8, TT])
        tmp = C.sb("mg_tmp", [128, 8, TT])
        rs = C.sb("mg_rs", [128, TT])
        lg = C.sb("mg_lg", [128, 32])
        m8 = C.sb("mg_m8", [128, 8])
        nmx = C.sb("mg_nmx", [128, 1])
        msk = C.sb("mg_msk", [128, 32])
        ex = C.sb("mg_ex", [128, 32])
        ssum = C.sb("mg_ssum", [128, 1])
        rwo = [C.sb(f"mg_rwo{i}", [128, 32]) for i in range(2)]
        psb = [C.ps(f"mg_ps{i}", [128, 512]) for i in range(3)]
        psm = C.ps("mg_psm", [128, 512])
        ps_s = C.ps("mg_pss", [128, 512])
        psl = C.ps("mg_psl", [128, 512])
        for k_ in ("ps0", "ps1", "ps2", "psm", "ps_s", "psl"):
            R[k_].excl = True
        xv = x_in.rearrange("(k p) s -> p k s", p=128)
        yv = {n: scr["y%sT" % n].rearrange("(k p) s -> p k s", p=128) for n in ("a", "b", "c")}
        gv = scr["gatesT"].rearrange("(g k p) s -> p g k s", p=128, k=8)
        x1v = scr["x1T"].rearrange("(k p) s -> p k s", p=128)
        h2v = scr["h2T"].rearrange("(k p) s -> p k s", p=128)
        pw = (("a", "p_lru"), ("b", "p_sb"), ("c", "p_rwkv"))
        gi = 0
        for ti in range(S // TT):
            ts_ = slice(ti * TT, (ti + 1) * TT)
            for n in ("a", "b", "c"):
                P.dma(yt[n][:], yv[n][:, :, ts_], w=[R["y" + n]])
            P.dma(xt[:], xv[:, :, ts_], w=[R["xt"]])
            for oc in range(8):
                g_ = gt[gi % 2]
                gr = R[f"g{gi % 2}"]
                gi += 1
                P.dma(g_[:], gv[:, :, oc, ts_], w=[gr])
                for bi, (yn, wn) in enumerate(pw):
                    for k in range(8):
                        P.mm(psb[bi][:, :TT], wts[wn][:, k, oc * 128:(oc + 1) * 128], yt[yn][:, k, :],
                             start=(k == 0), stop=(k == 7), r=[R["w_" + wn], R["y" + yn]], w=[R[f"ps{bi}"]])
                P.tt("dve", m1[:], psb[0][:, :TT], g_[:, 0, :], ALU.mult, r=[R["ps0"], gr], w=[R["m1"]])
                P.tt("dve", m2[:], psb[1][:, :TT], g_[:, 1, :], ALU.mult, r=[R["ps1"], gr], w=[R["m2"]])
                P.tt("pool", m1[:], m1[:], m2[:], ALU.add, r=[R["m1"], R["m2"]], w=[R["m1"]])
                P.tt("dve", m2[:], psb[2][:, :TT], g_[:, 2, :], ALU.mult, r=[R["ps2"], gr, R["m2"]], w=[R["m2"]])
                P.tt("pool", mrg[:, oc, :], m1[:], m2[:], ALU.add, r=[R["m1"], R["m2"]], w=[R["mrg"]])
            for oc in range(8):
                for k in range(8):
                    P.mm(psm[:, :TT], wts["w_out"][:, k, oc * 128:(oc + 1) * 128], mrg[:, k, :],
                         start=(k == 0), stop=(k == 7), r=[R["w_w_out"], R["mrg"]], w=[R["psm"]])
                P.stt(x1[:, oc, :], psm[:, :TT], pt[:, 2, oc:oc + 1], xt[:, oc, :], ALU.mult, ALU.add,
                      r=[R["psm"], R["xt"]], w=[R["xt1"]])
            P.dma(x1v[:, :, ts_], x1[:], r=[R["xt1"]], w=[P.region()])
            emit_norm_tile(C, R, x1[:], lambda k: h2f[:, k, :], pt[:, 3, :], pt[:, 4, :], cst["ones"], sq, rs, tmp,
                           ps_s, TT, cst["eps"], "1")
            P.cp("pool", h2b[:], h2f[:], r=[R["hT"]], w=[R["h2b"]])
            P.dma(h2v[:, :, ts_], h2b[:], r=[R["h2b"]], w=[P.region()])
            for sub in range(TT // 128):
                for k in range(8):
                    P.mm(psl[:, 0:32], h2f[:, k, sub * 128:(sub + 1) * 128], wr[:, k, :], start=(k == 0), stop=(k == 7),
                         r=[R["hT"], R["wr"]], w=[R["psl"]])
                P.tt("dve", lg[:], psl[:, 0:32], brt[:], ALU.add, r=[R["psl"], R["br"]], w=[R["lg"]])
                P.op("dve", lambda e: e.max(out=m8[:], in_=lg[:]), r=[R["lg"]], w=[R["m8"]])
                P.ts("dve", nmx[:], m8[:, 0:1], -1.0, None, op0=ALU.mult, r=[R["m8"]], w=[R["nmx"]])
                P.ts("dve", msk[:], lg[:], m8[:, 3:4], None, op0=ALU.is_ge, r=[R["lg"], R["m8"]], w=[R["msk"]])
                P.act(ex[:], lg[:], AF.Exp, bias=nmx[:], r=[R["lg"], R["nmx"]], w=[R["ex"]])
                P.tt("dve", ex[:], ex[:], msk[:], ALU.mult, r=[R["ex"], R["msk"]], w=[R["ex"]])
                P.op("dve", lambda e: e.tensor_reduce(out=ssum[:], in_=ex[:], axis=AX.X, op=ALU.add),
                     r=[R["ex"]], w=[R["ssum"]])
                P.op("dve", lambda e: e.reciprocal(out=ssum[:], in_=ssum[:]), r=[R["ssum"]], w=[R["ssum"]])
                ro = rwo[sub % 2]
                P.ts("dve", ro[:], ex[:], ssum[:, 0:1], None, op0=ALU.mult, r=[R["ex"], R["ssum"]], w=[R[f"rwo{sub % 2}"]])
                t0 = ti * TT + sub * 128
                P.dma(scr["rwt"][t0:t0 + 128, :], ro[:], r=[R[f"rwo{sub % 2}"]], w=[P.region()])
    P.end_phase()


def phase_moe(C, l, cst, Wl, per, scr, nfin_d, out_d):
    P, nc, S = C.P, C.nc, C.S
    import os
    NE = int(os.environ.get("MOE_NE", "32"))
    ST = min(int(os.environ.get("MOE_ST", "512")), S)
    NTB = ST // 128
    with contextlib.ExitStack() as st:
        C.stack = st
        R = C.regs("moe_")
        pt = per["t"]
        gub = scr["gub"]
        dnb = scr["dnb"]
        for e in range(NE):
            P.dma(gub[e], Wl["w_gu"][e], w=[P.region()], q="pool")
            P.dma(dnb[e], Wl["w_down"][e], w=[P.region()], q="pool")
        P.end_phase()
        C.stack = st
        h2 = C.sb("moe_h2", [128, 8, ST], BF16)
        yacc = C.sb("moe_yacc", [128, NTB, 1024])
        wgu = [C.sb(f"moe_wgu{i}", [128, 8, 2048], BF16) for i in range(2)]
        wdn = [C.sb(f"moe_wdn{i}", [128, 8, 1024], BF16) for i in range(2)]
        actT = [C.sb(f"moe_act{i}", [128, 8, 512], BF16) for i in range(2)]
        bgu = C.sb("moe_bgu", [128, 32, 16])
        bdn = C.sb("moe_bdn", [32, 1024])
        rwt = C.sb("moe_rwt", [128, NTB, 32])
        rwT = C.sb("moe_rwT", [32, 128])
        g_t = [C.sb(f"moe_g{i}", [128, 512]) for i in range(2)]
        u_t = [C.sb(f"moe_u{i}", [128, 512]) for i in range(2)]
        s_t = [C.sb(f"moe_s{i}", [128, 512]) for i in range(2)]
        x1t = C.sb("moe_x1", [128, 8, 128])
        x2t = C.sb("moe_x2", [128, 8, 128])
        nfin = C.sb("moe_nfin", [128, 8])
        sq = C.sb("moe_sq", [128, 8, 128])
        rs = C.sb("moe_rs", [128, 128])
        o_t = sq
        psg = [C.ps(f"moe_psg{i}", [128, 512]) for i in range(2)]
        psu = [C.ps(f"moe_psu{i}", [128, 512]) for i in range(2)]
        psd = [C.ps(f"moe_psd{i}", [128, 512]) for i in range(2)]
        psx = C.ps("moe_psx", [128, 1024])
        for k_ in ("psg0", "psg1", "psu0", "psu1", "psd0", "psd1", "psx"):
            R[k_].excl = True
        P.dma(bgu[:], Wl["b_gu"][:, :, :], w=[R["bgu"]])
        P.dma(bdn[:], Wl["b_down"][:, :], w=[R["bdn"]])
        if nfin_d is not None:
            P.dma(nfin[:], nfin_d[:, :], w=[R["nfin"]])
        h2v = scr["h2T"].rearrange("(k p) s -> p k s", p=128)
        x1v = scr["x1T"].rearrange("(k p) s -> p k s", p=128)
        ov = out_d.rearrange("(k p) s -> p k s", p=128)
        rwv_ = scr["rwt"].rearrange("(n p) e -> p n e", p=128)
        wi = 0
        ci = 0
        for si in range(S // ST):
            s0 = si * ST
            P.dma(h2[:], h2v[:, :, s0:s0 + ST], w=[R["h2"]])
            P.dma(rwt[:], rwv_[:, si * NTB:(si + 1) * NTB, :], w=[R["rwt"]])
            for tb in range(NTB):
                P.tr(psx[0:32, 0:128], rwt[:, tb, :], cst["ident"], r=[R["rwt"]], w=[R["psx"]])
                P.cp("dve", rwT[:], psx[0:32, 0:128], r=[R["psx"]], w=[R["rwT"]])
                for dh in range(2):
                    P.mm(psd[dh][:], rwT[:], bdn[:, dh * 512:(dh + 1) * 512], r=[R["rwT"], R["bdn"]], w=[R[f"psd{dh}"]])
                    P.cp("act", yacc[:, tb, dh * 512:(dh + 1) * 512], psd[dh][:], r=[R[f"psd{dh}"]], w=[R[f"yacc{tb}"]])
            for e in range(NE):
                wg, wd = wgu[wi % 2], wdn[wi % 2]
                wgr, wdr = R[f"wgu{wi % 2}"], R[f"wdn{wi % 2}"]
                wi += 1
                P.dma(wg[:], gub[e].rearrange("(k p) j -> p k j", p=128), w=[wgr])
                P.dma(wd[:], dnb[e].rearrange("(k p) j -> p k j", p=128), w=[wdr])
                for tt_ in range(ST // 512):
                    tsl = slice(tt_ * 512, (tt_ + 1) * 512)
                    at, atr = actT[ci % 2], R[f"act{ci % 2}"]
                    ci += 1
                    def fin(fc_):
                        j2 = fc_ % 2
                        P.stt(at[:, fc_, :], s_t[j2][:], 1.0 / 1.702, u_t[j2][:], ALU.mult, ALU.mult,
                              r=[R[f"s{j2}"], R[f"u{j2}"]], w=[atr])
                    for fc in range(8):
                        i2 = fc % 2
                        for k in range(8):
                            P.mm(psg[i2][:], wg[:, k, fc * 128:(fc + 1) * 128], h2[:, k, tsl], start=(k == 0), stop=(k == 7),
                                 r=[wgr, R["h2"]], w=[R[f"psg{i2}"]])
                        for k in range(8):
                            P.mm(psu[i2][:], wg[:, k, 1024 + fc * 128:1024 + (fc + 1) * 128], h2[:, k, tsl],
                                 start=(k == 0), stop=(k == 7), r=[wgr, R["h2"]], w=[R[f"psu{i2}"]])
                        P.ts("dve", g_t[i2][:], psg[i2][:], bgu[:, e, fc:fc + 1], 7.0, op0=ALU.add, op1=ALU.min,
                             r=[R[f"psg{i2}"], R["bgu"]], w=[R[f"g{i2}"]])
                        P.act(s_t[i2][:], g_t[i2][:], AF.Silu, scale=1.702, r=[R[f"g{i2}"]], w=[R[f"s{i2}"]])
                        P.ts("dve", u_t[i2][:], psu[i2][:], bgu[:, e, 8 + fc:9 + fc], 7.0, op0=ALU.add, op1=ALU.min,
                             r=[R[f"psu{i2}"], R["bgu"]], w=[R[f"u{i2}"]])
                        P.ts("dve", u_t[i2][:], u_t[i2][:], -7.0, 1.0, op0=ALU.max, op1=ALU.add, r=[R[f"u{i2}"]], w=[R[f"u{i2}"]])
                        if fc >= 1:
                            fin(fc - 1)
                    fin(7)
                    for sub in range(4):
                        tb = tt_ * 4 + sub
                        for dh in range(2):
                            for fc in range(8):
                                P.mm(psd[dh][:], at[:, fc, sub * 128:(sub + 1) * 128], wd[:, fc, dh * 512:(dh + 1) * 512],
                                     start=(fc == 0), stop=(fc == 7), r=[atr, wdr], w=[R[f"psd{dh}"]])
                            P.stt(yacc[:, tb, dh * 512:(dh + 1) * 512], psd[dh][:], rwt[:, tb, e:e + 1],
                                  yacc[:, tb, dh * 512:(dh + 1) * 512], ALU.mult, ALU.add,
                                  r=[R[f"psd{dh}"], R["rwt"], R[f"yacc{tb}"]], w=[R[f"yacc{tb}"]])
            for tb in range(NTB):
                t0 = s0 + tb * 128
                P.dma(x1t[:], x1v[:, :, t0:t0 + 128], w=[R["x1t"]])
                for k in range(8):
                    P.tr(psx[:, k * 128:(k + 1) * 128], yacc[:, tb, k * 128:(k + 1) * 128], cst["ident"],
                         r=[R[f"yacc{tb}"]], w=[R["psx"]])
                for k in range(8):
                    P.stt(x2t[:, k, :], psx[:, k * 128:(k + 1) * 128], pt[:, 5, k:k + 1], x1t[:, k, :], ALU.mult, ALU.add,
                          r=[R["psx"], R["x1t"]], w=[R["x2t"]])
                if nfin_d is None:
                    P.dma(ov[:, :, t0:t0 + 128], x2t[:], r=[R["x2t"]], w=[P.region()])
                else:
                    P.act(sq[:], x2t[:], AF.Square, r=[R["x2t"]], w=[R["sq"]])
                    for k in range(8):
                        P.mm(psd[0][:, :128], cst["ones"], sq[:, k, :], start=(k == 0), stop=(k == 7), r=[R["sq"]], w=[R["psd0"]])
                    P.act(rs[:], psd[0][:, :128], AF.Ln, scale=1.0 / 1024, bias=cst["eps"], r=[R["psd0"]], w=[R["rs"]])
                    P.act(rs[:], rs[:], AF.Exp, scale=-0.5, r=[R["rs"]], w=[R["rs"]])
                    for k in range(8):
                        P.stt(o_t[:, k, :], x2t[:, k, :], nfin[:, k:k + 1], rs[:], ALU.mult, ALU.mult,
                              r=[R["x2t"], R["rs"], R["nfin"], R["sq"]], w=[R["sq"]])
                    P.dma(ov[:, :, t0:t0 + 128], o_t[:], r=[R["sq"]], w=[P.region()], is_out=True)
    P.end_phase()
from concourse.bass_utils import run_bass_kernel_spmd

LAYER_W = ["w_ada", "b_ada", "norm_mix", "norm_moe", "w_in", "conv_w", "conv_b", "lru_wa", "lru_ba", "lru_wx",
           "lru_bx", "lru_lambda", "rw_mu", "rw_w0", "rw_w_up", "rw_a0", "rw_a_up", "rw_g_up", "rw_k_k", "rw_k_a",
           "rw_r_k", "rw_lnx_w", "rw_lnx_b", "p_lru", "p_sb", "p_rwkv", "w_out", "w_router", "b_router",
           "w_gu", "b_gu", "w_down", "b_down"]


def fm(v):
    v = np.asarray(v, np.float32).reshape(-1, 128)
    return np.ascontiguousarray(v.T)


def bc(v):
    v = np.asarray(v, np.float32).reshape(1, -1)
    return np.ascontiguousarray(np.broadcast_to(v, (128, v.shape[1])))


def layer_inputs(inp, l):
    g = lambda n: np.asarray(inp[n][l], np.float32)
    d = {}
    d["w_ada"] = g("w_ada")
    d["b_ada"] = fm(g("b_ada"))
    d["norm_mix"] = fm(g("norm_mix"))
    d["norm_moe"] = fm(g("norm_moe"))
    d["w_in"] = g("w_in")
    d["mu"] = bc(g("rw_mu"))
    cw = g("conv_w")
    vecs = [cw[0], cw[1], cw[2], cw[3], g("conv_b"), g("lru_ba"), g("lru_bx"), g("lru_lambda")]
    d["lruv"] = np.ascontiguousarray(np.stack([fm(v) for v in vecs], axis=1))
    for nm in ("lru_wa", "lru_wx"):
        w = g(nm)
        bd = np.zeros((128, 8, 128), np.float32)
        for c in range(8):
            bd[0:64, c, 0:64] = w[2 * c]
            bd[64:128, c, 64:128] = w[2 * c + 1]
        d[nm] = bd
    d["rwv"] = np.ascontiguousarray(np.stack(
        [bc(g(n).reshape(-1)) for n in ("rw_w0", "rw_a0", "rw_k_k", "rw_k_a", "rw_r_k", "rw_lnx_w", "rw_lnx_b")], axis=1))
    d["rw_w_up"] = g("rw_w_up")
    d["rw_a_up"] = g("rw_a_up")
    d["rw_g_up"] = g("rw_g_up")
    for nm in ("p_lru", "p_sb", "p_rwkv", "w_out", "w_router", "w_gu", "w_down", "b_down"):
        d[nm] = g(nm)
    d["b_router"] = bc(g("b_router"))
    bg = g("b_gu")
    d["b_gu"] = np.ascontiguousarray(bg.reshape(32, 16, 128).transpose(2, 0, 1))
    return d


LAYER_SHAPES = {
    "w_ada": (1024, 6144), "b_ada": (128, 48), "norm_mix": (128, 8), "norm_moe": (128, 8), "w_in": (1024, 11520),
    "mu": (128, 3328), "lruv": (128, 8, 8), "lru_wa": (128, 8, 128), "lru_wx": (128, 8, 128),
    "rwv": (128, 7, 1024), "rw_w_up": (64, 1024), "rw_a_up": (64, 1024), "rw_g_up": (128, 1024),
    "p_lru": (1024, 1024), "p_sb": (1024, 1024), "p_rwkv": (1024, 1024), "w_out": (1024, 1024),
    "w_router": (1024, 32), "w_gu": (32, 1024, 2048), "w_down": (32, 1024, 1024), "b_down": (32, 1024),
    "b_router": (128, 32), "b_gu": (128, 32, 16),
}


def build(S, depth, debug=False, phases=None):
    nc = bass.Bass("TRN2", target_bir_lowering=False)
    P = Prog(nc)
    C = Ctx(nc, P, S, debug)
    cst_np, ccols = make_consts()
    NCC = cst_np.shape[1]
    xT_d = C.din("xT", [1024, S])
    cvec_d = C.din("cvec", [128, 8])
    nfin_d = C.din("norm_final", [128, 8])
    cst_d = C.din("consts", [128, NCC])
    class LazyW(dict):
        def __init__(self, l):
            super().__init__()
            self.l = l

        def __missing__(self, n):
            ap = C.din(f"{n}_{self.l}", LAYER_SHAPES[n])
            self[n] = ap
            C.declared.add(f"{n}_{self.l}")
            return ap
    C.declared = set()
    W = [LazyW(l) for l in range(depth)]
    out_d = C.dout("outT", [1024, S])
    scr = {
        "lruT": C.dscr("s_lruT", [2048, S], F32),
        "qT": C.dscr("s_qT", [1024, S], BF16),
        "kT": C.dscr("s_kT", [1024, S], BF16),
        "v": C.dscr("s_v", [S, 1024], BF16),
        "rkv": C.dscr("s_rkv", [S, 3072], F32),
        "loraT": C.dscr("s_loraT", [256, S], BF16),
        "gatesT": C.dscr("s_gatesT", [3072, S], F32),
        "yaT": C.dscr("s_yaT", [1024, S], BF16),
        "ybT": C.dscr("s_ybT", [1024, S], BF16),
        "ycT": C.dscr("s_ycT", [1024, S], BF16),
        "x1T": C.dscr("s_x1T", [1024, S], F32),
        "x2T": C.dscr("s_x2T", [1024, S], F32),
        "h2T": C.dscr("s_h2T", [1024, S], BF16),
        "rwt": C.dscr("s_rwt", [S, 32], F32),
        "gub": nc.dram_tensor("s_gub", [32, 1024, 2048], BF16, kind="Internal").ap(),
        "dnb": nc.dram_tensor("s_dnb", [32, 1024, 1024], BF16, kind="Internal").ap(),
    }
    with contextlib.ExitStack() as top:
        C.stack = top
        so, sw = ccols.pop("__sbm__")
        NCA = so
        cst_t = top.enter_context(nc.sbuf_tensor("cst", [128, NCA], F32))
        cstb_t = top.enter_context(nc.sbuf_tensor("cstb", [128, NCA], BF16))
        sbm_t = top.enter_context(nc.sbuf_tensor("sbm", [128, sw], BF16))
        eps_t = top.enter_context(nc.sbuf_tensor("eps", [128, 1], F32))
        per_t = top.enter_context(nc.sbuf_tensor("per", [128, 6, 8], F32))
        R0 = C.regs("init_")
        with contextlib.ExitStack() as st0:
            tmpc = st0.enter_context(nc.sbuf_tensor("cst_tmp", [128, sw], F32))
            P.dma(cst_t[:], cst_d[:, 0:NCA], w=[R0["cst"]])
            P.cp("dve", cstb_t[:], cst_t[:], r=[R0["cst"]], w=[R0["cstb"]])
            P.dma(tmpc[:], cst_d[:, so:so + sw], w=[R0["tmpc"]])
            P.cp("dve", sbm_t[:], tmpc[:], r=[R0["tmpc"]], w=[R0["sbm"]])
            P.memset("pool", eps_t[:], EPS, w=[R0["eps"]])
            P.end_phase()
        cst = {"eps": eps_t[:], "sbmask_b": sbm_t[:]}
        for n, (o, wd) in ccols.items():
            cst[n] = cst_t[:, o:o + wd]
            cst[n + "_b"] = cstb_t[:, o:o + wd]
        per = {"t": per_t, "reg": P.region("per", persistent=True)}
        x_in = xT_d
        for l in range(depth):
            Wl = W[l]
            if phases is None or "ada" in phases:
                phase_adaln(C, l, cst, cvec_d, Wl["w_ada"], Wl["b_ada"], Wl["norm_mix"], Wl["norm_moe"], per)
            if phases is None or "proj" in phases:
                phase_proj(C, l, cst, x_in, Wl["w_in"], Wl["mu"], per, scr)
            if phases is None or "lru" in phases:
                phase_lru(C, l, cst, Wl, scr)
            if phases is None or "sb" in phases:
                phase_sb(C, l, cst, scr)
            if phases is None or "rwkv" in phases:
                phase_rwkv(C, l, cst, Wl, scr)
            if phases is None or "merge" in phases:
                phase_merge(C, l, cst, Wl, x_in, per, scr)
            if phases is None or "moe" in phases:
                last = (l == depth - 1)
                phase_moe(C, l, cst, Wl, per, scr, nfin_d if last else None, out_d if last else scr["x2T"])
            x_in = scr["x2T"]
        if phases is not None and "moe" not in phases:
            with contextlib.ExitStack() as st:
                C.stack = st
                z = C.sb("zz", [128, 8])
                rz = P.region()
                P.memset("pool", z[:], 0.0, w=[rz])
                P.dma(out_d[0:128, 0:8], z[:], r=[rz], w=[P.region()], is_out=True)
        P.emit()
    nc.declared_inputs = set(C.declared)
    return nc, cst_np


def core_inputs(inp, b, S, depth, cst_np, layer_cache, declared=None):
    m = {"xT": np.ascontiguousarray(np.asarray(inp["x"][b, :S], np.float32).T),
         "cvec": fm(np.asarray(inp["c"][b], np.float32)),
         "norm_final": fm(np.asarray(inp["norm_final"], np.float32)),
         "consts": cst_np}
    for l in range(depth):
        for n, a in layer_cache[l].items():
            if declared is None or f"{n}_{l}" in declared:
                m[f"{n}_{l}"] = a
    return m


N_ACTIVE = 4


def kernel(**inputs):
    S, depth, B = 8192, 2, 4
    nc, cst_np = build(S, depth)
    lc = [layer_inputs(inputs, l) for l in range(depth)]
    in_maps = [core_inputs(inputs, b, S, depth, cst_np, lc) for b in range(B)]
    res = run_bass_kernel_spmd(nc, in_maps, core_ids=list(range(B)))
    out = np.stack([np.asarray(res.results[b]["outT"], np.float32).T for b in range(B)], axis=0)
    return np.ascontiguousarray(out)
prog_extend(Prog)
```

```python
import concourse.bass as bass
import concourse.mybir as mybir

F32 = mybir.dt.float32
BF16 = mybir.dt.bfloat16
ALU = mybir.AluOpType
AF = mybir.ActivationFunctionType
AX = mybir.AxisListType


class Region:
    __slots__ = ("w", "rs", "name", "excl")

    def __init__(self, name=""):
        self.w = None
        self.rs = {}
        self.name = name
        self.excl = False


class Ins:
    __slots__ = ("eng", "fn", "deps", "sig", "val", "dma", "dsem", "dval", "dprev")

    def __init__(self, eng, fn, dma=False):
        self.eng = eng
        self.fn = fn
        self.deps = []
        self.sig = False
        self.val = 0
        self.dma = dma
        self.dsem = None
        self.dval = 0
        self.dprev = 0


class Prog:
    ENGS = ("pe", "act", "dve", "pool", "sp")
    NDMA = 10

    def __init__(self, nc):
        self.nc = nc
        self.q = {e: [] for e in self.ENGS}
        self.dcount = {"sp": 0, "pool": 0, "act": 0}
        self.out_dmas = []

    def op(self, eng, fn, r=(), w=(), dma=False):
        ins = Ins(eng, fn, dma)
        deps = {}

        def add(d):
            if d is None or d is ins:
                return
            if (not d.dma) and (not dma) and d.eng == "pe" and eng == "pe":
                return
            deps[id(d)] = d

        for reg in r:
            add(reg.w)
            if reg.excl:
                for k, v in reg.rs.items():
                    if k != eng and k != "dma":
                        add(v)
        for reg in w:
            add(reg.w)
            for k, v in reg.rs.items():
                if k == "dma":
                    for d in v:
                        add(d)
                else:
                    add(v)
        ins.deps = list(deps.values())
        for d in ins.deps:
            d.sig = True
        for reg in r:
            if dma:
                reg.rs.setdefault("dma", []).append(ins)
            else:
                reg.rs[eng] = ins
        for reg in w:
            reg.w = ins
            reg.rs = {}
        if dma:
            i = self.dcount[eng]
            self.dcount[eng] = i + 1
            ins.dsem = (eng, i % self.NDMA)
            ins.dval = 16 * (i // self.NDMA + 1)
            ins.dprev = 16 * (i // self.NDMA)
        self.q[eng].append(ins)
        return ins

    def mm(self, out, lhsT, rhs, start=True, stop=True, r=(), w=(), **kw):
        return self.op("pe", lambda e: e.matmul(out, lhsT, rhs, start=start, stop=stop, **kw), r, w)

    def tr(self, out, in_, ident, r=(), w=()):
        return self.op("pe", lambda e: e.transpose(out, in_, ident), r, w)

    def act(self, out, in_, func, r=(), w=(), **kw):
        return self.op("act", lambda e: e.activation(out, in_, func, **kw), r, w)

    def dma(self, out, in_, r=(), w=(), q="sp", is_out=False, **kw):
        ins = self.op(q, lambda e: e.dma_start(out=out, in_=in_, **kw), r, w, dma=True)
        if is_out:
            self.out_dmas.append(ins)
        return ins

    def emit(self):
        nc = self.nc
        for e in self.ENGS:
            c = 0
            for ins in self.q[e]:
                if ins.sig and not ins.dma:
                    c += 1
                    ins.val = c
        import contextlib
        with contextlib.ExitStack() as st:
            esem = {e: st.enter_context(nc.semaphore("es_" + e)) for e in self.ENGS}
            dsem = {}
            for qn in ("sp", "pool"):
                for i in range(self.NDMA):
                    dsem[(qn, i)] = st.enter_context(nc.semaphore(f"ds_{qn}{i}"))
            block = st.enter_context(nc.Block())
            final = self.out_dmas

            def body(ename, eng):
                waited = {}

                def wait(key, sem, val):
                    if val <= 0:
                        return
                    if waited.get(key, 0) >= val:
                        return
                    eng.wait_ge(sem, val)
                    waited[key] = val

                for ins in self.q[ename]:
                    need = {}
                    for d in ins.deps:
                        if d.dma:
                            key, sem, val = d.dsem, dsem[d.dsem], d.dval
                        else:
                            key, sem, val = d.eng, esem[d.eng], d.val
                        if key not in need or need[key][1] < val:
                            need[key] = (sem, val)
                    for key, (sem, val) in need.items():
                        wait(key, sem, val)
                    if ins.dma:
                        wait(ins.dsem, dsem[ins.dsem], ins.dprev)
                    i = ins.fn(eng)
                    if ins.dma:
                        i.then_inc(dsem[ins.dsem], 16)
                    elif ins.sig:
                        i.then_inc(esem[ename], 1)
                if ename == "sp":
                    for d in final:
                        wait(d.dsem, dsem[d.dsem], d.dval)

            @block.tensor
            def _(e):
                body("pe", e)

            @block.scalar
            def _(e):
                body("act", e)

            @block.vector
            def _(e):
                body("dve", e)

            @block.gpsimd
            def _(e):
                body("pool", e)

            @block.sync
            def _(e):
                body("sp", e)
import contextlib
import numpy as np

D = 1024
KC = 8
N_IN = 11520
EPS = 1e-6


class RegMap(dict):
    def __init__(self, P, name, persistent=False):
        super().__init__()
        self.P = P
        self.name = name
        self.persistent = persistent

    def __missing__(self, k):
        r = self.P.region(f"{self.name}{k}", self.persistent)
        self[k] = r
        return r


class Ctx:
    def __init__(self, nc, P, S, debug):
        self.nc = nc
        self.P = P
        self.S = S
        self.debug = debug
        self.dram_regs = {}
        self.stack = None

    def din(self, name, shape, dt=F32):
        return self.nc.dram_tensor(name, list(shape), dt, kind="ExternalInput").ap()

    def dout(self, name, shape, dt=F32):
        return self.nc.dram_tensor(name, list(shape), dt, kind="ExternalOutput").ap()

    def dscr(self, name, shape, dt):
        kind = "ExternalOutput" if self.debug else "Internal"
        return self.nc.dram_tensor(name, list(shape), dt, kind=kind).ap()

    def sb(self, name, shape, dt=F32):
        self.uid = getattr(self, "uid", 0) + 1
        return self.stack.enter_context(self.nc.sbuf_tensor(f"{name}_{self.uid}", list(shape), dt))

    def ps(self, name, shape, dt=F32):
        self.uid = getattr(self, "uid", 0) + 1
        return self.stack.enter_context(self.nc.psum_tensor(f"{name}_{self.uid}", list(shape), dt))

    def regs(self, name, persistent=False):
        return RegMap(self.P, name, persistent)


def prog_extend(Prog):
    def region(self, name="", persistent=False):
        r = Region(name)
        if not persistent:
            r.w = self.cur_bar
            self.phase_regions.append(r)
        return r

    def end_phase(self):
        bar = self.op("sp", lambda e: e.nop(), w=list(self.phase_regions))
        self.cur_bar = bar
        self.phase_regions = []

    def tt(self, eng, out, in0, in1, op, r=(), w=()):
        return self.op(eng, lambda e: e.tensor_tensor(out=out, in0=in0, in1=in1, op=op), r, w)

    def ts(self, eng, out, in0, s1, s2=None, op0=ALU.mult, op1=None, r=(), w=()):
        if op1 is None:
            return self.op(eng, lambda e: e.tensor_scalar(out=out, in0=in0, scalar1=s1, scalar2=None, op0=op0), r, w)
        return self.op(eng, lambda e: e.tensor_scalar(out=out, in0=in0, scalar1=s1, scalar2=s2, op0=op0, op1=op1), r, w)

    def stt(self, out, in0, scalar, in1, op0, op1, r=(), w=()):
        return self.op("dve", lambda e: e.scalar_tensor_tensor(out=out, in0=in0, scalar=scalar, in1=in1, op0=op0, op1=op1), r, w)

    def cp(self, eng, out, in_, r=(), w=()):
        if eng == "act":
            return self.op(eng, lambda e: e.activation(out, in_, AF.Copy), r, w)
        return self.op(eng, lambda e: e.tensor_copy(out=out, in_=in_), r, w)

    def memset(self, eng, ap, val, r=(), w=()):
        return self.op(eng, lambda e: e.memset(ap, val), r, w)

    Prog.region = region
    Prog.end_phase = end_phase
    Prog.tt = tt
    Prog.ts = ts
    Prog.stt = stt
    Prog.cp = cp
    Prog.memset = memset
    Prog.cur_bar = None
    Prog.phase_regions = []


def make_consts():
    cols = {}
    parts = []
    off = 0

    def add(name, arr):
        nonlocal off
        arr = np.asarray(arr, np.float32).reshape(128, -1)
        cols[name] = (off, arr.shape[1])
        parts.append(arr)
        off += arr.shape[1]

    i = np.arange(128)
    add("ident", np.eye(128))
    add("ones", np.ones((128, 128)))
    add("tri_ge", (i[:, None] >= i[None, :]))
    add("tri_lt", (i[:, None] < i[None, :]))
    add("tri_le", (i[:, None] <= i[None, :]))
    t = np.arange(512)
    m = np.stack([(128 * k + i[:, None] < t[None, :]) for k in range(4)], axis=1)
    sbm = np.asarray(m, np.float32).reshape(128, -1)
    add("m_sl", (i[None, :] < i[:, None]))
    add("m_li", (i[None, :] <= i[:, None]))
    add("m_su", (i[:, None] < i[None, :]))
    add("m_ui", (i[:, None] <= i[None, :]))
    add("mask3", np.concatenate([(i[None, :] < i[:, None]), (i[:, None] < i[None, :]), (i[:, None] < i[None, :])], axis=1))
    cols["__sbm__"] = (off, sbm.shape[1])
    parts.append(sbm)
    return np.concatenate(parts, axis=1), cols


def phase_adaln(C, l, cst, cvec_d, w_ada_d, b_ada_d, nmix_d, nmoe_d, per):
    P, nc = C.P, C.nc
    with contextlib.ExitStack() as st:
        C.stack = st
        R = C.regs("p0_")
        cact = C.sb("cact", [128, 8])
        wst = [C.sb(f"wada{i}", [128, 8, 768]) for i in range(2)]
        ada = C.sb("ada", [128, 48])
        bada = C.sb("bada", [128, 48])
        nm = C.sb("nm", [128, 16])
        psa = C.ps("psa", [128, 48])
        P.dma(cact[:], cvec_d[:, :], w=[R["cact"]])
        P.dma(bada[:], b_ada_d[:, :], w=[R["bada"]])
        P.dma(nm[:, 0:8], nmix_d[:, :], w=[R["nm"]])
        P.dma(nm[:, 8:16], nmoe_d[:, :], w=[R["nm"]])
        P.act(cact[:], cact[:], AF.Silu, r=[R["cact"]], w=[R["cact"]])
        wv = w_ada_d.rearrange("(k p) j -> p k j", p=128)
        for g in range(8):
            wt = wst[g % 2]
            P.dma(wt[:], wv[:, :, g * 768:(g + 1) * 768], w=[R[f"w{g % 2}"]])
            for cc in range(6):
                j = g * 6 + cc
                for k in range(8):
                    P.mm(psa[:, j:j + 1], wt[:, k, cc * 128:(cc + 1) * 128], cact[:, k:k + 1],
                         start=(k == 0), stop=(k == 7), r=[R[f"w{g % 2}"], R["cact"]], w=[R["psa"]])
        P.tt("dve", ada[:], psa[:], bada[:], ALU.add, r=[R["psa"], R["bada"]], w=[R["ada"]])
        Rp = per["reg"]
        pt = per["t"]
        for (dst, sc_i, nofs) in ((0, 1, 0), (3, 4, 8)):
            P.ts("dve", pt[:, dst, :], ada[:, sc_i * 8:(sc_i + 1) * 8], 1.0, None, op0=ALU.add, r=[R["ada"]], w=[Rp])
            P.tt("dve", pt[:, dst, :], pt[:, dst, :], nm[:, nofs:nofs + 8], ALU.mult, r=[Rp, R["nm"]], w=[Rp])
        for (dst, src) in ((1, 0), (2, 2), (4, 3), (5, 5)):
            P.cp("dve", pt[:, dst, :], ada[:, src * 8:(src + 1) * 8], r=[R["ada"]], w=[Rp])
    P.end_phase()


def emit_norm_tile(C, R, xt, hT_out_fn, scale_ap, shift_ap, ones_f, sq, rs, tmp, ps_s, TT, eps_ap, tag):
    P = C.P
    P.act(sq[:], xt, AF.Square, r=[R["xt" + tag]], w=[R["sq"]])
    for k in range(8):
        P.mm(ps_s[:, :TT], ones_f, sq[:, k, :], start=(k == 0), stop=(k == 7), r=[R["sq"]], w=[R["ps_s"]])
    P.act(rs[:], ps_s[:, :TT], AF.Ln, scale=1.0 / 1024, bias=eps_ap, r=[R["ps_s"]], w=[R["rs"]])
    P.act(rs[:], rs[:], AF.Exp, scale=-0.5, r=[R["rs"]], w=[R["rs"]])
    for k in range(8):
        if shift_ap is None:
            P.stt(hT_out_fn(k), xt[:, k, :], scale_ap[:, k:k + 1], rs[:], ALU.mult, ALU.mult,
                  r=[R["xt" + tag], R["rs"]], w=[R["hT"]])
        else:
            P.stt(tmp[:, k, :], xt[:, k, :], scale_ap[:, k:k + 1], rs[:], ALU.mult, ALU.mult,
                  r=[R["xt" + tag], R["rs"]], w=[R["tmp"]])
            P.ts("pool", hT_out_fn(k), tmp[:, k, :], shift_ap[:, k:k + 1], None, op0=ALU.add,
                 r=[R["tmp"]], w=[R["hT"]])


def phase_proj(C, l, cst, xT_d, w_in_d, mu_d, per, scr):
    P, nc, S = C.P, C.nc, C.S
    TT = 256
    with contextlib.ExitStack() as st:
        C.stack = st
        R = C.regs("p1_")
        import os
        TOK = min(int(os.environ.get("PROJ_TOK", "4096")), S)
        hT = C.sb("hT", [128, 8, TOK + 1], BF16)
        off = {"p0": 0}
        xb = [C.sb(f"xb{i}", [128, 8, TT]) for i in range(2)]
        sq = C.sb("sq", [128, 8, TT])
        tmp = C.sb("tmp", [128, 8, TT])
        rs = C.sb("rs", [128, TT])
        ps_s = C.ps("ps_s", [128, 512])
        pt = per["t"]
        xv = xT_d.rearrange("(k p) s -> p k s", p=128)
        def norm_pass(p0):
            if p0 == 0:
                P.memset("pool", hT[:, :, 0:1], 0.0, w=[R["hT"]])
            else:
                P.cp("dve", hT[:, :, 0:1], hT[:, :, TOK:TOK + 1], r=[R["hT"]], w=[R["hT"]])
            for ti in range(TOK // TT):
                xt = xb[ti % 2]
                tag = str(ti % 2)
                P.dma(xt[:], xv[:, :, p0 + ti * TT:p0 + (ti + 1) * TT], w=[R["xt" + tag]])
                emit_norm_tile(C, R, xt[:], lambda k: hT[:, k, 1 + ti * TT:1 + (ti + 1) * TT],
                               pt[:, 0, :], pt[:, 1, :], cst["ones"], sq, rs, tmp, ps_s, TT, cst["eps"], tag)
        wst = [C.sb(f"wst{i}", [128, 8, 512]) for i in range(2)]
        wb = [C.sb(f"wb{i}", [128, 8, 512], BF16) for i in range(2)]
        wb2 = [C.sb(f"wb2{i}", [128, 8, 512], BF16) for i in range(2)]
        mug = [C.sb(f"mug{i}", [128, 512]) for i in range(2)]
        omug = [C.sb(f"omug{i}", [128, 512]) for i in range(2)]
        ev = [C.sb(f"ev{i}", [128, 512]) for i in range(4)]
        evb = [C.sb(f"evb{i}", [128, 512], BF16) for i in range(4)]
        pso = [C.ps(f"pso{i}", [128, 512]) for i in range(6)]
        wv = w_in_d.rearrange("(k p) j -> p k j", p=128)
        cnt = {"g": 0, "ps": 0, "ev": 0}
        NT5 = TOK // 512
        NT1 = TOK // 128

        def load_group(c0, width, rw):
            g = cnt["g"] % 2
            cnt["g"] += 1
            P.dma(wst[g][:, :, :width], wv[:, :, c0:c0 + width], w=[R[f"wst{g}"]])
            if not rw:
                P.cp("pool", wb[g][:, :, :width], wst[g][:, :, :width], r=[R[f"wst{g}"]], w=[R[f"wb{g}"]])
                return wb[g], None, [R[f"wb{g}"]]
            m0 = c0 - 5120
            P.dma(mug[g][:, :width], mu_d[:, m0:m0 + width], w=[R[f"mug{g}"]])
            P.ts("pool", omug[g][:, :width], mug[g][:, :width], -1.0, 1.0, op0=ALU.mult, op1=ALU.add,
                 r=[R[f"mug{g}"]], w=[R[f"omug{g}"]])
            for k in range(8):
                P.tt("pool" if k % 2 else "dve", wb[g][:, k, :width], wst[g][:, k, :width], omug[g][:, :width], ALU.mult,
                     r=[R[f"wst{g}"], R[f"omug{g}"]], w=[R[f"wb{g}"]])
                P.tt("dve" if k % 2 else "pool", wb2[g][:, k, :width], wst[g][:, k, :width], mug[g][:, :width], ALU.mult,
                     r=[R[f"wst{g}"], R[f"mug{g}"]], w=[R[f"wb2{g}"]])
            return wb[g], wb2[g], [R[f"wb{g}"], R[f"wb2{g}"]]

        def next_ps():
            i = cnt["ps"] % 6
            cnt["ps"] += 1
            return pso[i], R[f"pso{i}"]

        def next_ev(bf):
            i = cnt["ev"] % 4
            cnt["ev"] += 1
            return (evb[i], R[f"evb{i}"]) if bf else (ev[i], R[f"ev{i}"])

        def fm_group(c0, width, rw, evac):
            w1, w2, wr = load_group(c0, width, rw)
            for cc in range(width // 128):
                for tt_ in range(NT5):
                    pt_, pr = next_ps()
                    n = 16 if rw else 8
                    for k in range(8):
                        P.mm(pt_[:], w1[:, k, cc * 128:(cc + 1) * 128], hT[:, k, 1 + tt_ * 512:1 + (tt_ + 1) * 512],
                             start=(k == 0), stop=(k == 7 and not rw), r=wr + [R["hT"]], w=[pr])
                    if rw:
                        for k in range(8):
                            P.mm(pt_[:], w2[:, k, cc * 128:(cc + 1) * 128], hT[:, k, tt_ * 512:(tt_ + 1) * 512],
                                 start=False, stop=(k == 7), r=wr + [R["hT"]], w=[pr])
                    evac(c0 + cc * 128, tt_, pt_, pr)

        def tm_group(c0, width, rw, evac):
            w1, w2, wr = load_group(c0, width, rw)
            for t1 in range(NT1):
                pt_, pr = next_ps()
                for k in range(8):
                    P.mm(pt_[:, :width], hT[:, k, 1 + t1 * 128:1 + (t1 + 1) * 128], w1[:, k, :width],
                         start=(k == 0), stop=(k == 7 and not rw), r=wr + [R["hT"]], w=[pr])
                if rw:
                    for k in range(8):
                        P.mm(pt_[:, :width], hT[:, k, t1 * 128:(t1 + 1) * 128], w2[:, k, :width],
                             start=False, stop=(k == 7), r=wr + [R["hT"]], w=[pr])
                evac(c0, t1, pt_, pr)

        def ev_lru(col, tt_, pt_, pr):
            e, er = next_ev(False)
            P.cp("act", e[:], pt_[:], r=[pr], w=[er])
            P.dma(scr["lruT"][col:col + 128, off['p0'] + tt_ * 512:off['p0'] + (tt_ + 1) * 512], e[:], r=[er], w=[P.region()])

        def ev_q(col, tt_, pt_, pr):
            e, er = next_ev(True)
            P.ts("dve", e[:], pt_[:], float(128 ** -0.5), None, op0=ALU.mult, r=[pr], w=[er])
            c = col - 2048
            P.dma(scr["qT"][c:c + 128, off['p0'] + tt_ * 512:off['p0'] + (tt_ + 1) * 512], e[:], r=[er], w=[P.region()])

        def ev_k(col, tt_, pt_, pr):
            e, er = next_ev(True)
            P.cp("act", e[:], pt_[:], r=[pr], w=[er])
            c = col - 3072
            P.dma(scr["kT"][c:c + 128, off['p0'] + tt_ * 512:off['p0'] + (tt_ + 1) * 512], e[:], r=[er], w=[P.region()])

        def ev_v(c0, t1, pt_, pr):
            e, er = next_ev(True)
            P.cp("dve", e[:], pt_[:], r=[pr], w=[er])
            c = c0 - 4096
            P.dma(scr["v"][off['p0'] + t1 * 128:off['p0'] + (t1 + 1) * 128, c:c + 512], e[:], r=[er], w=[P.region()])

        def ev_rkv(c0, t1, pt_, pr):
            e, er = next_ev(False)
            P.cp("act" if t1 % 2 else "dve", e[:], pt_[:], r=[pr], w=[er])
            c = c0 - 5120
            P.dma(scr["rkv"][off['p0'] + t1 * 128:off['p0'] + (t1 + 1) * 128, c:c + 512], e[:], r=[er], w=[P.region()])

        def ev_lora(col, tt_, pt_, pr):
            e, er = next_ev(True)
            if col == 8192:
                P.act(e[0:64, :], pt_[0:64, :], AF.Tanh, r=[pr], w=[er])
                P.cp("dve", e[64:128, :], pt_[64:128, :], r=[pr], w=[er])
            else:
                P.act(e[:], pt_[:], AF.Sigmoid, r=[pr], w=[er])
            c = col - 8192
            P.dma(scr["loraT"][c:c + 128, off['p0'] + tt_ * 512:off['p0'] + (tt_ + 1) * 512], e[:], r=[er], w=[P.region()])

        def ev_gate(col, tt_, pt_, pr):
            e, er = next_ev(False)
            P.act(e[:], pt_[:], AF.Sigmoid, r=[pr], w=[er])
            c = col - 8448
            P.dma(scr["gatesT"][c:c + 128, off['p0'] + tt_ * 512:off['p0'] + (tt_ + 1) * 512], e[:], r=[er], w=[P.region()])

        for p0 in range(0, S, TOK):
          off["p0"] = p0
          norm_pass(p0)
          for c0 in range(0, 2048, 512):
            fm_group(c0, 512, False, ev_lru)
          for c0 in range(2048, 3072, 512):
              fm_group(c0, 512, False, ev_q)
          for c0 in range(3072, 4096, 512):
              fm_group(c0, 512, False, ev_k)
          for c0 in range(4096, 5120, 512):
              tm_group(c0, 512, False, ev_v)
          for c0 in range(5120, 8192, 512):
              tm_group(c0, 512, True, ev_rkv)
          fm_group(8192, 256, True, ev_lora)
          for c0 in range(8448, 11520, 512):
              fm_group(c0, 512, False, ev_gate)
    P.end_phase()
GELU_K = 1.5957691216057308


def phase_lru(C, l, cst, Wl, scr):
    P, nc, S = C.P, C.nc, C.S
    import os
    TL = min(int(os.environ.get("LRU_TL", "2048")), S)
    with contextlib.ExitStack() as st:
        C.stack = st
        R = C.regs("lru_")
        lv = C.sb("lv", [128, 8, 8])
        wa = C.sb("wa", [128, 8, 128])
        wx = C.sb("wx", [128, 8, 128])
        c12 = C.sb("c12", [128, 2, 8])
        tiny = C.sb("tiny", [128, 1])
        carry = C.sb("carry", [128, 1])
        xin = [C.sb(f"xin{i}", [128, TL + 3]) for i in range(2)]
        gin = [C.sb(f"gin{i}", [128, TL]) for i in range(2)]
        xc = C.sb("xc", [128, TL])
        rg = C.sb("rg", [128, TL])
        ig = C.sb("ig", [128, TL])
        a_t = C.sb("a_t", [128, TL])
        e2 = C.sb("e2", [128, TL])
        b_t = C.sb("b_t", [128, TL])
        h_t = C.sb("h_t", [128, TL])
        u_t = C.sb("u_t", [128, TL])
        y_t = [C.sb(f"y_t{i}", [128, TL], BF16) for i in range(2)]
        psr = [C.ps(f"psr{i}", [128, 512]) for i in range(2)]
        psi = [C.ps(f"psi{i}", [128, 512]) for i in range(2)]
        P.dma(lv[:], Wl["lruv"][:, :, :], w=[R["lv"]])
        P.dma(wa[:], Wl["lru_wa"][:, :, :], w=[R["wa"]])
        P.dma(wx[:], Wl["lru_wx"][:, :, :], w=[R["wx"]])
        P.memset("pool", tiny[:], 1e-20, w=[R["tiny"]])
        P.act(c12[:, 0, :], lv[:, 7, :], AF.Exp, scale=-1.0, r=[R["lv"]], w=[R["c12"]])
        P.act(c12[:, 0, :], c12[:, 0, :], AF.Ln, bias=1.0, r=[R["c12"]], w=[R["c12"]])
        P.ts("dve", c12[:, 1, :], c12[:, 0, :], -16.0, None, op0=ALU.mult, r=[R["c12"]], w=[R["c12b"]])
        P.ts("dve", c12[:, 0, :], c12[:, 0, :], -8.0, None, op0=ALU.mult, r=[R["c12"], R["c12b"]], w=[R["c12"]])
        it = 0
        for c in range(8):
            rows = slice(c * 128, (c + 1) * 128)
            grows = slice(1024 + c * 128, 1024 + (c + 1) * 128)
            for ti in range(S // TL):
                t0 = ti * TL
                xi, gi = xin[it % 2], gin[it % 2]
                xr, gr = R[f"xin{it % 2}"], R[f"gin{it % 2}"]
                if ti == 0:
                    P.memset("pool", xi[:, 0:3], 0.0, w=[xr])
                    P.dma(xi[:, 3:3 + TL], scr["lruT"][rows, 0:TL], w=[xr])
                else:
                    P.dma(xi[:, 0:3 + TL], scr["lruT"][rows, t0 - 3:t0 + TL], w=[xr])
                P.dma(gi[:], scr["lruT"][grows, t0:t0 + TL], w=[gr])
                cw = lambda k: lv[:, k, c:c + 1]
                P.ts("dve", xc[:], xi[:, 3:3 + TL], cw(0), cw(4), op0=ALU.mult, op1=ALU.add, r=[xr, R["lv"]], w=[R["xc"]])
                for k in (1, 2, 3):
                    P.stt(xc[:], xi[:, 3 - k:3 - k + TL], cw(k), xc[:], ALU.mult, ALU.add, r=[xr, R["xc"]], w=[R["xc"]])
                for j in range(TL // 512):
                    sl = slice(j * 512, (j + 1) * 512)
                    P.mm(psr[j % 2][:], wa[:, c, :], xc[:, sl], r=[R["wa"], R["xc"]], w=[R[f"psr{j % 2}"]])
                    P.mm(psi[j % 2][:], wx[:, c, :], xc[:, sl], r=[R["wx"], R["xc"]], w=[R[f"psi{j % 2}"]])
                    P.act(rg[:, sl], psr[j % 2][:], AF.Sigmoid, bias=lv[:, 5, c:c + 1], r=[R[f"psr{j % 2}"]], w=[R["rg"]])
                    P.act(ig[:, sl], psi[j % 2][:], AF.Sigmoid, bias=lv[:, 6, c:c + 1], r=[R[f"psi{j % 2}"]], w=[R["ig"]])
                P.act(a_t[:], rg[:], AF.Exp, scale=c12[:, 0, c:c + 1], r=[R["rg"], R["c12"]], w=[R["a"]])
                P.act(e2[:], rg[:], AF.Exp, scale=c12[:, 1, c:c + 1], r=[R["rg"], R["c12b"]], w=[R["e2"]])
                P.ts("pool", e2[:], e2[:], -1.0, 1.0, op0=ALU.mult, op1=ALU.add, r=[R["e2"]], w=[R["e2"]])
                P.act(e2[:], e2[:], AF.Ln, bias=tiny[:], r=[R["e2"], R["tiny"]], w=[R["e2"]])
                P.act(e2[:], e2[:], AF.Exp, scale=0.5, r=[R["e2"]], w=[R["e2"]])
                P.tt("dve", b_t[:], e2[:], ig[:], ALU.mult, r=[R["e2"], R["ig"]], w=[R["b"]])
                P.tt("pool", b_t[:], b_t[:], xc[:], ALU.mult, r=[R["b"], R["xc"]], w=[R["b"]])
                init = 0.0 if ti == 0 else carry[:, 0:1]
                P.op("dve", (lambda init=init: (lambda e: e.tensor_tensor_scan(out=h_t[:], data0=a_t[:], data1=b_t[:],
                                                                             initial=init, op0=ALU.mult, op1=ALU.add)))(),
                     r=[R["a"], R["b"], R["carry"]], w=[R["h"]])
                P.cp("pool", carry[:], h_t[:, TL - 1:TL], r=[R["h"]], w=[R["carry"]])
                P.tt("pool", u_t[:], gi[:], gi[:], ALU.mult, r=[gr], w=[R["u"]])
                P.ts("pool", u_t[:], u_t[:], 0.044715, 1.0, op0=ALU.mult, op1=ALU.add, r=[R["u"]], w=[R["u"]])
                P.tt("dve", u_t[:], u_t[:], gi[:], ALU.mult, r=[R["u"], gr], w=[R["u"]])
                P.act(u_t[:], u_t[:], AF.Sigmoid, scale=GELU_K, r=[R["u"]], w=[R["u"]])
                P.tt("pool", u_t[:], u_t[:], gi[:], ALU.mult, r=[R["u"], gr], w=[R["u"]])
                yt, yr = y_t[it % 2], R[f"y{it % 2}"]
                P.tt("dve", yt[:], u_t[:], h_t[:], ALU.mult, r=[R["u"], R["h"]], w=[yr])
                P.dma(scr["yaT"][rows, t0:t0 + TL], yt[:], r=[yr], w=[P.region()])
                it += 1
    P.end_phase()


def phase_sb(C, l, cst, scr):
    P, nc, S = C.P, C.nc, C.S
    NQ = S // 512
    NB = S // 128
    with contextlib.ExitStack() as st:
        C.stack = st
        R = C.regs("sb_")
        qh = [C.sb(f"qh{i}", [128, S], BF16) for i in range(2)]
        kh = [C.sb(f"kh{i}", [128, S], BF16) for i in range(2)]
        vh = [C.sb(f"vh{i}", [128, NB, 128], BF16) for i in range(2)]
        NS = 2
        e_b = [[C.sb(f"e{s}_{i}", [128, 512]) for i in range(2)] for s in range(NS)]
        sp_b = [[C.sb(f"sp{s}_{i}", [128, 512], BF16) for i in range(2)] for s in range(NS)]
        d_b = [[C.sb(f"d{s}_{i}", [128, 512]) for i in range(2)] for s in range(NS)]
        at_b = [[C.sb(f"at{s}_{i}", [128, 512], BF16) for i in range(2)] for s in range(NS)]
        yo = [C.sb(f"yo{s}", [128, 512], BF16) for s in range(NS)]
        pz = [[C.ps(f"pz{s}_{i}", [128, 512]) for i in range(2)] for s in range(NS)]
        pc = [C.ps(f"pc{s}", [128, 512]) for s in range(NS)]
        po = [C.ps(f"po{s}", [128, 512]) for s in range(NS)]
        tri_ge, tri_lt, mask = cst["tri_ge_b"], cst["tri_lt_b"], cst["sbmask_b"]
        vv = scr["v"].rearrange("(n p) d -> p n d", p=128)

        def stream(s, hd, qt, hb):
            q_t, k_t, v_t = qh[hb], kh[hb], vh[hb]
            hr = [R[f"q{hb}"], R[f"k{hb}"], R[f"v{hb}"]]
            kbs = list(range(4 * qt + 3, -1, -1))
            pcr, por = R[f"pc{s}"], R[f"po{s}"]

            def front(idx):
                kb = kbs[idx]
                i2 = idx % 2
                z, zr = pz[s][i2], R[f"pz{s}_{i2}"]
                et, er = e_b[s][i2], R[f"e{s}_{i2}"]
                spt, spr = sp_b[s][i2], R[f"sp{s}_{i2}"]
                P.mm(z[:], k_t[:, kb * 128:(kb + 1) * 128], q_t[:, qt * 512:(qt + 1) * 512], r=hr[0:2], w=[zr])
                P.act(et[:], z[:], AF.Exp, r=[zr], w=[er])
                P.act(spt[:], et[:], AF.Ln, bias=1.0, r=[er], w=[spr])
                mi = kb - 4 * qt
                if mi >= 0:
                    P.tt("dve", spt[:], spt[:], mask[:, mi * 512:(mi + 1) * 512], ALU.mult, r=[spr], w=[spr])

            def back1(idx):
                last = idx == len(kbs) - 1
                i2 = idx % 2
                spt, spr = sp_b[s][i2], R[f"sp{s}_{i2}"]
                dt_, dr = d_b[s][i2], R[f"d{s}_{i2}"]
                P.mm(pc[s][:], tri_ge, spt[:], start=(idx == 0), stop=last, r=[spr], w=[pcr], skip_group_check=True)
                P.act(dt_[:], pc[s][:], AF.Exp, scale=-1.0, r=[pcr], w=[dr])

            def back2(idx):
                kb = kbs[idx]
                last = idx == len(kbs) - 1
                i2 = idx % 2
                et, er = e_b[s][i2], R[f"e{s}_{i2}"]
                spt, spr = sp_b[s][i2], R[f"sp{s}_{i2}"]
                dt_, dr = d_b[s][i2], R[f"d{s}_{i2}"]
                att, atr = at_b[s][i2], R[f"at{s}_{i2}"]
                if not last:
                    P.mm(pc[s][:], tri_lt, spt[:], start=False, stop=False, r=[spr], w=[pcr], skip_group_check=True)
                P.tt("dve", att[:], et[:], dt_[:], ALU.mult, r=[er, dr], w=[atr])
                mi = kb - 4 * qt
                if mi >= 0:
                    P.tt("dve", att[:], att[:], mask[:, mi * 512:(mi + 1) * 512], ALU.mult, r=[atr], w=[atr])

            def av(idx):
                kb = kbs[idx]
                last = idx == len(kbs) - 1
                i2 = idx % 2
                att, atr = at_b[s][i2], R[f"at{s}_{i2}"]
                P.mm(po[s][:], v_t[:, kb, :], att[:], start=(idx == 0), stop=last, r=[atr, hr[2]], w=[por])

            front(0)
            for idx in range(len(kbs)):
                if idx + 1 < len(kbs):
                    front(idx + 1)
                back1(idx)
                yield
                back2(idx)
                if idx >= 1:
                    av(idx - 1)
                yield
            av(len(kbs) - 1)
            P.cp("dve", yo[s][:], po[s][:], r=[R[f"po{s}"]], w=[R[f"yo{s}"]])
            P.dma(scr["ybT"][hd * 128:(hd + 1) * 128, qt * 512:(qt + 1) * 512], yo[s][:], r=[R[f"yo{s}"]], w=[P.region()])

        for hd in range(8):
            hb = hd % 2
            rows = slice(hd * 128, (hd + 1) * 128)
            P.dma(qh[hb][:], scr["qT"][rows, :], w=[R[f"q{hb}"]])
            P.dma(kh[hb][:], scr["kT"][rows, :], w=[R[f"k{hb}"]])
            for n0 in range(0, NB, 16):
                n1 = min(NB, n0 + 16)
                P.dma(vh[hb][:, n0:n1, :], vv[:, n0:n1, rows], w=[R[f"v{hb}"]])
            order = []
            lo, hi = 0, NQ - 1
            while lo <= hi:
                order.append(hi)
                if lo != hi:
                    order.append(lo)
                lo += 1
                hi -= 1
            pending = list(order)
            active = []
            free_slots = list(range(NS))
            while pending or active:
                while pending and free_slots:
                    s = free_slots.pop(0)
                    active.append((s, stream(s, hd, pending.pop(0), hb)))
                nxt = []
                for (s, g) in active:
                    try:
                        next(g)
                        nxt.append((s, g))
                    except StopIteration:
                        free_slots.append(s)
                active = nxt
    P.end_phase()


RW_C0 = 0.6065306597126334


def phase_rwkv(C, l, cst, Wl, scr):
    P, nc, S = C.P, C.nc, C.S
    NCH = S // 128
    with contextlib.ExitStack() as st:
        C.stack = st
        R = C.regs("rw_")
        rwv = C.sb("rwv", [128, 7, 1024])
        wst = C.sb("rw_wst", [128, 1024])
        waup = C.sb("waup", [128, 1024], BF16)
        gup = C.sb("gup", [128, 1024], BF16)
        epsg = C.sb("epsg", [128, 1])
        T1 = C.sb("T1", [128, 3, 1024])
        lt = C.sb("lt", [128, 2, 128], BF16)
        lta = C.sb("lta", [64, 128], BF16)
        aup = C.sb("aup", [64, 1024], BF16)
        sgw = C.sb("sgw", [128, 1024])
        a_t = C.sb("rwa", [128, 1024])
        kk = C.sb("kk", [128, 1024])
        b_t = C.sb("rwb", [128, 1024])
        clsb = C.sb("clsb", [128, 1024])
        tmp = C.sb("rwtmp", [128, 1024])
        E = [C.sb(f"rwE{i}", [128, 1024]) for i in range(2)]
        gC = C.sb("gC", [64, 1024])
        gate = C.sb("rwgate", [128, 1024])
        small = C.sb("rwsmall", [128, 8, 16])
        prod = {n: C.sb("pr_" + n, [128, 1024], BF16) for n in ("kq", "bh", "kh", "rq", "bE", "kE", "v")}
        XT = {n: C.sb("xt_" + n, [64, 16, 128], BF16) for n in ("kq", "bh", "kh", "rq")}
        G = 4
        xs = [C.sb(f"xs{i}", [128, 256]) for i in range(G)]
        akt = [C.sb(f"akt{i}", [128, 128], BF16) for i in range(G)]
        Mn = [[C.sb(f"Mn{i}_{j}", [128, 256]) for j in range(2)] for i in range(G)]
        TTb = [[C.sb(f"TT{i}_{j}", [128, 128]) for j in range(2)] for i in range(G)]
        TTf = [C.sb(f"TTf{i}", [128, 128], BF16) for i in range(G)]
        kcw = [C.sb(f"kcw{i}", [128, 128], BF16) for i in range(G)]
        dg = [C.sb(f"dg{i}", [64, 64]) for i in range(G)]
        BbT = C.sb("BbT", [128, 16, 128], BF16)
        BkT = C.sb("BkT", [128, 16, 128], BF16)
        Uv = C.sb("Uv", [128, 16, 64], BF16)
        RcT = C.sb("RcT", [64, 16, 128], BF16)
        GT = C.sb("GT", [64, 16, 64])
        H_a = C.sb("H_a", [64, 16, 64])
        st_f = C.sb("st_f", [64, 16, 64])
        st_b = C.sb("st_b", [64, 16, 64], BF16)
        ysb = C.sb("ysb", [128, 1024])
        ycs = C.sb("ycs", [128, 8, 128], BF16)
        PW = C.ps("PW", [128, 1024])
        PT = C.ps("PT", [128, 2048], BF16)
        banks = [C.ps(f"rwbk{i}", [128, 512]) for i in range(4)]
        for _k in ("PW", "PT", "bk0", "bk1", "bk2", "bk3"):
            R[_k].excl = True
        bcnt = [0]

        def bank():
            i = bcnt[0] % 4
            bcnt[0] += 1
            return banks[i], R[f"bk{i}"]

        ident_f, ident_b, ones_f, tri_le = cst["ident"], cst["ident_b"], cst["ones"], cst["tri_le"]
        mask3, m_ui = cst["mask3"], cst["m_ui"]
        P.dma(rwv[:], Wl["rwv"][:, :, :], w=[R["rwv"]])
        P.dma(wst[0:64, :], Wl["rw_w_up"][:, :], w=[R["wst"]])
        P.dma(wst[64:128, :], Wl["rw_a_up"][:, :], w=[R["wst"]])
        P.cp("dve", waup[:], wst[:], r=[R["wst"]], w=[R["waup"]])
        P.dma(wst[0:64, :], Wl["rw_a_up"][:, :], w=[R["wst"]])
        P.cp("dve", aup[:], wst[0:64, :], r=[R["wst"]], w=[R["aup"]])
        P.dma(wst[:], Wl["rw_g_up"][:, :], w=[R["wst"]])
        P.cp("dve", gup[:], wst[:], r=[R["wst"]], w=[R["gup"]])
        P.memset("pool", epsg[:], 64e-5, w=[R["epsg"]])
        P.memset("pool", st_f[:], 0.0, w=[R["st_f"]])
        P.memset("pool", st_b[:], 0.0, w=[R["st_b"]])
        ycv = scr["ycT"].rearrange("(k p) s -> p k s", p=128)
        hv = lambda t: t.rearrange("p (h j) -> p h j", j=64)
        rW, rA, rKK, rKA, rRK, rLW, rLB = (rwv[:, i, :] for i in range(7))

        for n in range(NCH):
            t0 = n * 128
            r_, k_, v_ = T1[:, 0, :], T1[:, 1, :], T1[:, 2, :]
            P.dma(T1[:], scr["rkv"][t0:t0 + 128, :].rearrange("p (a c) -> p a c", a=3), w=[R["T1"]])
            P.dma(lt[:, 0, :], scr["loraT"][0:128, t0:t0 + 128], w=[R["lt"]])
            P.dma(lt[:, 1, :], scr["loraT"][128:256, t0:t0 + 128], w=[R["lt"]])
            P.dma(lta[:], scr["loraT"][64:128, t0:t0 + 128], w=[R["lta"]])
            for hf in range(2):
                sl = slice(hf * 512, (hf + 1) * 512)
                P.mm(PW[:, sl], lt[0:64, 0, :], waup[0:64, sl], r=[R["lt"], R["waup"]], w=[R["PW"]])
            P.tt("dve", sgw[:], PW[:], rW, ALU.add, r=[R["PW"], R["rwv"]], w=[R["sgw"]])
            P.act(sgw[:], sgw[:], AF.Sigmoid, r=[R["sgw"]], w=[R["sgw"]])
            for hf in range(2):
                sl = slice(hf * 512, (hf + 1) * 512)
                P.mm(PW[:, sl], lta[:], aup[:, sl], r=[R["lta"], R["aup"]], w=[R["PW"]])
            P.tt("dve", a_t[:], PW[:], rA, ALU.add, r=[R["PW"], R["rwv"]], w=[R["a"]])
            P.act(a_t[:], a_t[:], AF.Sigmoid, r=[R["a"]], w=[R["a"]])
            for hf in range(2):
                sl = slice(hf * 512, (hf + 1) * 512)
                P.mm(PW[:, sl], lt[:, 1, :], gup[:, sl], r=[R["lt"], R["gup"]], w=[R["PW"]])
            P.cp("act", gate[:], PW[:], r=[R["PW"]], w=[R["gate"]])
            P.tt("dve", kk[:], k_, rKK, ALU.mult, r=[R["T1"], R["rwv"]], w=[R["kk"]])
            P.tt("pool", tmp[:], kk[:], kk[:], ALU.mult, r=[R["kk"]], w=[R["tmp"]])
            P.op("dve", lambda e: e.tensor_reduce(out=small[:, 0, :], in_=hv(tmp[:]), axis=AX.X, op=ALU.add),
                 r=[R["tmp"]], w=[R["sm0"]])
            P.ts("dve", small[:, 0, :], small[:, 0, :], 1e-24, None, op0=ALU.max, r=[R["sm0"]], w=[R["sm0"]])
            P.act(small[:, 0, :], small[:, 0, :], AF.Ln, r=[R["sm0"]], w=[R["sm0"]])
            P.act(small[:, 0, :], small[:, 0, :], AF.Exp, scale=-0.5, r=[R["sm0"]], w=[R["sm0"]])
            for h in range(16):
                hs = slice(h * 64, (h + 1) * 64)
                P.ts("dve" if h % 2 else "pool", kk[:, hs], kk[:, hs], small[:, 0, h:h + 1], None, op0=ALU.mult,
                     r=[R["kk"], R["sm0"]], w=[R["kk"]])
            P.stt(tmp[:], a_t[:], -1.0, rKA, ALU.add, ALU.mult, r=[R["a"], R["rwv"], R["tmp"]], w=[R["tmp"]])
            P.stt(k_, tmp[:], 1.0, k_, ALU.add, ALU.mult, r=[R["tmp"], R["T1"], R["kk"]], w=[R["T1"]])
            P.tt("pool", b_t[:], kk[:], a_t[:], ALU.mult, r=[R["kk"], R["a"]], w=[R["b"]])
            P.tt("pool", tmp[:], r_, k_, ALU.mult, r=[R["T1"]], w=[R["tmp"]])
            P.tt("pool", tmp[:], tmp[:], rRK, ALU.mult, r=[R["tmp"], R["rwv"]], w=[R["tmp"]])
            P.op("dve", lambda e: e.tensor_reduce(out=small[:, 1, :], in_=hv(tmp[:]), axis=AX.X, op=ALU.add),
                 r=[R["tmp"]], w=[R["sm1"]])
            for hf in range(2):
                sl = slice(hf * 512, (hf + 1) * 512)
                P.mm(PW[:, sl], tri_le, sgw[:, sl], r=[R["sgw"]], w=[R["PW"]])
            P.cp("act", clsb[:], PW[:], r=[R["PW"]], w=[R["clsb"]])
            for hf in range(2):
                sl = slice(hf * 512, (hf + 1) * 512)
                P.mm(PW[:, sl], ones_f, sgw[:, sl], r=[R["sgw"]], w=[R["PW"]])
            E0, E1 = E[0], E[1]
            P.act(E0[:], clsb[:], AF.Exp, scale=-RW_C0, r=[R["clsb"]], w=[R["E0"]])
            P.tt("dve", prod["rq"][:], r_, E0[:], ALU.mult, r=[R["T1"], R["E0"]], w=[R["p_rq"]])
            P.act(E1[:], clsb[:], AF.Exp, scale=RW_C0, r=[R["clsb"]], w=[R["E1"]])
            P.tt("pool", prod["bh"][:], b_t[:], E1[:], ALU.mult, r=[R["b"], R["E1"]], w=[R["p_bh"]])
            P.tt("dve", prod["kh"][:], k_, E1[:], ALU.mult, r=[R["T1"], R["E1"]], w=[R["p_kh"]])
            P.tt("pool", tmp[:], clsb[:], sgw[:], ALU.subtract, r=[R["clsb"], R["sgw"], R["tmp"]], w=[R["tmp"]])
            P.act(E0[:], tmp[:], AF.Exp, scale=-RW_C0, r=[R["tmp"], R["E0"]], w=[R["E0"]])
            P.tt("dve", prod["kq"][:], kk[:], E0[:], ALU.mult, r=[R["kk"], R["E0"]], w=[R["p_kq"]])
            P.tt("dve", tmp[:], PW[:], clsb[:], ALU.subtract, r=[R["PW"], R["clsb"], R["tmp"]], w=[R["tmp"]])
            P.act(E1[:], tmp[:], AF.Exp, scale=-RW_C0, r=[R["tmp"], R["E1"]], w=[R["E1"]])
            P.tt("pool", prod["bE"][:], b_t[:], E1[:], ALU.mult, r=[R["b"], R["E1"]], w=[R["p_bE"]])
            P.tt("dve", prod["kE"][:], k_, E1[:], ALU.mult, r=[R["T1"], R["E1"]], w=[R["p_kE"]])
            P.act(gC[:], PW[0:64, :], AF.Exp, scale=-RW_C0, r=[R["PW"]], w=[R["gC"]])
            P.cp("pool", prod["v"][:], v_, r=[R["T1"]], w=[R["p_v"]])
            import os as _os
            _stop = _os.environ.get("RW_STOP", "")
            if _stop == "A":
                continue
            PTv = PT[0:64, :].rearrange("p (h t) -> p h t", t=128)
            for qi, qn in enumerate(("kq", "bh", "kh", "rq")):
                for h in range(16):
                    P.tr(PTv[:, h, :], prod[qn][:, h * 64:(h + 1) * 64], ident_b, r=[R["p_" + qn]], w=[R["PT"]])
                P.cp("act" if qi % 2 else "dve", XT[qn][:], PTv, r=[R["PT"]], w=[R["xt_" + qn]])

            if _stop == "T":
                continue
            def head(slot, h):
                hs = slice(h * 64, (h + 1) * 64)
                kqT, bhT, khT, rqT = (XT[q][:, h, :] for q in ("kq", "bh", "kh", "rq"))
                xr = [R["xt_kq"], R["xt_bh"], R["xt_kh"], R["xt_rq"]]
                X, Xr = banks[slot], R[f"bk{slot}"]
                P.mm(X[:, 0:128], kqT, bhT, r=xr, w=[Xr])
                P.mm(X[:, 128:256], bhT, kqT, r=xr, w=[Xr])
                P.mm(X[:, 256:384], khT, kqT, r=xr, w=[Xr])
                P.mm(X[:, 384:512], bhT, rqT, r=xr, w=[Xr])
                yield
                xs_, xsr = xs[slot], R[f"xs{slot}"]
                P.tt("dve", xs_[:], X[:, 0:256], mask3[:, 0:256], ALU.mult, r=[Xr], w=[xsr])
                P.tt("dve", akt[slot][:], X[:, 256:384], mask3[:, 256:384], ALU.mult, r=[Xr], w=[R[f"akt{slot}"]])
                P.tt("dve", BbT[:, h, :], X[:, 384:512], m_ui, ALU.mult, r=[Xr], w=[R[f"BbT{h}"]])
                tcur = 0
                TT_, TTr = TTb[slot][tcur], R[f"TT{slot}_{tcur}"]
                P.tt("pool", TT_[:], ident_f, xs_[:, 128:256], ALU.subtract, r=[xsr], w=[TTr])
                M_, MT_, Mr = xs_[:, 0:128], xs_[:, 128:256], xsr
                yield
                for lev in range(6):
                    L, Lr = X, Xr
                    P.mm(L[:, 0:128], MT_, M_, r=[Mr], w=[Lr])
                    if lev < 5:
                        P.mm(L[:, 128:256], M_, MT_, r=[Mr], w=[Lr])
                    if lev == 0:
                        P.mm(L[:, 384:512], khT, rqT, r=xr, w=[Lr])
                    yield
                    if lev == 0:
                        P.tt("dve", BkT[:, h, :], L[:, 384:512], m_ui, ALU.mult, r=[Lr], w=[R[f"BkT{h}"]])
                    mn, mnr = Mn[slot][lev % 2], R[f"Mn{slot}_{lev % 2}"]
                    wdt = 256 if lev < 5 else 128
                    P.cp("act" if (h + lev) % 2 else "dve", mn[:, 0:wdt], L[:, 0:wdt], r=[Lr], w=[mnr])
                    P.mm(L[:, 256:384], mn[:, 0:128], TT_[:], r=[mnr, TTr], w=[Lr])
                    yield
                    tn = 1 - tcur
                    TTn, TTnr = TTb[slot][tn], R[f"TT{slot}_{tn}"]
                    P.tt("dve", TTn[:], L[:, 256:384], TT_[:], ALU.add, r=[Lr, TTr], w=[TTnr])
                    TT_, TTr, tcur = TTn, TTnr, tn
                    M_, MT_, Mr = mn[:, 0:128], mn[:, 128:256], mnr
                Z2, Z2r = X, Xr
                P.cp("pool", TTf[slot][:], TT_[:], r=[TTr], w=[R[f"TTf{slot}"]])
                TT_, TTr = TTf[slot], R[f"TTf{slot}"]
                P.mm(Z2[:, 0:64], TT_[:], prod["kq"][:, hs], r=[TTr, R["p_kq"]], w=[Z2r])
                P.mm(Z2[:, 64:128], akt[slot][:], prod["v"][:, hs], r=[R[f"akt{slot}"], R["p_v"]], w=[Z2r])
                yield
                kc_, kcr = kcw[slot], R[f"kcw{slot}"]
                P.cp("act", kc_[:], Z2[:, 0:128], r=[Z2r], w=[kcr])
                P.mm(Z2[:, 128:192], TT_[:], kc_[:, 64:128], r=[TTr, kcr], w=[Z2r])
                P.mm(Z2[0:64, 192:256], kc_[:, 0:64], prod["bE"][:, hs], r=[kcr, R["p_bE"]], w=[Z2r])
                P.mm(Z2[0:64, 256:384], kc_[:, 0:64], BbT[:, h, :], r=[kcr, R[f"BbT{h}"]], w=[Z2r])
                yield
                P.ts("dve", Uv[:, h, :], Z2[:, 128:192], -1.0, None, op0=ALU.mult, r=[Z2r], w=[R[f"Uv{h}"]])
                P.tt("pool", dg[slot][:], ident_f[0:64, 0:64], gC[:, hs], ALU.mult, r=[R["gC"]], w=[R[f"dg{slot}"]])
                P.tt("dve", GT[:, h, :], dg[slot][:], Z2[0:64, 192:256], ALU.subtract, r=[R[f"dg{slot}"], Z2r], w=[R[f"GT{h}"]])
                P.tt("dve", RcT[:, h, :], rqT, Z2[0:64, 256:384], ALU.subtract, r=[R["xt_rq"], Z2r], w=[R[f"RcT{h}"]])
                P.mm(Z2[0:64, 384:448], prod["bE"][:, hs], Uv[:, h, :], start=True, stop=False,
                     r=[R["p_bE"], R[f"Uv{h}"]], w=[Z2r])
                P.mm(Z2[0:64, 384:448], prod["kE"][:, hs], prod["v"][:, hs], start=False, stop=True,
                     r=[R["p_kE"], R["p_v"]], w=[Z2r])
                yield
                P.cp("act", H_a[:, h, :], Z2[0:64, 384:448], r=[Z2r], w=[R[f"H{h}"]])

            pending = list(range(16))
            active = []
            _hc = {}
            free_slots = list(range(G))
            while pending or active:
                while pending and free_slots:
                    s_ = free_slots.pop(0)
                    active.append((s_, head(s_, pending.pop(0))))
                nxt = []
                for (s_, g_) in active:
                    try:
                        next(g_)
                        _hc[s_] = _hc.get(s_, 0) + 1
                        if _hc[s_] >= int(_os.environ.get("RW_HSTOP", "999")):
                            _hc[s_] = 0
                            g_.close()
                            raise StopIteration
                        nxt.append((s_, g_))
                    except StopIteration:
                        _hc[s_] = 0
                        free_slots.append(s_)
                active = nxt
            if _stop == "B":
                continue
            for h in range(16):
                hs = slice(h * 64, (h + 1) * 64)
                P.mm(PW[:, hs], RcT[:, h, :], st_b[:, h, :], start=True, stop=False,
                     r=[R[f"RcT{h}"], R["st_b"]], w=[R["PW"]])
                P.mm(PW[:, hs], BbT[:, h, :], Uv[:, h, :], start=False, stop=False,
                     r=[R[f"BbT{h}"], R[f"Uv{h}"]], w=[R["PW"]])
                P.mm(PW[:, hs], BkT[:, h, :], prod["v"][:, hs], start=False, stop=True,
                     r=[R[f"BkT{h}"], R["p_v"]], w=[R["PW"]])
            P.cp("act", ysb[:], PW[:], r=[R["PW"]], w=[R["ysb"]])
            for h in range(16):
                hs = slice(h * 64, (h + 1) * 64)
                P.mm(PW[0:64, hs], GT[:, h, :], st_f[:, h, :], r=[R[f"GT{h}"], R["st_f"]], w=[R["PW"]])
            P.tt("dve", st_f[:].rearrange("p h j -> p (h j)"), PW[0:64, :], H_a[:].rearrange("p h j -> p (h j)"), ALU.add,
                 r=[R["PW"]] + [R[f"H{h}"] for h in range(16)], w=[R["st_f"]])
            P.cp("act", st_b[:], st_f[:], r=[R["st_f"]], w=[R["st_b"]])
            P.op("dve", lambda e: e.tensor_reduce(out=small[:, 2, :], in_=hv(ysb[:]), axis=AX.X, op=ALU.add),
                 r=[R["ysb"]], w=[R["sm2"]])
            P.tt("pool", tmp[:], ysb[:], ysb[:], ALU.mult, r=[R["ysb"], R["tmp"]], w=[R["tmp"]])
            P.op("dve", lambda e: e.tensor_reduce(out=small[:, 3, :], in_=hv(tmp[:]), axis=AX.X, op=ALU.add),
                 r=[R["tmp"]], w=[R["sm3"]])
            P.ts("dve", small[:, 4, :], small[:, 2, :], 1.0 / 64, None, op0=ALU.mult, r=[R["sm2"]], w=[R["sm4"]])
            P.tt("dve", small[:, 5, :], small[:, 4, :], small[:, 4, :], ALU.mult, r=[R["sm4"]], w=[R["sm5"]])
            P.stt(small[:, 6, :], small[:, 3, :], 1.0 / 64, small[:, 5, :], ALU.mult, ALU.subtract,
                  r=[R["sm3"], R["sm5"]], w=[R["sm6"]])
            P.act(small[:, 6, :], small[:, 6, :], AF.Ln, bias=epsg[:], r=[R["sm6"], R["epsg"]], w=[R["sm6"]])
            P.act(small[:, 6, :], small[:, 6, :], AF.Exp, scale=-0.5, r=[R["sm6"]], w=[R["sm6"]])
            for h in range(16):
                hs = slice(h * 64, (h + 1) * 64)
                P.ts("dve" if h % 2 else "pool", ysb[:, hs], ysb[:, hs], small[:, 4, h:h + 1], small[:, 6, h:h + 1],
                     op0=ALU.subtract, op1=ALU.mult, r=[R["ysb"], R["sm4"], R["sm6"]], w=[R["ysb"]])
            P.tt("pool", ysb[:], ysb[:], rLW, ALU.mult, r=[R["ysb"], R["rwv"]], w=[R["ysb"]])
            P.tt("dve", ysb[:], ysb[:], rLB, ALU.add, r=[R["ysb"], R["rwv"]], w=[R["ysb"]])
            for h in range(16):
                hs = slice(h * 64, (h + 1) * 64)
                P.stt(ysb[:, hs], v_[:, hs], small[:, 1, h:h + 1], ysb[:, hs], ALU.mult, ALU.add,
                      r=[R["T1"], R["sm1"], R["ysb"]], w=[R["ysb"]])
            P.tt("pool", ysb[:], ysb[:], gate[:], ALU.mult, r=[R["ysb"], R["gate"]], w=[R["ysb"]])
            for k in range(8):
                P.tr(PW[:, k * 128:(k + 1) * 128], ysb[:, k * 128:(k + 1) * 128], ident_f, r=[R["ysb"]], w=[R["PW"]])
            P.cp("act", ycs[:].rearrange("p k t -> p (k t)"), PW[:], r=[R["PW"]], w=[R["ycs"]])
            P.dma(ycv[:, :, t0:t0 + 128], ycs[:], r=[R["ycs"]], w=[P.region()])
    P.end_phase()
def phase_merge(C, l, cst, Wl, x_in, per, scr, sel=None):
    P, nc, S = C.P, C.nc, C.S
    TT = 256
    SL = S // 2 if sel is not None else S
    with contextlib.ExitStack() as st:
        C.stack = st
        R = C.regs("mg_")
        wts = {n: C.sb("mg_" + n, [128, 8, 1024], BF16) for n in ("p_lru", "p_sb", "p_rwkv", "w_out")}
        wr = C.sb("mg_wr", [128, 8, 32])
        brt = C.sb("mg_br", [128, 32])
        pt = per["t"]
        with contextlib.ExitStack() as st2:
            C.stack = st2
            wst = C.sb("mg_wst", [128, 8, 1024])
            for i, n in enumerate(("p_lru", "p_sb", "p_rwkv", "w_out")):
                P.dma(wst[:], Wl[n].rearrange("(k p) j -> p k j", p=128), w=[R["wst"]])
                P.cp("pool" if i % 2 else "dve", wts[n][:], wst[:], r=[R["wst"]], w=[R["w_" + n]])
            P.end_phase()
        C.stack = st
        P.dma(wr[:], Wl["w_router"].rearrange("(k p) j -> p k j", p=128), w=[R["wr"]])
        P.dma(brt[:], Wl["b_router"][:, :], w=[R["br"]])
        yt = {n: C.sb("mg_y" + n, [128, 8, TT], BF16) for n in ("a", "b", "c")}
        gt = [C.sb(f"mg_g{i}", [128, 3, TT]) for i in range(2)]
        mrg = C.sb("mg_mrg", [128, 8, TT], BF16)
        m1 = C.sb("mg_m1", [128, TT])
        m2 = C.sb("mg_m2", [128, TT])
        xt = C.sb("mg_xt", [128, 8, TT])
        x1 = C.sb("mg_x1", [128, 8, TT])
        h2f = C.sb("mg_h2f", [128, 8, TT])
        h2b = C.sb("mg_h2b", [128, 8, TT], BF16)
        sq = C.sb("mg_sq", [128, 8, TT])
        tmp = C.sb("mg_tmp", [128, 8, TT])
        rs = C.sb("mg_rs", [128, TT])
        lg = C.sb("mg_lg", [128, 32])
        m8 = C.sb("mg_m8", [128, 8])
        nmx = C.sb("mg_nmx", [128, 1])
        msk = C.sb("mg_msk", [128, 32])
        ex = C.sb("mg_ex", [128, 32])
        ssum = C.sb("mg_ssum", [128, 1])
        rwo = [C.sb(f"mg_rwo{i}", [128, 32]) for i in range(2)]
        psb = [C.ps(f"mg_ps{i}", [128, 512]) for i in range(3)]
        psm = C.ps("mg_psm", [128, 512])
        ps_s = C.ps("mg_pss", [128, 512])
        psl = C.ps("mg_psl", [128, 512])
        for k_ in ("ps0", "ps1", "ps2", "psm", "ps_s", "psl"):
            R[k_].excl = True
        xv = x_in.rearrange("(k p) s -> p k s", p=128)
        yv = {n: scr["y%sT" % n].rearrange("(k p) s -> p k s", p=128) for n in ("a", "b", "c")}
        gv = scr["gatesT"].rearrange("(g k p) s -> p g k s", p=128, k=8)
        x1v = scr["x1T"].rearrange("(k p) s -> p k s", p=128)
        h2v = scr["h2T"].rearrange("(k p) s -> p k s", p=128)
        pw = (("a", "p_lru"), ("b", "p_sb"), ("c", "p_rwkv"))
        gi = 0
        if sel is not None:
            fl = C.sb("mg_fl", [128, 2])
            yt2 = {n: C.sb("mg_y2" + n, [128, 8, TT], BF16) for n in ("a", "b", "c")}
            xt2 = C.sb("mg_xt2", [128, 8, TT])
            gt2 = [C.sb(f"mg_g2{i}", [128, 3, TT]) for i in range(2)]
            P.dma(fl[:, 0:1], sel[:, :], w=[R["fl"]])
            P.ts("dve", fl[:, 1:2], fl[:, 0:1], -1.0, 1.0, op0=ALU.mult, op1=ALU.add, r=[R["fl"]], w=[R["fl2"]])

            def blend(eng, dst, other, dr, orr):
                P.ts(eng, other, other, fl[:, 0:1], None, op0=ALU.mult, r=[orr, R["fl"]], w=[orr])
                P.stt(dst, dst, fl[:, 1:2], other, ALU.mult, ALU.add, r=[dr, orr, R["fl2"]], w=[dr])
        for ti in range(SL // TT):
            ts_ = slice(ti * TT, (ti + 1) * TT)
            ts2 = slice(SL + ti * TT, SL + (ti + 1) * TT)
            for n in ("a", "b", "c"):
                P.dma(yt[n][:], yv[n][:, :, ts_], w=[R["y" + n]])
                if sel is not None:
                    P.dma(yt2[n][:], yv[n][:, :, ts2], w=[R["y2" + n]])
                    blend("dve", yt[n][:], yt2[n][:], R["y" + n], R["y2" + n])
            P.dma(xt[:], xv[:, :, ts_], w=[R["xt"]])
            if sel is not None:
                P.dma(xt2[:], xv[:, :, ts2], w=[R["xt2"]])
                blend("pool", xt[:], xt2[:], R["xt"], R["xt2"])
            for oc in range(8):
                g_ = gt[gi % 2]
                gr = R[f"g{gi % 2}"]
                P.dma(g_[:], gv[:, :, oc, ts_], w=[gr])
                if sel is not None:
                    g2_, g2r = gt2[gi % 2], R[f"g2{gi % 2}"]
                    P.dma(g2_[:], gv[:, :, oc, ts2], w=[g2r])
                    blend("pool", g_[:], g2_[:], gr, g2r)
                gi += 1
                for bi, (yn, wn) in enumerate(pw):
                    for k in range(8):
                        P.mm(psb[bi][:, :TT], wts[wn][:, k, oc * 128:(oc + 1) * 128], yt[yn][:, k, :],
                             start=(k == 0), stop=(k == 7), r=[R["w_" + wn], R["y" + yn]], w=[R[f"ps{bi}"]])
                P.tt("dve", m1[:], psb[0][:, :TT], g_[:, 0, :], ALU.mult, r=[R["ps0"], gr], w=[R["m1"]])
                P.tt("dve", m2[:], psb[1][:, :TT], g_[:, 1, :], ALU.mult, r=[R["ps1"], gr], w=[R["m2"]])
                P.tt("pool", m1[:], m1[:], m2[:], ALU.add, r=[R["m1"], R["m2"]], w=[R["m1"]])
                P.tt("dve", m2[:], psb[2][:, :TT], g_[:, 2, :], ALU.mult, r=[R["ps2"], gr, R["m2"]], w=[R["m2"]])
                P.tt("pool", mrg[:, oc, :], m1[:], m2[:], ALU.add, r=[R["m1"], R["m2"]], w=[R["mrg"]])
            for oc in range(8):
                for k in range(8):
                    P.mm(psm[:, :TT], wts["w_out"][:, k, oc * 128:(oc + 1) * 128], mrg[:, k, :],
                         start=(k == 0), stop=(k == 7), r=[R["w_w_out"], R["mrg"]], w=[R["psm"]])
                P.stt(x1[:, oc, :], psm[:, :TT], pt[:, 2, oc:oc + 1], xt[:, oc, :], ALU.mult, ALU.add,
                      r=[R["psm"], R["xt"]], w=[R["xt1"]])
            P.dma(x1v[:, :, ts_], x1[:], r=[R["xt1"]], w=[P.region()])
            emit_norm_tile(C, R, x1[:], lambda k: h2f[:, k, :], pt[:, 3, :], pt[:, 4, :], cst["ones"], sq, rs, tmp,
                           ps_s, TT, cst["eps"], "1")
            P.cp("pool", h2b[:], h2f[:], r=[R["hT"]], w=[R["h2b"]])
            P.dma(h2v[:, :, ts_], h2b[:], r=[R["h2b"]], w=[P.region()])
            for sub in range(TT // 128):
                for k in range(8):
                    P.mm(psl[:, 0:32], h2f[:, k, sub * 128:(sub + 1) * 128], wr[:, k, :], start=(k == 0), stop=(k == 7),
                         r=[R["hT"], R["wr"]], w=[R["psl"]])
                P.tt("dve", lg[:], psl[:, 0:32], brt[:], ALU.add, r=[R["psl"], R["br"]], w=[R["lg"]])
                P.op("dve", lambda e: e.max(out=m8[:], in_=lg[:]), r=[R["lg"]], w=[R["m8"]])
                P.ts("dve", nmx[:], m8[:, 0:1], -1.0, None, op0=ALU.mult, r=[R["m8"]], w=[R["nmx"]])
                P.ts("dve", msk[:], lg[:], m8[:, 3:4], None, op0=ALU.is_ge, r=[R["lg"], R["m8"]], w=[R["msk"]])
                P.act(ex[:], lg[:], AF.Exp, bias=nmx[:], r=[R["lg"], R["nmx"]], w=[R["ex"]])
                P.tt("dve", ex[:], ex[:], msk[:], ALU.mult, r=[R["ex"], R["msk"]], w=[R["ex"]])
                P.op("dve", lambda e: e.tensor_reduce(out=ssum[:], in_=ex[:], axis=AX.X, op=ALU.add),
                     r=[R["ex"]], w=[R["ssum"]])
                P.op("dve", lambda e: e.reciprocal(out=ssum[:], in_=ssum[:]), r=[R["ssum"]], w=[R["ssum"]])
                ro = rwo[sub % 2]
                P.ts("dve", ro[:], ex[:], ssum[:, 0:1], None, op0=ALU.mult, r=[R["ex"], R["ssum"]], w=[R[f"rwo{sub % 2}"]])
                t0 = ti * TT + sub * 128
                P.dma(scr["rwt"][t0:t0 + 128, :], ro[:], r=[R[f"rwo{sub % 2}"]], w=[P.region()])
    P.end_phase()


def phase_moe(C, l, cst, Wl, per, scr, nfin_d, out_d, S_eff=None):
    P, nc, S = C.P, C.nc, (S_eff or C.S)
    import os
    NE = int(os.environ.get("MOE_NE", "32"))
    ST = min(int(os.environ.get("MOE_ST", "512")), S)
    NTB = ST // 128
    with contextlib.ExitStack() as st:
        C.stack = st
        R = C.regs("moe_")
        pt = per["t"]
        gub = scr["gub"]
        dnb = scr["dnb"]
        for e in range(NE):
            P.dma(gub[e], Wl["w_gu"][e], w=[P.region()], q="pool")
            P.dma(dnb[e], Wl["w_down"][e], w=[P.region()], q="pool")
        P.end_phase()
        C.stack = st
        h2 = C.sb("moe_h2", [128, 8, ST], BF16)
        yacc = C.sb("moe_yacc", [128, NTB, 1024])
        wgu = [C.sb(f"moe_wgu{i}", [128, 8, 2048], BF16) for i in range(2)]
        wdn = [C.sb(f"moe_wdn{i}", [128, 8, 1024], BF16) for i in range(2)]
        actT = [C.sb(f"moe_act{i}", [128, 8, 512], BF16) for i in range(2)]
        bgu = C.sb("moe_bgu", [128, 32, 16])
        bdn = C.sb("moe_bdn", [32, 1024])
        rwt = C.sb("moe_rwt", [128, NTB, 32])
        rwT = C.sb("moe_rwT", [32, 128])
        g_t = [C.sb(f"moe_g{i}", [128, 512]) for i in range(2)]
        u_t = [C.sb(f"moe_u{i}", [128, 512]) for i in range(2)]
        s_t = [C.sb(f"moe_s{i}", [128, 512]) for i in range(2)]
        x1t = C.sb("moe_x1", [128, 8, 128])
        x2t = C.sb("moe_x2", [128, 8, 128])
        nfin = C.sb("moe_nfin", [128, 8])
        sq = C.sb("moe_sq", [128, 8, 128])
        rs = C.sb("moe_rs", [128, 128])
        o_t = sq
        psg = [C.ps(f"moe_psg{i}", [128, 512]) for i in range(2)]
        psu = [C.ps(f"moe_psu{i}", [128, 512]) for i in range(2)]
        psd = [C.ps(f"moe_psd{i}", [128, 512]) for i in range(2)]
        psx = C.ps("moe_psx", [128, 1024])
        for k_ in ("psg0", "psg1", "psu0", "psu1", "psd0", "psd1", "psx"):
            R[k_].excl = True
        P.dma(bgu[:], Wl["b_gu"][:, :, :], w=[R["bgu"]])
        P.dma(bdn[:], Wl["b_down"][:, :], w=[R["bdn"]])
        if nfin_d is not None:
            P.dma(nfin[:], nfin_d[:, :], w=[R["nfin"]])
        h2v = scr["h2T"].rearrange("(k p) s -> p k s", p=128)
        x1v = scr["x1T"].rearrange("(k p) s -> p k s", p=128)
        ov = out_d.rearrange("(k p) s -> p k s", p=128)
        rwv_ = scr["rwt"].rearrange("(n p) e -> p n e", p=128)
        wi = 0
        ci = 0
        for si in range(S // ST):
            s0 = si * ST
            P.dma(h2[:], h2v[:, :, s0:s0 + ST], w=[R["h2"]])
            P.dma(rwt[:], rwv_[:, si * NTB:(si + 1) * NTB, :], w=[R["rwt"]])
            for tb in range(NTB):
                P.tr(psx[0:32, 0:128], rwt[:, tb, :], cst["ident"], r=[R["rwt"]], w=[R["psx"]])
                P.cp("dve", rwT[:], psx[0:32, 0:128], r=[R["psx"]], w=[R["rwT"]])
                for dh in range(2):
                    P.mm(psd[dh][:], rwT[:], bdn[:, dh * 512:(dh + 1) * 512], r=[R["rwT"], R["bdn"]], w=[R[f"psd{dh}"]])
                    P.cp("act", yacc[:, tb, dh * 512:(dh + 1) * 512], psd[dh][:], r=[R[f"psd{dh}"]], w=[R[f"yacc{tb}"]])
            for e in range(NE):
                wg, wd = wgu[wi % 2], wdn[wi % 2]
                wgr, wdr = R[f"wgu{wi % 2}"], R[f"wdn{wi % 2}"]
                wi += 1
                P.dma(wg[:], gub[e].rearrange("(k p) j -> p k j", p=128), w=[wgr])
                P.dma(wd[:], dnb[e].rearrange("(k p) j -> p k j", p=128), w=[wdr])
                for tt_ in range(ST // 512):
                    tsl = slice(tt_ * 512, (tt_ + 1) * 512)
                    at, atr = actT[ci % 2], R[f"act{ci % 2}"]
                    ci += 1
                    def fin(fc_):
                        j2 = fc_ % 2
                        P.stt(at[:, fc_, :], s_t[j2][:], 1.0 / 1.702, u_t[j2][:], ALU.mult, ALU.mult,
                              r=[R[f"s{j2}"], R[f"u{j2}"]], w=[atr])
                    for fc in range(8):
                        i2 = fc % 2
                        for k in range(8):
                            P.mm(psg[i2][:], wg[:, k, fc * 128:(fc + 1) * 128], h2[:, k, tsl], start=(k == 0), stop=(k == 7),
                                 r=[wgr, R["h2"]], w=[R[f"psg{i2}"]])
                        for k in range(8):
                            P.mm(psu[i2][:], wg[:, k, 1024 + fc * 128:1024 + (fc + 1) * 128], h2[:, k, tsl],
                                 start=(k == 0), stop=(k == 7), r=[wgr, R["h2"]], w=[R[f"psu{i2}"]])
                        P.ts("dve", g_t[i2][:], psg[i2][:], bgu[:, e, fc:fc + 1], 7.0, op0=ALU.add, op1=ALU.min,
                             r=[R[f"psg{i2}"], R["bgu"]], w=[R[f"g{i2}"]])
                        P.act(s_t[i2][:], g_t[i2][:], AF.Silu, scale=1.702, r=[R[f"g{i2}"]], w=[R[f"s{i2}"]])
                        P.ts("dve", u_t[i2][:], psu[i2][:], bgu[:, e, 8 + fc:9 + fc], 7.0, op0=ALU.add, op1=ALU.min,
                             r=[R[f"psu{i2}"], R["bgu"]], w=[R[f"u{i2}"]])
                        P.ts("dve", u_t[i2][:], u_t[i2][:], -7.0, 1.0, op0=ALU.max, op1=ALU.add, r=[R[f"u{i2}"]], w=[R[f"u{i2}"]])
                        if fc >= 1:
                            fin(fc - 1)
                    fin(7)
                    for sub in range(4):
                        tb = tt_ * 4 + sub
                        for dh in range(2):
                            for fc in range(8):
                                P.mm(psd[dh][:], at[:, fc, sub * 128:(sub + 1) * 128], wd[:, fc, dh * 512:(dh + 1) * 512],
                                     start=(fc == 0), stop=(fc == 7), r=[atr, wdr], w=[R[f"psd{dh}"]])
                            P.stt(yacc[:, tb, dh * 512:(dh + 1) * 512], psd[dh][:], rwt[:, tb, e:e + 1],
                                  yacc[:, tb, dh * 512:(dh + 1) * 512], ALU.mult, ALU.add,
                                  r=[R[f"psd{dh}"], R["rwt"], R[f"yacc{tb}"]], w=[R[f"yacc{tb}"]])
            for tb in range(NTB):
                t0 = s0 + tb * 128
                P.dma(x1t[:], x1v[:, :, t0:t0 + 128], w=[R["x1t"]])
                for k in range(8):
                    P.tr(psx[:, k * 128:(k + 1) * 128], yacc[:, tb, k * 128:(k + 1) * 128], cst["ident"],
                         r=[R[f"yacc{tb}"]], w=[R["psx"]])
                for k in range(8):
                    P.stt(x2t[:, k, :], psx[:, k * 128:(k + 1) * 128], pt[:, 5, k:k + 1], x1t[:, k, :], ALU.mult, ALU.add,
                          r=[R["psx"], R["x1t"]], w=[R["x2t"]])
                if nfin_d is None:
                    P.dma(ov[:, :, t0:t0 + 128], x2t[:], r=[R["x2t"]], w=[P.region()])
                else:
                    P.act(sq[:], x2t[:], AF.Square, r=[R["x2t"]], w=[R["sq"]])
                    for k in range(8):
                        P.mm(psd[0][:, :128], cst["ones"], sq[:, k, :], start=(k == 0), stop=(k == 7), r=[R["sq"]], w=[R["psd0"]])
                    P.act(rs[:], psd[0][:, :128], AF.Ln, scale=1.0 / 1024, bias=cst["eps"], r=[R["psd0"]], w=[R["rs"]])
                    P.act(rs[:], rs[:], AF.Exp, scale=-0.5, r=[R["rs"]], w=[R["rs"]])
                    for k in range(8):
                        P.stt(o_t[:, k, :], x2t[:, k, :], nfin[:, k:k + 1], rs[:], ALU.mult, ALU.mult,
                              r=[R["x2t"], R["rs"], R["nfin"], R["sq"]], w=[R["sq"]])
                    P.dma(ov[:, :, t0:t0 + 128], o_t[:], r=[R["sq"]], w=[P.region()], is_out=True)
    P.end_phase()
from concourse.bass_utils import run_bass_kernel_spmd

LAYER_W = ["w_ada", "b_ada", "norm_mix", "norm_moe", "w_in", "conv_w", "conv_b", "lru_wa", "lru_ba", "lru_wx",
           "lru_bx", "lru_lambda", "rw_mu", "rw_w0", "rw_w_up", "rw_a0", "rw_a_up", "rw_g_up", "rw_k_k", "rw_k_a",
           "rw_r_k", "rw_lnx_w", "rw_lnx_b", "p_lru", "p_sb", "p_rwkv", "w_out", "w_router", "b_router",
           "w_gu", "b_gu", "w_down", "b_down"]


def fm(v):
    v = np.asarray(v, np.float32).reshape(-1, 128)
    return np.ascontiguousarray(v.T)


def bc(v):
    v = np.asarray(v, np.float32).reshape(1, -1)
    return np.ascontiguousarray(np.broadcast_to(v, (128, v.shape[1])))


def layer_inputs(inp, l):
    g = lambda n: np.asarray(inp[n][l], np.float32)
    d = {}
    d["w_ada"] = g("w_ada")
    d["b_ada"] = fm(g("b_ada"))
    d["norm_mix"] = fm(g("norm_mix"))
    d["norm_moe"] = fm(g("norm_moe"))
    d["w_in"] = g("w_in")
    d["mu"] = bc(g("rw_mu"))
    cw = g("conv_w")
    vecs = [cw[0], cw[1], cw[2], cw[3], g("conv_b"), g("lru_ba"), g("lru_bx"), g("lru_lambda")]
    d["lruv"] = np.ascontiguousarray(np.stack([fm(v) for v in vecs], axis=1))
    for nm in ("lru_wa", "lru_wx"):
        w = g(nm)
        bd = np.zeros((128, 8, 128), np.float32)
        for c in range(8):
            bd[0:64, c, 0:64] = w[2 * c]
            bd[64:128, c, 64:128] = w[2 * c + 1]
        d[nm] = bd
    d["rwv"] = np.ascontiguousarray(np.stack(
        [bc(g(n).reshape(-1)) for n in ("rw_w0", "rw_a0", "rw_k_k", "rw_k_a", "rw_r_k", "rw_lnx_w", "rw_lnx_b")], axis=1))
    d["rw_w_up"] = g("rw_w_up")
    d["rw_a_up"] = g("rw_a_up")
    d["rw_g_up"] = g("rw_g_up")
    for nm in ("p_lru", "p_sb", "p_rwkv", "w_out", "w_router", "w_gu", "w_down", "b_down"):
        d[nm] = g(nm)
    d["b_router"] = bc(g("b_router"))
    bg = g("b_gu")
    d["b_gu"] = np.ascontiguousarray(bg.reshape(32, 16, 128).transpose(2, 0, 1))
    return d


LAYER_SHAPES = {
    "w_ada": (1024, 6144), "b_ada": (128, 48), "norm_mix": (128, 8), "norm_moe": (128, 8), "w_in": (1024, 11520),
    "mu": (128, 3328), "lruv": (128, 8, 8), "lru_wa": (128, 8, 128), "lru_wx": (128, 8, 128),
    "rwv": (128, 7, 1024), "rw_w_up": (64, 1024), "rw_a_up": (64, 1024), "rw_g_up": (128, 1024),
    "p_lru": (1024, 1024), "p_sb": (1024, 1024), "p_rwkv": (1024, 1024), "w_out": (1024, 1024),
    "w_router": (1024, 32), "w_gu": (32, 1024, 2048), "w_down": (32, 1024, 1024), "b_down": (32, 1024),
    "b_router": (128, 32), "b_gu": (128, 32, 16),
}


def build(S, depth, debug=False, phases=None, split=False):
    nc = bass.Bass("TRN2", target_bir_lowering=False)
    P = Prog(nc)
    C = Ctx(nc, P, S, debug)
    cst_np, ccols = make_consts()
    NCC = cst_np.shape[1]
    xT_d = C.din("xT", [1024, S])
    cvec_d = C.din("cvec", [128, 8])
    nfin_d = C.din("norm_final", [128, 8])
    cst_d = C.din("consts", [128, NCC])
    class LazyW(dict):
        def __init__(self, l):
            super().__init__()
            self.l = l

        def __missing__(self, n):
            ap = C.din(f"{n}_{self.l}", LAYER_SHAPES[n])
            self[n] = ap
            C.declared.add(f"{n}_{self.l}")
            return ap
    C.declared = set()
    W = [LazyW(l) for l in range(depth)]
    S_out = S // 2 if split else S
    out_d = C.dout("outT", [1024, S_out])
    flag_d = C.din("flag", [128, 1]) if split else None
    scr = {
        "lruT": C.dscr("s_lruT", [2048, S], F32),
        "qT": C.dscr("s_qT", [1024, S], BF16),
        "kT": C.dscr("s_kT", [1024, S], BF16),
        "v": C.dscr("s_v", [S, 1024], BF16),
        "rkv": C.dscr("s_rkv", [S, 3072], F32),
        "loraT": C.dscr("s_loraT", [256, S], BF16),
        "gatesT": C.dscr("s_gatesT", [3072, S], F32),
        "yaT": C.dscr("s_yaT", [1024, S], BF16),
        "ybT": C.dscr("s_ybT", [1024, S], BF16),
        "ycT": C.dscr("s_ycT", [1024, S], BF16),
        "x1T": C.dscr("s_x1T", [1024, S], F32),
        "x2T": C.dscr("s_x2T", [1024, S], F32),
        "h2T": C.dscr("s_h2T", [1024, S], BF16),
        "rwt": C.dscr("s_rwt", [S, 32], F32),
        "gub": nc.dram_tensor("s_gub", [32, 1024, 2048], BF16, kind="Internal").ap(),
        "dnb": nc.dram_tensor("s_dnb", [32, 1024, 1024], BF16, kind="Internal").ap(),
    }
    with contextlib.ExitStack() as top:
        C.stack = top
        so, sw = ccols.pop("__sbm__")
        NCA = so
        cst_t = top.enter_context(nc.sbuf_tensor("cst", [128, NCA], F32))
        cstb_t = top.enter_context(nc.sbuf_tensor("cstb", [128, NCA], BF16))
        sbm_t = top.enter_context(nc.sbuf_tensor("sbm", [128, sw], BF16))
        eps_t = top.enter_context(nc.sbuf_tensor("eps", [128, 1], F32))
        per_t = top.enter_context(nc.sbuf_tensor("per", [128, 6, 8], F32))
        R0 = C.regs("init_")
        with contextlib.ExitStack() as st0:
            tmpc = st0.enter_context(nc.sbuf_tensor("cst_tmp", [128, sw], F32))
            P.dma(cst_t[:], cst_d[:, 0:NCA], w=[R0["cst"]])
            P.cp("dve", cstb_t[:], cst_t[:], r=[R0["cst"]], w=[R0["cstb"]])
            P.dma(tmpc[:], cst_d[:, so:so + sw], w=[R0["tmpc"]])
            P.cp("dve", sbm_t[:], tmpc[:], r=[R0["tmpc"]], w=[R0["sbm"]])
            P.memset("pool", eps_t[:], EPS, w=[R0["eps"]])
            P.end_phase()
        cst = {"eps": eps_t[:], "sbmask_b": sbm_t[:]}
        for n, (o, wd) in ccols.items():
            cst[n] = cst_t[:, o:o + wd]
            cst[n + "_b"] = cstb_t[:, o:o + wd]
        per = {"t": per_t, "reg": P.region("per", persistent=True)}
        x_in = xT_d
        for l in range(depth):
            Wl = W[l]
            if phases is None or "ada" in phases:
                phase_adaln(C, l, cst, cvec_d, Wl["w_ada"], Wl["b_ada"], Wl["norm_mix"], Wl["norm_moe"], per)
            if phases is None or "proj" in phases:
                phase_proj(C, l, cst, x_in, Wl["w_in"], Wl["mu"], per, scr)
            if phases is None or "lru" in phases:
                phase_lru(C, l, cst, Wl, scr)
            if phases is None or "sb" in phases:
                phase_sb(C, l, cst, scr)
            if phases is None or "rwkv" in phases:
                phase_rwkv(C, l, cst, Wl, scr)
            last = (l == depth - 1)
            if phases is None or "merge" in phases:
                phase_merge(C, l, cst, Wl, x_in, per, scr, sel=(flag_d if (split and last) else None))
            if phases is None or "moe" in phases:
                phase_moe(C, l, cst, Wl, per, scr, nfin_d if last else None, out_d if last else scr["x2T"],
                          S_eff=(S_out if last else S))
            x_in = scr["x2T"]
        if phases is not None and "moe" not in phases:
            with contextlib.ExitStack() as st:
                C.stack = st
                z = C.sb("zz", [128, 8])
                rz = P.region()
                P.memset("pool", z[:], 0.0, w=[rz])
                P.dma(out_d[0:128, 0:8], z[:], r=[rz], w=[P.region()], is_out=True)
        P.emit()
    nc.declared_inputs = set(C.declared)
    return nc, cst_np


def core_inputs(inp, b, S, depth, cst_np, layer_cache, declared=None):
    m = {"xT": np.ascontiguousarray(np.asarray(inp["x"][b, :S], np.float32).T),
         "cvec": fm(np.asarray(inp["c"][b], np.float32)),
         "norm_final": fm(np.asarray(inp["norm_final"], np.float32)),
         "consts": cst_np}
    for l in range(depth):
        for n, a in layer_cache[l].items():
            if declared is None or f"{n}_{l}" in declared:
                m[f"{n}_{l}"] = a
    return m


N_ACTIVE = 4


def kernel(**inputs):
    S, depth, B = 8192, 2, 4
    nc, cst_np = build(S, depth, split=True)
    lc = [layer_inputs(inputs, l) for l in range(depth)]
    in_maps = []
    for c in range(2 * B):
        m = dict(core_inputs(inputs, c // 2, S, depth, cst_np, lc))
        m["flag"] = np.full((128, 1), float(c % 2), np.float32)
        in_maps.append(m)
    res = run_bass_kernel_spmd(nc, in_maps, core_ids=list(range(2 * B)))
    out = np.empty((B, S, 1024), np.float32)
    h = S // 2
    for c in range(2 * B):
        out[c // 2, (c % 2) * h:(c % 2 + 1) * h, :] = np.asarray(res.results[c]["outT"], np.float32).T
    return out
prog_extend(Prog)
```
